# Optimizing a Trainium2 kernel written in Bass

```python
import math
import jax, jax.numpy as jnp
from jax import lax
import numpy as np

D_MODEL = 1024
BATCH = 4
SEQ = 8192
DEPTH = 2

CHUNK = 64
Q_BLOCK = 128
LN_EPS = 1e-5
SB_HEADS = 8
SB_HEAD_DIM = 64
SB_WIDTH = SB_HEADS * SB_HEAD_DIM
RET_HEADS = 4
RET_HEAD_DIM = 128
RET_WIDTH = RET_HEADS * RET_HEAD_DIM
EVEN_IN = 3 * SB_WIDTH + 4 * RET_WIDTH
EVEN_MIX = SB_WIDTH + RET_WIDTH
DIFF_HEADS = 8
DIFF_HEAD_DIM = 64
DIFF_V_DIM = 2 * DIFF_HEAD_DIM
DIFF_QK = DIFF_HEADS * 2 * DIFF_HEAD_DIM
DIFF_VW = DIFF_HEADS * DIFF_V_DIM
ODD_IN = 2 * DIFF_QK + DIFF_VW
N_GROUPS = 4
EXPERTS_PER_GROUP = 8
N_EXPERTS = N_GROUPS * EXPERTS_PER_GROUP
TOP_K = 2
EXPERT_FF = 512
MOE_BLOCK = 128
N_EVEN = (DEPTH + 1) // 2
N_ODD = DEPTH // 2

kernel_name = 'hybrid_sb_retention_diffattn_hmoe'


def layer_norm(x, g, b):
    xf = x.astype(jnp.float32)
    mu = jnp.mean(xf, axis=-1, keepdims=True)
    var = jnp.mean(jnp.square(xf - mu), axis=-1, keepdims=True)
    y = (xf - mu) * lax.rsqrt(var + LN_EPS) * g.astype(jnp.float32) + b.astype(jnp.float32)
    return y.astype(x.dtype)


def split_cols(a, sizes):
    out = []
    o = 0
    for s in sizes:
        out.append(a[..., o:o + s])
        o += s
    return out


def stick_breaking_attention(q, k, v):
    B, S, H, dh = q.shape
    nb = S // Q_BLOCK
    kh = k.transpose(0, 2, 1, 3)
    vh = v.transpose(0, 2, 1, 3)
    q_blocks = jnp.moveaxis(q.transpose(0, 2, 1, 3).reshape(B, H, nb, Q_BLOCK, dh), 2, 0)
    key_pos = jnp.arange(S)
    scale = dh ** -0.5

    def block(args):
        qb, start = args
        z = jnp.einsum('bhqd,bhkd->bhqk', qb, kh, preferred_element_type=jnp.float32) * scale
        q_pos = start + jnp.arange(Q_BLOCK)
        mask = key_pos[None, :] < q_pos[:, None]
        log_1m_beta = jnp.where(mask, jax.nn.log_sigmoid(-z), 0.0)
        between = lax.cumsum(log_1m_beta, axis=3, reverse=True) - log_1m_beta
        w = jnp.where(mask, jnp.exp(jax.nn.log_sigmoid(z) + between), 0.0)
        return jnp.einsum('bhqk,bhkd->bhqd', w.astype(vh.dtype), vh)

    out = lax.map(block, (q_blocks, jnp.arange(nb) * Q_BLOCK))
    return jnp.moveaxis(out, 0, 2).reshape(B, H, S, dh).transpose(0, 2, 1, 3).reshape(B, S, H * dh)


def retention(q, k, v, g, gn_gain):
    B, S, H, d = q.shape
    nc = S // CHUNK
    f32 = jnp.float32

    def chunked(t):
        return t.astype(f32).transpose(0, 2, 1, 3).reshape(B, H, nc, CHUNK, d)

    qc = chunked(q)
    kc = chunked(k) * (d ** -0.5)
    vc = chunked(v)
    log_gamma = jnp.log1p(-jnp.exp2(-5.0 - jnp.arange(H, dtype=f32)))
    pos = jnp.arange(CHUNK, dtype=f32)
    intra_decay = jnp.exp(log_gamma[:, None, None] * jnp.abs(pos[:, None] - pos[None, :]))
    scores = jnp.einsum('bhncd,bhnsd->bhncs', qc, kc) * intra_decay[None, :, None]
    intra = jnp.einsum('bhncs,bhnse->bhnce', scores, vc)
    k_decay = jnp.exp(log_gamma[:, None] * (CHUNK - 1 - pos)[None, :])
    q_decay = jnp.exp(log_gamma[:, None] * (pos + 1.0)[None, :])
    chunk_kv = jnp.einsum('bhnsd,bhnse->nbhde', kc * k_decay[None, :, None, :, None], vc)
    chunk_decay = jnp.exp(log_gamma * CHUNK)[None, :, None, None]

    def step(state, kv):
        return chunk_decay * state + kv, state

    _, prev_state = lax.scan(step, jnp.zeros((B, H, d, d), f32), chunk_kv)
    inter = jnp.einsum('bhncd,nbhde->bhnce', qc * q_decay[None, :, None, :, None], prev_state)
    o = (intra + inter).reshape(B, H, S, d)
    mu = jnp.mean(o, axis=-1, keepdims=True)
    var = jnp.mean(jnp.square(o - mu), axis=-1, keepdims=True)
    o = ((o - mu) * lax.rsqrt(var + LN_EPS)).transpose(0, 2, 1, 3).reshape(B, S, H * d)
    o = o * gn_gain.astype(f32) * jax.nn.silu(g.astype(f32).reshape(B, S, H * d))
    return o.astype(q.dtype)


def differential_attention(q, k, v, lam, lambda_init, subln_gain):
    B, S, H, _, dh = q.shape
    dv = v.shape[-1]
    nb = S // Q_BLOCK
    f32 = jnp.float32
    kh = k.transpose(0, 2, 3, 1, 4)
    vh = v.transpose(0, 2, 1, 3)
    q_blocks = jnp.moveaxis(q.transpose(0, 2, 3, 1, 4).reshape(B, H, 2, nb, Q_BLOCK, dh), 3, 0)
    slopes = jnp.exp2(-8.0 / H * (jnp.arange(H, dtype=f32) + 1.0))
    key_pos = jnp.arange(S)
    key_chunk = key_pos // CHUNK
    scale = dh ** -0.5

    def block(args):
        qb, start = args
        z = jnp.einsum('bhmqd,bhmkd->bhmqk', qb, kh, preferred_element_type=f32) * scale
        q_pos = start + jnp.arange(Q_BLOCK)
        dist = jnp.abs(q_pos[:, None] - key_pos[None, :]).astype(f32)
        z = z - slopes[:, None, None, None] * dist
        mask = key_chunk[None, :] <= (q_pos // CHUNK)[:, None]
        p = jax.nn.softmax(jnp.where(mask, z, -jnp.inf), axis=-1)
        a = p[:, :, 0] - lam * p[:, :, 1]
        return jnp.einsum('bhqk,bhkd->bhqd', a.astype(vh.dtype), vh)

    out = lax.map(block, (q_blocks, jnp.arange(nb) * Q_BLOCK))
    o = jnp.moveaxis(out, 0, 2).reshape(B, H, S, dv).astype(f32)
    o = o * lax.rsqrt(jnp.mean(jnp.square(o), axis=-1, keepdims=True) + LN_EPS)
    o = o * subln_gain.astype(f32) * (1.0 - lambda_init)
    return o.transpose(0, 2, 1, 3).reshape(B, S, H * dv).astype(v.dtype)


def hierarchical_moe(h, w_group, b_group, w_router, b_router, w1, w3, w2):
    B, S, D = h.shape
    T = B * S
    f32 = jnp.float32
    xt = h.reshape(T, D)
    group_logits = (xt @ w_group).astype(f32) + b_group.astype(f32)
    grp = jnp.argmax(group_logits, axis=-1)
    p_grp = jnp.take_along_axis(jax.nn.softmax(group_logits, axis=-1), grp[:, None], axis=1)[:, 0]
    expert_logits = ((xt @ w_router).astype(f32) + b_router.astype(f32)).reshape(T, N_GROUPS, EXPERTS_PER_GROUP)
    in_group = jnp.take_along_axis(expert_logits, grp[:, None, None], axis=1)[:, 0]
    top_vals, top_idx = lax.top_k(in_group, TOP_K)
    gates = jax.nn.softmax(top_vals, axis=-1) * p_grp[:, None]
    expert_id = (grp[:, None] * EXPERTS_PER_GROUP + top_idx).astype(jnp.int32)
    M = T * TOP_K
    flat_e = expert_id.reshape(M)
    flat_tok = jnp.arange(M, dtype=jnp.int32) // TOP_K
    order = jnp.argsort(flat_e)
    e_sorted = flat_e[order]
    tok_sorted = flat_tok[order]
    gate_sorted = gates.reshape(M)[order]
    counts = jnp.zeros((N_EXPERTS,), jnp.int32).at[flat_e].add(1)
    padded = (counts + MOE_BLOCK - 1) // MOE_BLOCK * MOE_BLOCK
    pad_end = jnp.cumsum(padded)
    pad_start = pad_end - padded
    start = jnp.cumsum(counts) - counts
    dest = pad_start[e_sorted] + jnp.arange(M, dtype=jnp.int32) - start[e_sorted]
    P = M + N_EXPERTS * MOE_BLOCK
    n_blk = P // MOE_BLOCK
    slot_tok = jnp.full((P,), T, jnp.int32).at[dest].set(tok_sorted)
    x_pad = jnp.concatenate([xt, jnp.zeros((1, D), xt.dtype)], axis=0)
    x_slots = x_pad[slot_tok].reshape(n_blk, MOE_BLOCK, D)
    blk_expert = jnp.minimum(
        jnp.searchsorted(pad_end, jnp.arange(n_blk, dtype=jnp.int32) * MOE_BLOCK, side='right'),
        N_EXPERTS - 1)

    def expert_block(args):
        xb, e = args
        return (jax.nn.silu(xb @ w1[e]) * (xb @ w3[e])) @ w2[e]

    y_slots = lax.map(expert_block, (x_slots, blk_expert)).reshape(P, D)
    y = y_slots[dest] * gate_sorted[:, None].astype(y_slots.dtype)
    return jax.ops.segment_sum(y, tok_sorted, num_segments=T).reshape(B, S, D)


def setup_inputs(seed: int = 0) -> dict:
    key = jax.random.key(seed)
    ks = jax.random.split(key, 26)
    D = D_MODEL
    beta = (8.0 * DEPTH) ** -0.25
    nrm = lambda k, shape, s: jax.random.normal(k, shape, jnp.float32) * s
    return {
        'x': nrm(ks[0], (BATCH, SEQ, D), 1.0),
        'c': nrm(ks[1], (BATCH, D), 1.0),
        'ln1_g': 1.0 + nrm(ks[2], (DEPTH, D), 0.02),
        'ln1_b': nrm(ks[3], (DEPTH, D), 0.02),
        'ln2_g': 1.0 + nrm(ks[4], (DEPTH, D), 0.02),
        'ln2_b': nrm(ks[5], (DEPTH, D), 0.02),
        'w_ada': nrm(ks[6], (DEPTH, D, 6 * D), 0.1 * D ** -0.5),
        'b_ada': nrm(ks[7], (DEPTH, 6 * D), 0.01),
        'even_w_in': nrm(ks[8], (N_EVEN, D, EVEN_IN), D ** -0.5),
        'even_w_out': nrm(ks[9], (N_EVEN, EVEN_MIX, D), beta * EVEN_MIX ** -0.5),
        'ret_gn_g': 1.0 + nrm(ks[10], (N_EVEN, RET_WIDTH), 0.02),
        'odd_w_in': nrm(ks[11], (N_ODD, D, ODD_IN), D ** -0.5),
        'odd_w_out': nrm(ks[12], (N_ODD, DIFF_VW, D), beta * DIFF_VW ** -0.5),
        'lambda_q1': nrm(ks[13], (N_ODD, DIFF_HEAD_DIM), 0.1),
        'lambda_k1': nrm(ks[14], (N_ODD, DIFF_HEAD_DIM), 0.1),
        'lambda_q2': nrm(ks[15], (N_ODD, DIFF_HEAD_DIM), 0.1),
        'lambda_k2': nrm(ks[16], (N_ODD, DIFF_HEAD_DIM), 0.1),
        'diff_subln_g': 1.0 + nrm(ks[17], (N_ODD, DIFF_V_DIM), 0.02),
        'moe_w_group': nrm(ks[18], (DEPTH, D, N_GROUPS), D ** -0.5),
        'moe_b_group': nrm(ks[19], (DEPTH, N_GROUPS), 0.01),
        'moe_w_router': nrm(ks[20], (DEPTH, D, N_EXPERTS), D ** -0.5),
        'moe_b_router': nrm(ks[21], (DEPTH, N_EXPERTS), 0.01),
        'moe_w1': nrm(ks[22], (DEPTH, N_EXPERTS, D, EXPERT_FF), D ** -0.5),
        'moe_w3': nrm(ks[23], (DEPTH, N_EXPERTS, D, EXPERT_FF), D ** -0.5),
        'moe_w2': nrm(ks[24], (DEPTH, N_EXPERTS, EXPERT_FF, D), beta * EXPERT_FF ** -0.5),
    }


def reference(x, c, ln1_g, ln1_b, ln2_g, ln2_b, w_ada, b_ada,
              even_w_in, even_w_out, ret_gn_g,
              odd_w_in, odd_w_out, lambda_q1, lambda_k1, lambda_q2, lambda_k2, diff_subln_g,
              moe_w_group, moe_b_group, moe_w_router, moe_b_router, moe_w1, moe_w3, moe_w2):
    B, S, D = x.shape
    alpha = (2.0 * DEPTH) ** 0.25
    cond = jax.nn.silu(c)
    for l in range(DEPTH):
        mod = (cond @ w_ada[l] + b_ada[l])[:, None, :]
        sh1, sc1, g1, sh2, sc2, g2 = jnp.split(mod, 6, axis=-1)
        u = x * (1.0 + sc1) + sh1
        i = l // 2
        if l % 2 == 0:
            proj = u @ even_w_in[i]
            sq, sk, sv, rq, rk, rv, rg = split_cols(proj, [SB_WIDTH] * 3 + [RET_WIDTH] * 4)
            sb_shape = (B, S, SB_HEADS, SB_HEAD_DIM)
            ret_shape = (B, S, RET_HEADS, RET_HEAD_DIM)
            a_out = stick_breaking_attention(sq.reshape(sb_shape), sk.reshape(sb_shape), sv.reshape(sb_shape))
            b_out = retention(rq.reshape(ret_shape), rk.reshape(ret_shape), rv.reshape(ret_shape),
                              rg.reshape(ret_shape), ret_gn_g[i])
            mix = jnp.concatenate([a_out, b_out], axis=-1) @ even_w_out[i]
        else:
            proj = u @ odd_w_in[i]
            dq, dk, dv = split_cols(proj, [DIFF_QK, DIFF_QK, DIFF_VW])
            lambda_init = 0.8 - 0.6 * math.exp(-0.3 * l)
            lam = (jnp.exp(jnp.sum(lambda_q1[i].astype(jnp.float32) * lambda_k1[i].astype(jnp.float32)))
                   - jnp.exp(jnp.sum(lambda_q2[i].astype(jnp.float32) * lambda_k2[i].astype(jnp.float32)))
                   + lambda_init)
            c_out = differential_attention(dq.reshape(B, S, DIFF_HEADS, 2, DIFF_HEAD_DIM),
                                           dk.reshape(B, S, DIFF_HEADS, 2, DIFF_HEAD_DIM),
                                           dv.reshape(B, S, DIFF_HEADS, DIFF_V_DIM),
                                           lam, lambda_init, diff_subln_g[i])
            mix = c_out @ odd_w_out[i]
        x = layer_norm(alpha * x + (1.0 + g1) * mix, ln1_g[l], ln1_b[l])
        u = x * (1.0 + sc2) + sh2
        f = hierarchical_moe(u, moe_w_group[l], moe_b_group[l], moe_w_router[l], moe_b_router[l],
                             moe_w1[l], moe_w3[l], moe_w2[l])
        x = layer_norm(alpha * x + (1.0 + g2) * f, ln2_g[l], ln2_b[l])
    return x
```

```python
from contextlib import ExitStack
import numpy as np
import ml_dtypes
import concourse.bass as bass
import concourse.mybir as mybir
from concourse.bass_utils import run_bass_kernel_spmd

F32 = mybir.dt.float32
BF16 = mybir.dt.bfloat16
I32 = mybir.dt.int32
AF = mybir.ActivationFunctionType
ALU = mybir.AluOpType
AX = mybir.AxisListType

D = 1024
S_LEN = 8192
NCORES = 8
ALPHA = (2.0 * 2) ** 0.25
EPS = 1e-5
NE = 32
CAP = 512
TOK = 4096
NT = TOK // 128
BIG = 1.0e30


class Sched:
    NDMA = 8
    ENGS = ("pe", "act", "dve", "pool", "sp")

    def __init__(self, nc, same_engine_sync=True):
        self.nc = nc
        self.ops = []
        self.state = {}
        self.same = same_engine_sync
        self.last = {}
        self.dma_since = set()

    def op(self, eng, fn, reads=(), writes=(), dma=False, nosame=False, extra=()):
        oid = len(self.ops)
        deps = set(extra)
        for k in reads:
            st = self.state.get(k)
            if st is not None and st[0] is not None:
                deps.add(st[0])
        for k in writes:
            st = self.state.get(k)
            if st is not None:
                if st[0] is not None:
                    deps.add(st[0])
                deps.update(st[1].values())
                deps.update(st[2])
        deps.discard(oid)
        self.ops.append(dict(eng=eng, fn=fn, deps=deps, dma=dma, nosame=nosame))
        for k in reads:
            st = self.state.setdefault(k, [None, {}, set()])
            if dma:
                st[2].add(oid)
            else:
                st[1][eng] = oid
        for k in writes:
            self.state[k] = [oid, {}, set()]
        if dma:
            self.dma_since.add(oid)
        else:
            self.last[eng] = oid
        return oid

    def dma(self, eng, fn, reads=(), writes=()):
        return self.op(eng, fn, reads, writes, dma=True)

    def cc(self, fn, reads=(), writes=()):
        oid = self.op("pool", fn, reads, writes, dma=True)
        self.ops[oid]["cc"] = True
        return oid

    def barrier(self):
        deps = set(self.last.values()) | self.dma_since
        self.dma_since = set()
        for e in self.ENGS:
            self.op(e, lambda eng: None, extra=deps, nosame=False)
        self.state = {}

    def finalize(self, sems):
        ops = self.ops

        def skip_same(o, po):
            if po["dma"] or o["dma"] or po["eng"] != o["eng"]:
                return False
            return o["eng"] in ("pe", "sp") or not self.same or o["nosame"]

        signal = [False] * len(ops)
        for o in ops:
            for p in o["deps"]:
                po = ops[p]
                if po["dma"] or skip_same(o, po):
                    continue
                signal[p] = True
        cnt = {e: 0 for e in self.ENGS}
        dcnt = {}
        event = [None] * len(ops)
        for i, o in enumerate(ops):
            if o.get("cc"):
                ncc = dcnt.get("cc", 0) + 1
                dcnt["cc"] = ncc
                event[i] = ("cc", ncc)
                o["prev"] = ("cc", ncc - 1) if ncc > 1 else None
            elif o["dma"]:
                e = o["eng"]
                n = dcnt.get(e, 0)
                dcnt[e] = n + 1
                j, m = n % self.NDMA, n // self.NDMA
                event[i] = (("dma", e, j), 16 * (m + 1))
                o["prev"] = (("dma", e, j), 16 * m) if m > 0 else None
            elif signal[i]:
                cnt[o["eng"]] += 1
                event[i] = (o["eng"], cnt[o["eng"]])
        seen = {e: {} for e in self.ENGS}
        per_eng = {e: [] for e in self.ENGS}
        nwaits = 0
        for i, o in enumerate(ops):
            e = o["eng"]
            need = {}
            for p in o["deps"]:
                po = ops[p]
                if skip_same(o, po):
                    continue
                ev = event[p]
                if ev is None:
                    continue
                if need.get(ev[0], 0) < ev[1]:
                    need[ev[0]] = ev[1]
            if o["dma"] and o["prev"] is not None:
                k, v = o["prev"]
                if need.get(k, 0) < v:
                    need[k] = v
            waits = []
            for k, v in need.items():
                if seen[e].get(k, 0) >= v:
                    continue
                seen[e][k] = v
                waits.append((k, v))
            nwaits += len(waits)
            per_eng[e].append((waits, o["fn"], event[i]))
        self.per_eng = per_eng
        self.sems = sems
        self.stats = dict(n_ops=len(ops), n_waits=nwaits, per_eng={e: len(v) for e, v in per_eng.items()})
        return per_eng

    def replay(self, ename, eng):
        sems = self.sems
        for waits, fn, ev in self.per_eng[ename]:
            for k, v in waits:
                eng.wait_ge(sems[k], v)
            ins = fn(eng)
            if ev is not None:
                if ins is None:
                    ins = eng.nop()
                ins.then_inc(sems[ev[0]], 16 if isinstance(ev[0], tuple) else 1)


class B:
    def __init__(self):
        self.nc = bass.Bass("TRN2", target_bir_lowering=False)
        self.S = Sched(self.nc)
        self.n = 0
        self.bregs = {}
        self.pfx = ""
        self.ovr = {}
        self.fused = False
        self.shared = {}
        self.no_act_copy = False

    def din(self, name, shape, dt):
        if name in self.ovr:
            return self.ovr[name]
        key = ("in", name, tuple(shape))
        if self.fused and name in ("ident",):
            if key not in self.shared:
                self.shared[key] = self.nc.dram_tensor(name, list(shape), dt, kind="ExternalInput").ap()
            return self.shared[key]
        return self.nc.dram_tensor(self.pfx + name, list(shape), dt, kind="ExternalInput").ap()

    def dout(self, name, shape, dt):
        if name in self.ovr:
            return self.ovr[name]
        if self.fused:
            return self.nc.dram_tensor(self.pfx + name, list(shape), dt, kind="Internal").ap()
        return self.nc.dram_tensor(name, list(shape), dt, kind="ExternalOutput").ap()

    def dscr(self, name, shape, dt):
        return self.nc.dram_tensor(self.pfx + name, list(shape), dt, kind="Internal").ap()

    def sb(self, es, name, shape, dt):
        return es.enter_context(self.nc.sbuf_tensor("sb_" + self.pfx + name, list(shape), dt))

    def ps(self, es, name, shape, dt):
        return es.enter_context(self.nc.psum_tensor("ps_" + self.pfx + name, list(shape), dt))

    def mm(self, out, lhsT, rhs, start, stop, r, w):
        self.S.op("pe", lambda e: e.matmul(out, lhsT=lhsT, rhs=rhs, start=start, stop=stop), r, w)

    def tr(self, out, in_, ident, r, w):
        self.S.op("pe", lambda e: e.transpose(out=out, in_=in_, identity=ident), r, w)

    def act(self, out, in_, func, r, w, bias=None, scale=1.0, accum=None):
        def f(e):
            kw = {}
            if bias is not None:
                kw["bias"] = bias
            if accum is not None:
                kw["accum_out"] = accum
            return e.activation(out=out, in_=in_, func=func, scale=scale, **kw)
        self.S.op("act", f, r, w)

    def tt(self, eng, out, in0, in1, op, r, w):
        self.S.op(eng, lambda e: e.tensor_tensor(out=out, in0=in0, in1=in1, op=op), r, w)

    def ts(self, eng, out, in0, s1, s2, op0, op1, r, w):
        if s2 is None:
            self.S.op(eng, lambda e: e.tensor_scalar(out=out, in0=in0, scalar1=s1, scalar2=None, op0=op0), r, w)
        else:
            self.S.op(eng, lambda e: e.tensor_scalar(out=out, in0=in0, scalar1=s1, scalar2=s2, op0=op0, op1=op1), r, w)

    def stt(self, eng, out, in0, scalar, in1, op0, op1, r, w):
        self.S.op(eng, lambda e: e.scalar_tensor_tensor(out=out, in0=in0, scalar=scalar, in1=in1, op0=op0, op1=op1), r, w)

    def copy(self, eng, out, in_, r, w):
        if eng == "act" and getattr(self, "no_act_copy", False):
            eng = "dve"
        if eng == "act":
            self.S.op("act", lambda e: e.activation(out=out, in_=in_, func=AF.Identity), r, w)
        else:
            self.S.op(eng, lambda e: e.tensor_copy(out=out, in_=in_), r, w)

    def memset(self, eng, out, val, w):
        self.S.op(eng, lambda e: e.memset(out, val), (), w)

    def red(self, eng, out, in_, op, r, w):
        if op == "max":
            self.S.op(eng, lambda e: e.reduce_max(out=out, in_=in_, axis=AX.X), r, w)
        else:
            self.S.op(eng, lambda e: e.reduce_sum(out=out, in_=in_, axis=AX.X), r, w)

    def dma(self, q, out, in_, r, w):
        self.S.dma(q, lambda e: e.dma_start(out=out, in_=in_), r, w)

    def _breg(self, e, bound):
        if bound not in self.bregs:
            self.bregs[bound] = e.to_reg(bound)
        return self.bregs[bound]

    def scatter(self, out_dram, idx, in_sb, bound, r, w):
        self.S.dma("pool", lambda e: e.indirect_dma_start(
            out=out_dram, out_offset=bass.IndirectOffsetOnAxis(ap=idx, axis=0), in_=in_sb, in_offset=None,
            bounds_check=self._breg(e, bound), oob_is_err=False), r, w)

    def gather(self, out_sb, in_dram, idx, bound, r, w):
        self.S.dma("pool", lambda e: e.indirect_dma_start(
            out=out_sb, out_offset=None, in_=in_dram, in_offset=bass.IndirectOffsetOnAxis(ap=idx, axis=0),
            bounds_check=self._breg(e, bound), oob_is_err=False), r, w)

    def layernorm(self, es_bufs, y, out, g_t, b_t, key_in, key_out, tag):
        st, mv, rstd, epsb = es_bufs["st"], es_bufs["mv"], es_bufs["rstd"], es_bufs["epsb"]
        S = self.S
        for c in range(2):
            S.op("dve", lambda e, c=c: e.bn_stats(out=st[:, c, :], in_=y[:, c * 512:(c + 1) * 512]),
                 [key_in], [("st", tag, c)])
        S.op("dve", lambda e: e.bn_aggr(out=mv[:], in_=st[:]), [("st", tag, 0), ("st", tag, 1)], [("mv", tag)])
        self.act(rstd[:], mv[:, 1:2], AF.Ln, [("mv", tag), "epsb"], [("rstd", tag)], bias=epsb[:])
        self.act(rstd[:], rstd[:], AF.Exp, [("rstd", tag)], [("rstd", tag)], scale=-0.5)
        self.ts("dve", out, y, mv[:, 0:1], rstd[:], ALU.subtract, ALU.mult, [key_in, ("mv", tag), ("rstd", tag)], [key_out])
        self.tt("pool", out, out, g_t, ALU.mult, [key_out, "lnp"], [key_out])
        self.tt("pool", out, out, b_t, ALU.add, [key_out, "lnp"], [key_out])

    def finish(self, out_keys):
        S = self.S
        nc = self.nc
        S.op("sp", lambda e: None, reads=list(out_keys))
        with ExitStack() as es:
            names = ["pe", "act", "dve", "pool", "sp", "cc"] + [("dma", q, j) for q in ("sp", "act", "pool") for j in range(Sched.NDMA)]
            sems = {n: es.enter_context(nc.semaphore("s_" + (n if isinstance(n, str) else "_".join(map(str, n))))) for n in names}
            S.finalize(sems)
            with nc.Block() as block:
                @block.tensor
                def _(e):
                    S.replay("pe", e)

                @block.scalar
                def _(e):
                    S.replay("act", e)

                @block.vector
                def _(e):
                    S.replay("dve", e)

                @block.gpsimd
                def _(e):
                    S.replay("pool", e)

                @block.sync
                def _(e):
                    S.replay("sp", e)
        return nc


def build_post(b=None, att_gather=None):
    b = b or B()
    S = b.S
    xs = b.din("xs", [TOK, D], F32)
    att = b.din("att", [TOK, D], BF16) if att_gather is None else None
    mod = b.din("mod", [6 * D], F32)
    w_out = b.din("w_out", [D, D], F32)
    lnp = b.din("lnp", [4, D], F32)
    wr = b.din("wr", [D, 36], F32)
    br = b.din("br", [36], F32)
    w1 = b.din("w1", [NE, D, 512], F32)
    w3 = b.din("w3", [NE, D, 512], F32)
    w2 = b.din("w2", [NE, 512, D], F32)
    ident_d = b.din("ident", [128, 128], F32)
    tri_d = b.din("tri", [128, 128], F32)
    eoff_d = b.din("eoff", [NE], F32)
    xo = b.dout("xo", [TOK, D], F32)
    X1 = b.dscr("X1", [TOK, D], F32)
    U = b.dscr("U", [TOK, D], BF16)
    US = b.dscr("US", [NE * CAP, D], BF16)
    YS = b.dscr("YS", [NE * CAP, D], F32)

    with ExitStack() as es0:
        ident = b.sb(es0, "ident", [128, 128], BF16)
        tri = b.sb(es0, "tri", [128, 128], BF16)
        ones = b.sb(es0, "ones", [128, 128], BF16)
        epsb = b.sb(es0, "epsb", [128, 1], F32)
        g1p = b.sb(es0, "g1p", [128, D], F32)
        sh2 = b.sb(es0, "sh2", [128, D], F32)
        sc2p = b.sb(es0, "sc2p", [128, D], F32)
        g2p = b.sb(es0, "g2p", [128, D], F32)
        lnt = b.sb(es0, "lnt", [128, 4, D], F32)
        st = b.sb(es0, "st", [128, 2, 6], F32)
        mv = b.sb(es0, "mv", [128, 2], F32)
        rstd = b.sb(es0, "rstd", [128, 1], F32)
        lnb = dict(st=st, mv=mv, rstd=rstd, epsb=epsb)
        gate1 = b.sb(es0, "gate1", [128, NT], F32)
        gate2 = b.sb(es0, "gate2", [128, NT], F32)
        dest1 = b.sb(es0, "dest1", [128, NT], I32)
        dest2 = b.sb(es0, "dest2", [128, NT], I32)

        b.dma("pool", ident[:], ident_d, [], ["ident"])
        b.dma("pool", tri[:], tri_d, [], ["tri"])
        if att_gather is not None:
            gidx_sb = b.sb(es0, "gidx", [128, 2, NT], I32)
            b.dma("sp", gidx_sb[:], att_gather[1], [], ["gidx"])
            att_gather = (att_gather[0], gidx_sb)
        b.memset("dve", ones[:], 1.0, ["ones"])
        b.memset("dve", epsb[:], EPS, ["epsb"])
        b.dma("sp", g1p[:], mod[2 * D:3 * D].partition_broadcast(128), [], ["g1p"])
        b.dma("sp", sh2[:], mod[3 * D:4 * D].partition_broadcast(128), [], ["sh2"])
        b.dma("sp", sc2p[:], mod[4 * D:5 * D].partition_broadcast(128), [], ["sc2p"])
        b.dma("sp", g2p[:], mod[5 * D:6 * D].partition_broadcast(128), [], ["g2p"])
        for i in range(4):
            b.dma("sp", lnt[:, i, :], lnp[i, :].partition_broadcast(128), [], ["lnp"] if i == 0 else [("lnp", i)])
        S.op("sp", lambda e: None, reads=[("lnp", 1), ("lnp", 2), ("lnp", 3)], writes=["lnp"])
        for t_, k_ in ((g1p, "g1p"), (sc2p, "sc2p"), (g2p, "g2p")):
            b.ts("dve", t_[:], t_[:], 1.0, None, ALU.add, None, [k_], [k_])

        esL = ExitStack()
        L_all = b.sb(esL, "L_all", [128, NT, 36], F32)
        with ExitStack() as es:
            wo = b.sb(es, "wo", [128, 8, D], BF16)
            wrt = b.sb(es, "wrt", [128, 8, 36], BF16)
            brt = b.sb(es, "brt", [128, 36], F32)
            xt = [b.sb(es, f"xt{i}", [128, D], F32) for i in range(2)]
            at = [b.sb(es, f"at{i}", [128, D], BF16) for i in range(2)]
            attT = [b.sb(es, f"attT{i}", [128, D], BF16) for i in range(2)]
            tmpA = b.sb(es, "tmpA", [128, D], F32)
            yA = b.sb(es, "yA", [128, D], F32)
            x1 = [b.sb(es, f"x1{i}", [128, D], F32) for i in range(2)]
            u2f = b.sb(es, "u2f", [128, D], F32)
            u2b = [b.sb(es, f"u2b{i}", [128, D], BF16) for i in range(2)]
            u2T = [b.sb(es, f"u2T{i}", [128, D], BF16) for i in range(2)]
            pT = [b.ps(es, f"pT{i}", [128, D], BF16) for i in range(2)]
            pmix = [[b.ps(es, f"pmix{i}{c}", [128, 512], F32) for c in range(2)] for i in range(2)]
            plog = b.ps(es, "plog", [128, 36], F32)

            b.dma("pool", wo[:], w_out.rearrange("(kc p) n -> p kc n", p=128), [], ["wo"])
            b.dma("pool", wrt[:], wr.rearrange("(kc p) n -> p kc n", p=128), [], ["wrt"])
            b.dma("sp", brt[:], br.partition_broadcast(128), [], ["brt"])

            def stage1(t):
                p = t % 2
                rows = slice(t * 128, (t + 1) * 128)
                b.dma("sp", xt[p][:], xs[rows, :], [], [("xt", p)])
                if att_gather is None:
                    b.dma("sp", at[p][:], att[rows, :], [], [("at", p)])
                else:
                    G_, gidx_ = att_gather
                    for r_ in range(2):
                        b.gather(at[p][:, r_ * 512:(r_ + 1) * 512], G_[:, :], gidx_[:, r_, t:t + 1], 2 * S_LEN - 1,
                                 ["gidx"] + ([("at", p)] if r_ == 0 else [("at", p, 0)]), [("at", p, 0)] if r_ == 0 else [("at", p)])
                for kc in range(8):
                    b.tr(pT[0][:, kc * 128:(kc + 1) * 128], at[p][:, kc * 128:(kc + 1) * 128], ident[:], [("at", p), "ident"], [("pT", 0)])
                b.copy("act", attT[p][:], pT[0][:], [("pT", 0)], [("attT", p)])
                for c in range(2):
                    for kc in range(8):
                        b.mm(pmix[p][c][:], attT[p][:, kc * 128:(kc + 1) * 128], wo[:, kc, c * 512:(c + 1) * 512],
                             kc == 0, kc == 7, [("attT", p), "wo"], [("pmix", p, c)])
                for c in range(2):
                    b.tt("dve", tmpA[:, c * 512:(c + 1) * 512], pmix[p][c][:], g1p[:, c * 512:(c + 1) * 512], ALU.mult,
                         [("pmix", p, c), "g1p"], [("tmpA", c)])
                b.stt("dve", yA[:], xt[p][:], ALPHA, tmpA[:], ALU.mult, ALU.add, [("xt", p), ("tmpA", 0), ("tmpA", 1)], ["yA"])
                b.layernorm(lnb, yA[:], x1[p][:], lnt[:, 0, :], lnt[:, 1, :], "yA", ("x1", p), "A")
                b.dma("sp", X1[rows, :], x1[p][:], [("x1", p)], [("X1", t)])
                b.tt("pool", u2f[:], x1[p][:], sc2p[:], ALU.mult, [("x1", p), "sc2p"], ["u2f"])
                b.tt("pool", u2b[p][:], u2f[:], sh2[:], ALU.add, ["u2f", "sh2"], [("u2b", p)])
                b.dma("sp", U[rows, :], u2b[p][:], [("u2b", p)], [("U", t)])

            def stage2(t):
                p = t % 2
                for kc in range(8):
                    b.tr(pT[1][:, kc * 128:(kc + 1) * 128], u2b[p][:, kc * 128:(kc + 1) * 128], ident[:], [("u2b", p), "ident"], [("pT", 1)])
                b.copy("act", u2T[p][:], pT[1][:], [("pT", 1)], [("u2T", p)])
                for kc in range(8):
                    b.mm(plog[:], u2T[p][:, kc * 128:(kc + 1) * 128], wrt[:, kc, :], kc == 0, kc == 7, [("u2T", p), "wrt"], ["plog"])
                b.tt("dve", L_all[:, t, :], plog[:], brt[:], ALU.add, ["plog", "brt"], ["L_all"])


            stage1(0)
            for t in range(NT):
                if t + 1 < NT:
                    stage1(t + 1)
                stage2(t)
        S.barrier()
        if True:
            with ExitStack() as esb:
                def t3(name, last, dt=F32):
                    return b.sb(esb, name, [128, NT, last] if last else [128, NT], dt)
                gmax = t3("gmax", 0)
                ohg = t3("ohg", 4)
                eg = t3("eg", 4)
                sume = t3("sume", 0)
                pgrp = t3("pgrp", 0)
                pen = t3("pen", 4)
                Lm = t3("Lm", 32)
                m1 = t3("m1", 0)
                mask1 = t3("mask1", 32)
                Lm2 = t3("Lm2", 32)
                m2 = t3("m2", 0)
                mask2 = t3("mask2", 32)
                dd = t3("dd", 0)
                s1 = t3("s1", 0)
                s2 = t3("s2", 0)
                A_bf = t3("A_bf", 32, BF16)
                rank = t3("rank", 32)
                tmp3 = t3("tmp3", 32)
                eoff = b.sb(esb, "eoff", [128, NE], F32)
                r1 = t3("r1", 0)
                e1 = t3("e1", 0)
                ov = t3("ov", 0)
                df = t3("df", 0)
                pR = [b.ps(esb, f"pR{i}", [128, 16, 32], F32) for i in range(2)]

                b.dma("sp", eoff[:], eoff_d.partition_broadcast(128), [], ["eoff"])
                Lg = L_all[:, :, 0:4]
                Le = L_all[:, :, 4:36]
                bc4 = lambda a: a.unsqueeze(2).to_broadcast([128, NT, 4])
                bc32 = lambda a: a.unsqueeze(2).to_broadcast([128, NT, 32])
                b.red("dve", gmax[:], Lg, "max", ["L_all"], ["gmax"])
                b.tt("dve", ohg[:], Lg, bc4(gmax[:]), ALU.is_equal, ["L_all", "gmax"], ["ohg"])
                b.tt("dve", eg[:], Lg, bc4(gmax[:]), ALU.subtract, ["L_all", "gmax"], ["eg"])
                b.act(eg[:], eg[:], AF.Exp, ["eg"], ["eg"])
                b.red("dve", sume[:], eg[:], "sum", ["eg"], ["sume"])
                S.op("dve", lambda e: e.reciprocal(out=pgrp[:], in_=sume[:]), ["sume"], ["pgrp"])
                b.ts("dve", pen[:], ohg[:], BIG, -BIG, ALU.mult, ALU.add, ["ohg"], ["pen"])
                b.tt("dve", Lm[:].rearrange("p t (g e) -> p t g e", g=4), Le.rearrange("p t (g e) -> p t g e", g=4),
                     pen[:].unsqueeze(3).to_broadcast([128, NT, 4, 8]), ALU.add, ["L_all", "pen"], ["Lm"])
                b.red("dve", m1[:], Lm[:], "max", ["Lm"], ["m1"])
                b.tt("dve", mask1[:], Lm[:], bc32(m1[:]), ALU.is_equal, ["Lm", "m1"], ["mask1"])
                b.stt("dve", Lm2[:], mask1[:], -BIG, Lm[:], ALU.mult, ALU.add, ["mask1", "Lm"], ["Lm2"])
                b.red("dve", m2[:], Lm2[:], "max", ["Lm2"], ["m2"])
                b.tt("dve", mask2[:], Lm2[:], bc32(m2[:]), ALU.is_equal, ["Lm2", "m2"], ["mask2"])
                b.tt("dve", dd[:], m2[:], m1[:], ALU.subtract, ["m1", "m2"], ["dd"])
                b.act(dd[:], dd[:], AF.Exp, ["dd"], ["dd"])
                b.ts("dve", dd[:], dd[:], 1.0, None, ALU.add, None, ["dd"], ["dd"])
                S.op("dve", lambda e: e.reciprocal(out=s1[:], in_=dd[:]), ["dd"], ["s1"])
                b.ts("dve", s2[:], s1[:], -1.0, 1.0, ALU.mult, ALU.add, ["s1"], ["s2"])
                b.tt("dve", gate1[:], s1[:], pgrp[:], ALU.mult, ["s1", "pgrp"], ["gate1"])
                b.tt("dve", gate2[:], s2[:], pgrp[:], ALU.mult, ["s2", "pgrp"], ["gate2"])
                b.tt("dve", A_bf[:], mask1[:], mask2[:], ALU.add, ["mask1", "mask2"], ["A_bf"])
                for t in range(NT):
                    reg = pR[t // 16][:, t % 16, :]
                    for tp in range(t):
                        b.mm(reg, ones[:], A_bf[:, tp, :], tp == 0, False, ["ones", "A_bf"], [("pR", t)])
                    b.mm(reg, tri[:], A_bf[:, t, :], t == 0, True, ["tri", "A_bf"], [("pR", t)])
                for h in range(2):
                    b.copy("dve", rank[:, h * 16:(h + 1) * 16, :], pR[h][:], [("pR", t) for t in range(h * 16, (h + 1) * 16)], [("rank", h)])
                rk = [("rank", 0), ("rank", 1)]
                for (mk, mkk, dst, gate, gk) in ((mask1, "mask1", dest1, gate1, "gate1"), (mask2, "mask2", dest2, gate2, "gate2")):
                    b.tt("dve", tmp3[:], mk[:], rank[:], ALU.mult, [mkk] + rk, ["tmp3"])
                    b.red("dve", r1[:], tmp3[:], "sum", ["tmp3"], ["r1"])
                    b.tt("dve", tmp3[:], mk[:], eoff[:].unsqueeze(1).to_broadcast([128, NT, 32]), ALU.mult, [mkk, "eoff"], ["tmp3"])
                    b.red("dve", e1[:], tmp3[:], "sum", ["tmp3"], ["e1"])
                    b.ts("dve", ov[:], r1[:], float(CAP), None, ALU.is_ge, None, ["r1"], ["ov"])
                    b.tt("dve", df[:], r1[:], e1[:], ALU.add, ["r1", "e1"], ["df"])
                    b.stt("dve", df[:], ov[:], 1.0e6, df[:], ALU.mult, ALU.add, ["ov", "df"], ["df"])
                    b.copy("dve", dst[:], df[:], ["df"], [gk + "d"])
                    b.ts("dve", ov[:], ov[:], -1.0, 1.0, ALU.mult, ALU.add, ["ov"], ["ov"])
                    b.tt("dve", gate[:], gate[:], ov[:], ALU.mult, [gk, "ov"], [gk])
        S.barrier()
        esL.close()

        with ExitStack() as es:
            ut = [b.sb(es, f"ut{i}", [128, D], BF16) for i in range(4)]
            for t in range(NT):
                p = t % 4
                b.dma("sp", ut[p][:], U[t * 128:(t + 1) * 128, :], [("U", t)], [("ut", p)])
                b.scatter(US[:, :], dest1[:, t:t + 1], ut[p][:, :], NE * CAP - 1, [("ut", p), "gate1d"], ["US"])
                b.scatter(US[:, :], dest2[:, t:t + 1], ut[p][:, :], NE * CAP - 1, [("ut", p), "gate2d"], ["US"])
        S.barrier()

        NJ = CAP // 128
        with ExitStack() as es:
            w1e = [b.sb(es, f"w1e{i}", [128, 8, 512], BF16) for i in range(2)]
            w3e = [b.sb(es, f"w3e{i}", [128, 8, 512], BF16) for i in range(2)]
            w2e = [b.sb(es, f"w2e{i}", [128, 4, D], BF16) for i in range(2)]
            us = [b.sb(es, f"us{i}", [128, D], BF16) for i in range(4)]
            uT = [b.sb(es, f"uT{i}", [128, 8, CAP], BF16) for i in range(2)]
            sil = [b.sb(es, f"sil{i}", [128, CAP], F32) for i in range(2)]
            hT = [b.sb(es, f"hT{i}", [128, 4, CAP], BF16) for i in range(2)]
            ysb = [b.sb(es, f"ysb{i}", [128, D], F32) for i in range(2)]
            pT = [b.ps(es, f"pTe{i}", [128, 8, 128], BF16) for i in range(2)]
            pa = [b.ps(es, f"pa{i}", [128, CAP], F32) for i in range(2)]
            pb = [b.ps(es, f"pb{i}", [128, CAP], F32) for i in range(2)]
            py = [b.ps(es, f"py{i}", [128, 512], F32) for i in range(2)]

            def load_w(e):
                q = e % 2
                b.dma("pool", w1e[q][:], w1[e].rearrange("(kc p) f -> p kc f", p=128), [], [("w1e", q)])
                b.dma("pool", w3e[q][:], w3[e].rearrange("(kc p) f -> p kc f", p=128), [], [("w3e", q)])
                b.dma("pool", w2e[q][:], w2[e].rearrange("(fc p) d -> p fc d", p=128), [], [("w2e", q)])

            load_w(0)
            nus = 0
            npy = 0
            for e in range(NE):
                q = e % 2
                if e + 1 < NE:
                    load_w(e + 1)
                for j in range(NJ):
                    ui = nus % 4
                    pi = nus % 2
                    nus += 1
                    r0 = e * CAP + j * 128
                    b.dma("sp", us[ui][:], US[r0:r0 + 128, :], ["US"], [("us", ui)])
                    for kc in range(8):
                        b.tr(pT[pi][:, kc, :], us[ui][:, kc * 128:(kc + 1) * 128], ident[:], [("us", ui), "ident"], [("pTe", pi)])
                    b.copy("act" if j % 2 == 0 else "dve", uT[q][:, :, j * 128:(j + 1) * 128], pT[pi][:], [("pTe", pi)], [("uT", q, j)])
                uTk = [("uT", q, j) for j in range(NJ)]
                for f in range(4):
                    fi = f % 2
                    for kc in range(8):
                        b.mm(pa[fi][:], w1e[q][:, kc, f * 128:(f + 1) * 128], uT[q][:, kc, :], kc == 0, kc == 7, uTk + [("w1e", q)], [("pa", fi)])
                    for kc in range(8):
                        b.mm(pb[fi][:], w3e[q][:, kc, f * 128:(f + 1) * 128], uT[q][:, kc, :], kc == 0, kc == 7, uTk + [("w3e", q)], [("pb", fi)])
                    if b.no_act_copy:
                        b.copy("dve", sil[fi][:], pa[fi][:], [("pa", fi)], [("sil", fi)])
                        b.act(sil[fi][:], sil[fi][:], AF.Silu, [("sil", fi)], [("sil", fi)])
                    else:
                        b.act(sil[fi][:], pa[fi][:], AF.Silu, [("pa", fi)], [("sil", fi)])
                    b.tt("dve", hT[q][:, f, :], sil[fi][:], pb[fi][:], ALU.mult, [("sil", fi), ("pb", fi)], [("hT", q, f)])
                hk = [("hT", q, f) for f in range(4)]
                for j in range(NJ):
                    yi = j % 2
                    for c in range(2):
                        pi = npy % 2
                        npy += 1
                        for f in range(4):
                            b.mm(py[pi][:], hT[q][:, f, j * 128:(j + 1) * 128], w2e[q][:, f, c * 512:(c + 1) * 512], f == 0, f == 3,
                                 hk + [("w2e", q)], [("py", pi)])
                        b.copy("act" if c == 0 else "dve", ysb[yi][:, c * 512:(c + 1) * 512], py[pi][:], [("py", pi)], [("ysb", yi, c)])
                    r0 = e * CAP + j * 128
                    b.dma("sp", YS[r0:r0 + 128, :], ysb[yi][:], [("ysb", yi, 0), ("ysb", yi, 1)], ["YS"])
        S.barrier()

        with ExitStack() as es:
            y1 = [b.sb(es, f"y1{i}", [128, D], F32) for i in range(2)]
            y2 = [b.sb(es, f"y2{i}", [128, D], F32) for i in range(2)]
            x1t = [b.sb(es, f"x1t{i}", [128, D], F32) for i in range(2)]
            fF = b.sb(es, "fF", [128, D], F32)
            zF = b.sb(es, "zF", [128, D], F32)
            oF = [b.sb(es, f"oF{i}", [128, D], F32) for i in range(2)]
            for i in range(2):
                b.memset("dve", y1[i][:], 0.0, [("y1", i)])
                b.memset("dve", y2[i][:], 0.0, [("y2", i)])
            for t in range(NT):
                p = t % 2
                rows = slice(t * 128, (t + 1) * 128)
                b.gather(y1[p][:, :], YS[:, :], dest1[:, t:t + 1], NE * CAP - 1, ["YS", "gate1d", ("y1", p)], [("y1", p)])
                b.gather(y2[p][:, :], YS[:, :], dest2[:, t:t + 1], NE * CAP - 1, ["YS", "gate2d", ("y2", p)], [("y2", p)])
                b.dma("sp", x1t[p][:], X1[rows, :], [("X1", t)], [("x1t", p)])
                b.ts("dve", fF[:], y1[p][:], gate1[:, t:t + 1], None, ALU.mult, None, [("y1", p), "gate1"], ["fF"])
                b.stt("dve", fF[:], y2[p][:], gate2[:, t:t + 1], fF[:], ALU.mult, ALU.add, [("y2", p), "gate2", "fF"], ["fF"])
                b.tt("pool", fF[:], fF[:], g2p[:], ALU.mult, ["fF", "g2p"], ["fF"])
                b.stt("dve", zF[:], x1t[p][:], ALPHA, fF[:], ALU.mult, ALU.add, [("x1t", p), "fF"], ["zF"])
                b.layernorm(lnb, zF[:], oF[p][:], lnt[:, 2, :], lnt[:, 3, :], "zF", ("oF", p), "F")
                b.dma("sp", xo[rows, :], oF[p][:], [("oF", p)], [("xo", t)])
        outs = [("xo", t) for t in range(NT)]
        if b.fused:
            S.op("sp", lambda e: None, reads=outs)
            S.barrier()
            return outs
        return b.finish(outs)


def post_consts():
    tri = np.triu(np.ones((128, 128), np.float32), 1)
    return dict(ident=np.eye(128, dtype=np.float32), tri=tri,
                eoff=(np.arange(NE) * CAP).astype(np.float32))


def emit_mod(b, es, c_row, w_ada, b_ada, mod_out, mod_scr=None, ps_name="pmod"):
    S = b.S
    with ExitStack() as esm:
        cf = b.sb(esm, "cf", [128, 8], F32)
        sg = b.sb(esm, "sg", [128, 8], F32)
        cb = b.sb(esm, "cb", [128, 8], BF16)
        wa = [b.sb(esm, f"wa{i}", [128, 8, 512], BF16) for i in range(2)]
        bad = b.sb(esm, "bad", [1, 6 * D], F32)
        modr = b.sb(esm, "modr", [1, 6 * D], F32)
        pm = [b.ps(esm, f"{ps_name}{i}", [1, 512], F32) for i in range(2)]
        b.dma("sp", cf[:], c_row.rearrange("(p kc) -> p kc", kc=8), [], ["cf"])
        b.dma("sp", bad[:], b_ada.rearrange("(o n) -> o n", o=1), [], ["bad"])
        b.act(sg[:], cf[:], AF.Exp, ["cf"], ["sg"], scale=-1.0)
        b.ts("dve", sg[:], sg[:], 1.0, None, ALU.add, None, ["sg"], ["sg"])
        S.op("dve", lambda e: e.reciprocal(out=sg[:], in_=sg[:]), ["sg"], ["sg"])
        b.tt("dve", cb[:], cf[:], sg[:], ALU.mult, ["cf", "sg"], ["cb"])
        for g in range(12):
            q = g % 2
            b.dma("pool", wa[q][:], w_ada[:, g * 512:(g + 1) * 512].rearrange("(p kc) n -> p kc n", kc=8), [], [("wa", q)])
            for kc in range(8):
                b.mm(pm[q][:], cb[:, kc:kc + 1], wa[q][:, kc, :], kc == 0, kc == 7, ["cb", ("wa", q)], [("pm", q)])
            b.tt("dve", modr[:, g * 512:(g + 1) * 512], pm[q][:], bad[:, g * 512:(g + 1) * 512], ALU.add, [("pm", q), "bad"], ["modr"])
        b.dma("sp", mod_out.rearrange("(o n) -> o n", o=1), modr[:], ["modr"], ["mod_d"])
        if mod_scr is not None:
            b.dma("sp", mod_scr.rearrange("(o n) -> o n", o=1), modr[:], ["modr"], ["mod_s"])
    S.barrier()


NG = S_LEN // 512
NB = S_LEN // 128


def emit_uT_group(b, g, x, sc1p, sh1, ident, xt, uf, ub, pT, uT, ubuf=0):
    for sub in range(4):
        r0 = g * 512 + sub * 128
        p = sub % 2
        b.dma("sp", xt[p][:], x[r0:r0 + 128, :], [], [("xt", p)])
        b.tt("dve", uf[p][:], xt[p][:], sc1p[:], ALU.mult, [("xt", p), "sc1p"], [("uf", p)])
        b.tt("pool", ub[p][:], uf[p][:], sh1[:], ALU.add, [("uf", p), "sh1"], [("ub", p)])
        for kc in range(8):
            b.tr(pT[p][:, kc, :], ub[p][:, kc * 128:(kc + 1) * 128], ident[:], [("ub", p), "ident"], [("pT", p)])
        b.copy("act" if sub % 2 == 0 else "dve", uT[:, :, sub * 128:(sub + 1) * 128], pT[p][:], [("pT", p)], [("uT", ubuf, sub)])


def build_attn0(phases=(1, 2, 3), dbg=False, ng=NG, skip=(), nq=NG, npr=2, b=None):
    b = b or B()
    if dbg:
        b.dscr = b.dout
    b.no_act_copy = True
    S = b.S
    x = b.din("x", [S_LEN, D], F32)
    c_row = b.din("c_row", [D], F32)
    w_ada = b.din("w_ada", [D, 6 * D], F32)
    b_ada = b.din("b_ada", [6 * D], F32)
    w_in = b.din("w_in", [D, 2048], F32)
    gn_g = b.din("gn_g", [256], F32)
    ident_d = b.din("ident", [128, 128], F32)
    triI_d = b.din("triI", [128, 128], F32)
    maskd_d = b.din("maskd", [4, 128, 512], F32)
    dm_d = b.din("dm", [2, 128, 128], F32)
    pat_d = b.din("pat", [4, 128], F32)
    kdec_d = b.din("kdec", [128, 4], F32)
    g64_d = b.din("g64", [128, 2], F32)
    att = b.dout("att", [S_LEN, 512], BF16)
    mod_o = b.dout("mod", [6 * D], F32)
    QS = b.dscr("QS", [2, 128, S_LEN], BF16)
    KS = b.dscr("KS", [2, 128, S_LEN], BF16)
    VS = b.dscr("VS", [S_LEN, 256], BF16)
    RQ = b.dscr("RQ", [2, 3, 128, S_LEN], BF16)
    RK = b.dscr("RK", [2, 128, S_LEN], BF16)
    RKd = b.dscr("RKd", [S_LEN, 512], BF16)
    RV = b.dscr("RV", [S_LEN, 256], BF16)
    RG = b.dscr("RG", [S_LEN, 256], BF16)

    with ExitStack() as es0:
        ident = b.sb(es0, "ident", [128, 128], BF16)
        b.dma("pool", ident[:], ident_d, [], ["ident"])
        modS = b.dscr("modS", [6 * D], F32)
        b.shared["modS"] = modS
        b.shared["att"] = att
        emit_mod(b, es0, c_row, w_ada, b_ada, mod_o, modS)

        with ExitStack() as es:
            if 1 not in phases:
                return b.finish(["mod_d"])
            sc1p = b.sb(es, "sc1p", [128, D], F32)
            sh1 = b.sb(es, "sh1", [128, D], F32)
            wcs = [b.sb(es, f"wc{h}", [128, 8, 512], BF16) for h in range(4)]

            class _WC:
                def __getitem__(self, key):
                    p_, kc_, cs_ = key
                    h_ = cs_.start // 512
                    return wcs[h_][p_, kc_, cs_.start - h_ * 512:cs_.stop - h_ * 512]
            wc = _WC()
            pat = b.sb(es, "pat", [128, 4, 128], F32)
            kdec = b.sb(es, "kdec", [128, 4], F32)
            xt = [b.sb(es, f"xt{i}", [128, D], F32) for i in range(2)]
            uf = [b.sb(es, f"uf{i}", [128, D], F32) for i in range(2)]
            ub = [b.sb(es, f"ub{i}", [128, D], BF16) for i in range(2)]
            uTs = [b.sb(es, f"uT{i}", [128, 8, 512], BF16) for i in range(2)]
            fst = [b.sb(es, f"fst{i}", [128, 512], BF16) for i in range(4)]
            gtmp = b.sb(es, "gtmp", [128, 256], F32)
            tst = [b.sb(es, f"tst{i}", [128, 4, 1280], BF16) for i in range(2)]
            pT = [b.ps(es, f"pT{i}", [128, 8, 128], BF16) for i in range(2)]
            pf = [b.ps(es, f"pf{i}", [128, 512], F32) for i in range(2)]
            pt = [b.ps(es, f"pt{i}", [128, 512], F32) for i in range(3)]

            b.dma("sp", sh1[:], modS[0:D].partition_broadcast(128), ["mod_s"], ["sh1"])
            b.dma("sp", sc1p[:], modS[D:2 * D].partition_broadcast(128), ["mod_s"], ["sc1p"])
            b.ts("dve", sc1p[:], sc1p[:], 1.0, None, ALU.add, None, ["sc1p"], ["sc1p"])
            for h in range(4):
                b.dma("pool", wcs[h][:], w_in[:, h * 512:(h + 1) * 512].rearrange("(kc p) n -> p kc n", p=128), [], ["wc"] if h == 0 else [("wc", h)])
            S.op("sp", lambda e: None, reads=[("wc", 1), ("wc", 2), ("wc", 3)], writes=["wc"])
            for i in range(4):
                b.dma("sp", pat[:, i, :], pat_d[i, :].partition_broadcast(128), [], ["pat"] if i == 0 else [("pat", i)])
            S.op("sp", lambda e: None, reads=[("pat", 1), ("pat", 2), ("pat", 3)], writes=["pat"])
            b.dma("sp", kdec[:], kdec_d, [], ["kdec"])

            nf = 0
            emit_uT_group(b, 0, x, sc1p, sh1, ident, xt, uf, ub, pT, uTs[0], 0)
            for g in range(ng):
                cols = slice(g * 512, (g + 1) * 512)
                if g + 1 < ng:
                    emit_uT_group(b, g + 1, x, sc1p, sh1, ident, xt, uf, ub, pT, uTs[(g + 1) % 2], (g + 1) % 2)
                uT = uTs[g % 2]
                uTk = [("uT", g % 2, s_) for s_ in range(4)]
                for fm in range(8):
                    if "fm" in skip:
                        break
                    pi = fm % 2
                    for kc in range(8):
                        b.mm(pf[pi][:], wc[:, kc, fm * 128:(fm + 1) * 128], uT[:, kc, :], kc == 0, kc == 7, uTk + ["wc"], [("pf", pi)])
                    if "ev" in skip:
                        continue
                    if fm < 4 or fm >= 6:
                        si = nf % 4
                        nf += 1
                        b.copy("act", fst[si][:], pf[pi][:], [("pf", pi)], [("fst", si)])
                        dst = (QS[fm, :, cols] if fm < 2 else KS[fm - 2, :, cols]) if fm < 4 else RK[fm - 6, :, cols]
                        if "st" not in skip:
                            b.dma("sp", dst, fst[si][:], [("fst", si)], [("FM", fm, g)])
                    else:
                        hd = fm - 4
                        for ver in range(3):
                            si = nf % 4
                            nf += 1
                            if ver == 0:
                                b.copy("act", fst[si][:], pf[pi][:], [("pf", pi)], [("fst", si)])
                            else:
                                b.tt("dve", fst[si][:].rearrange("p (a t) -> p a t", a=4), pf[pi][:].rearrange("p (a t) -> p a t", a=4),
                                     pat[:, hd * 2 + ver - 1, :].unsqueeze(1).to_broadcast([128, 4, 128]), ALU.mult,
                                     [("pf", pi), "pat"], [("fst", si)])
                            if "st" not in skip:
                                b.dma("sp", RQ[hd, ver, :, cols], fst[si][:], [("fst", si)], [("RQ", hd, ver, g)])
                ti = g % 2
                if "tm" in skip:
                    continue
                for sub in range(4):
                    for half in range(2):
                        pi = (sub * 2 + half) % 3
                        for kc in range(8):
                            b.mm(pt[pi][:], uT[:, kc, sub * 128:(sub + 1) * 128], wc[:, kc, 1024 + half * 512:1024 + (half + 1) * 512],
                                 kc == 0, kc == 7, uTk + ["wc"], [("pt", pi)])
                        wk = ("tst", ti, sub, half)
                        if half == 0:
                            b.copy("act", tst[ti][:, sub, 0:256], pt[pi][:, 0:256], [("pt", pi)], [wk])
                            for hd in range(2):
                                for par in range(2):
                                    o0 = 256 + (hd * 2 + par) * 128
                                    b.ts("dve", tst[ti][:, sub, o0:o0 + 128], pt[pi][:, 256 + hd * 128:256 + (hd + 1) * 128],
                                         kdec[:, hd * 2 + par:hd * 2 + par + 1], None, ALU.mult, None, [("pt", pi), "kdec"], [wk + (hd, par)])
                        else:
                            b.copy("dve", tst[ti][:, sub, 768:1024], pt[pi][:, 0:256], [("pt", pi)], [wk])
                            b.copy("dve", gtmp[:], pt[pi][:, 256:512], [("pt", pi)], ["gtmp"])
                            b.act(tst[ti][:, sub, 1024:1280], gtmp[:], AF.Silu, ["gtmp"], [wk + ("g",)])
                rk_ = [("tst", ti, sub, half) for sub in range(4) for half in range(2)] + \
                      [("tst", ti, sub, 0, hd, par) for sub in range(4) for hd in range(2) for par in range(2)] + \
                      [("tst", ti, sub, 1, "g") for sub in range(4)]
                rows = slice(g * 512, (g + 1) * 512)
                for (dst, c0, c1, nm) in ((VS, 0, 256, "VS"), (RKd, 256, 768, "RKd"), (RV, 768, 1024, "RV"), (RG, 1024, 1280, "RG")):
                    b.dma("sp", dst[rows, :].rearrange("(s p) c -> p s c", p=128), tst[ti][:, :, c0:c1], rk_, [(nm, g)])
        S.barrier()

        with ExitStack() as es:
            if 2 not in phases:
                npr = 0
            QT = b.sb(es, "QT", [128, 2, S_LEN], BF16)
            KT = b.sb(es, "KT", [128, 2, S_LEN], BF16)
            Vt = b.sb(es, "Vt", [128, NB, 256], BF16)
            triI = b.sb(es, "triI", [128, 128], BF16)
            onesr = b.sb(es, "onesr", [1, 128], BF16)
            maskd = b.sb(es, "maskd", [128, 4, 512], BF16)
            NS = 4
            E_ = [b.sb(es, f"E{i}", [128, 512], F32) for i in range(6)]
            SP = [b.sb(es, f"SP{i}", [128, 512], BF16) for i in range(6)]
            X_ = [b.sb(es, f"X{i}", [128, 512], F32) for i in range(NS)]
            C_ = [b.sb(es, f"C{i}", [128, 512], F32) for i in range(NS)]
            Z_ = [b.sb(es, f"Z{i}", [128, 512], F32) for i in range(NS)]
            W_ = [b.sb(es, f"W{i}", [128, 512], BF16) for i in range(NS)]
            car = [b.sb(es, f"car{i}", [1, 512], BF16) for i in range(2)]
            ost = [b.sb(es, f"ost{i}", [128, 4, 64], BF16) for i in range(2)]
            pz = [b.ps(es, f"pz{i}", [128, 512], F32) for i in range(NS)]
            pc = [b.ps(es, f"pc{i}", [128, 512], F32) for i in range(2)]
            poT = [b.ps(es, f"poT{i}", [64, 512], F32) for i in range(2)]
            OTs = [b.sb(es, f"OTs{i}", [64, 512], F32) for i in range(2)]
            identf = b.sb(es, "identf", [128, 128], F32)
            b.dma("sp", identf[:], ident_d, [], ["identf"])

            nqc = nq * 512
            for pr in range(2):
                b.dma("sp", QT[:, pr, 0:nqc], QS[pr, :, 0:nqc], [], [("QT", pr)])
                b.dma("sp", KT[:, pr, 0:nqc], KS[pr, :, 0:nqc], [], [("KT", pr)])
            for i8 in range(8):
                if i8 * 1024 >= nqc:
                    b.memset("dve", Vt[0:1, i8 * 8, 0:1], 0.0, [("Vt", i8)] if i8 else ["Vt"])
                    continue
                b.dma("sp", Vt[:, i8 * 8:(i8 + 1) * 8, :], VS[i8 * 1024:(i8 + 1) * 1024, :].rearrange("(n p) c -> p n c", p=128), [], ["Vt"] if i8 == 0 else [("Vt", i8)])
            S.op("sp", lambda e: None, reads=[("Vt", i8) for i8 in range(1, 8)], writes=["Vt"])
            b.dma("pool", triI[:], triI_d, [], ["triI"])
            b.dma("pool", maskd[:], maskd_d.rearrange("j p t -> p j t"), [], ["maskd"])
            b.memset("dve", onesr[:], 1.0, ["onesr"])
            zer = b.sb(es, "zer", [128, 256], BF16)
            b.memset("dve", zer[:], 0.0, ["zer"])

            for pr in range(npr):
                for qi in range(nq):
                    t0 = qi * 512
                    nkb = 4 * qi + 4
                    def stage_a(step):
                        kb = nkb - 1 - step
                        j = kb - 4 * qi
                        par = step % 2
                        for hp in range(2):
                            s = hp * 2 + par
                            ps_ = slice(hp * 64, (hp + 1) * 64)
                            b.mm(pz[s][:], KT[ps_, pr, kb * 128:(kb + 1) * 128], QT[ps_, pr, t0:t0 + 512], True, True,
                                 [("KT", pr), ("QT", pr)], [("pz", s)])
                        for hp in range(2):
                            s = hp * 2 + par
                            b.copy("dve", Z_[s][:], pz[s][:], [("pz", s)], [("Z", s)])
                        for hp in range(2):
                            s, s3 = hp * 2 + par, hp * 3 + step % 3
                            b.act(E_[s3][:], Z_[s][:], AF.Exp, [("Z", s)], [("E", s3)], scale=0.125)
                        for hp in range(2):
                            s3 = hp * 3 + step % 3
                            b.act(SP[s3][:], E_[s3][:], AF.Ln, [("E", s3)], [("SP", s3)], bias=1.0)
                        if j >= 0:
                            for hp in range(2):
                                s3 = hp * 3 + step % 3
                                b.tt("pool", SP[s3][:], SP[s3][:], maskd[:, j, :], ALU.mult, [("SP", s3), "maskd"], [("SP", s3)])
                                b.tt("pool", E_[s3][:], E_[s3][:], maskd[:, j, :], ALU.mult, [("E", s3), "maskd"], [("E", s3)])

                    def stage_b1(step):
                        par = step % 2
                        for hp in range(2):
                            s3 = hp * 3 + step % 3
                            b.mm(pc[hp][:], triI[:], SP[s3][:], True, step == 0, ["triI", ("SP", s3)], [("pc", hp)])
                            if step > 0:
                                b.mm(pc[hp][:], onesr[:], car[hp][:], False, True, ["onesr", ("car", hp)], [("pc", hp)])
                        for hp in range(2):
                            s = hp * 2 + par
                            if step < nkb - 1:
                                b.copy("dve", car[hp][:], pc[hp][0:1, :], [("pc", hp)], [("car", hp)])
                            b.copy("dve", C_[s][:], pc[hp][:], [("pc", hp)], [("C", s)])
                            b.act(X_[s][:], C_[s][:], AF.Exp, [("C", s)], [("X", s)], scale=-1.0)

                    def stage_b2(step):
                        kb = nkb - 1 - step
                        par = step % 2
                        for hp in range(2):
                            s, s3 = hp * 2 + par, hp * 3 + step % 3
                            b.tt("pool", W_[s][:], E_[s3][:], X_[s][:], ALU.mult, [("E", s3), ("X", s)], [("W", s)])
                        for hp in range(2):
                            s = hp * 2 + par
                            h = pr * 2 + hp
                            b.mm(poT[hp][:], Vt[:, kb, h * 64:(h + 1) * 64], W_[s][:], step == 0, step == nkb - 1, [("W", s), "Vt"], [("poT", hp)])

                    stage_a(0)
                    stage_a(1)
                    stage_b1(0)
                    for step in range(nkb):
                        if step + 2 < nkb:
                            stage_a(step + 2)
                        if step + 1 < nkb:
                            stage_b1(step + 1)
                        stage_b2(step)
                    for hp in range(2):
                        s = hp
                        h = pr * 2 + hp
                        b.copy("dve", OTs[hp][:], poT[hp][:], [("poT", hp)], [("OTs", hp)])
                        for sub in range(4):
                            b.tr(pc[hp][:, sub * 64:(sub + 1) * 64], OTs[hp][:, sub * 128:(sub + 1) * 128], identf[0:64, 0:64],
                                 [("OTs", hp), "identf"], [("pc", hp)])
                        b.copy("dve", ost[s][:].rearrange("p a c -> p (a c)"), pc[hp][:, 0:256], [("pc", hp)], [("ost", s)])
                        b.dma("sp", att[t0:t0 + 512, h * 64:(h + 1) * 64].rearrange("(s p) c -> p s c", p=128), ost[s][:],
                              [("ost", s)], [("att_sb", h, qi)])
        S.barrier()

        with ExitStack() as es:
            if 3 not in phases:
                return b.finish(["mod_d"] + [("att_sb", h, qi) for h in range(2 * npr) for qi in range(nq)])
            dm = b.sb(es, "dm", [128, 2, 128], F32)
            g64 = b.sb(es, "g64", [128, 2], F32)
            gng = b.sb(es, "gng", [128, 256], F32)
            epsb = b.sb(es, "epsb", [128, 1], F32)
            qt = [b.sb(es, f"rqt{i}", [128, 3, 512], BF16) for i in range(4)]
            kt = [b.sb(es, f"rkt{i}", [128, 512], BF16) for i in range(4)]
            kd = [b.sb(es, f"rkd{i}", [128, 4, 256], BF16) for i in range(4)]
            vt = [b.sb(es, f"rvt{i}", [128, 4, 128], BF16) for i in range(4)]
            gt = [b.sb(es, f"rgt{i}", [128, 4, 128], BF16) for i in range(4)]
            stf = [b.sb(es, f"stf{i}", [128, 128], F32) for i in range(2)]
            stb = [[b.sb(es, f"stb{i}{k}", [128, 128], BF16) for k in range(2)] for i in range(2)]
            Pm = [b.sb(es, f"Pm{i}", [128, 128], BF16) for i in range(2)]
            of = [b.sb(es, f"of{i}", [128, 128], F32) for i in range(2)]
            st_ = [b.sb(es, f"rst{i}", [128, 6], F32) for i in range(2)]
            mv = [b.sb(es, f"rmv{i}", [128, 2], F32) for i in range(2)]
            rs = [b.sb(es, f"rrs{i}", [128, 1], F32) for i in range(2)]
            gg = [b.sb(es, f"gg{i}", [128, 128], F32) for i in range(2)]
            oo = [b.sb(es, f"oo{i}", [128, 4, 128], BF16) for i in range(4)]
            psc = [b.ps(es, f"psc{i}", [128, 128], F32) for i in range(2)]
            pso = [b.ps(es, f"pso{i}", [128, 128], F32) for i in range(2)]
            pkv = [b.ps(es, f"pkv{i}", [128, 128], F32) for i in range(2)]

            b.dma("sp", dm[:], dm_d.rearrange("h s c -> s h c"), [], ["dm"])
            b.dma("sp", g64[:], g64_d, [], ["g64"])
            b.dma("sp", gng[:], gn_g.partition_broadcast(128), [], ["gng"])
            b.memset("dve", epsb[:], EPS, ["epsb"])
            for hd in range(2):
                b.memset("dve", stf[hd][:], 0.0, [("stf", hd)])
                b.memset("dve", stb[hd][0][:], 0.0, [("stb", hd, 0)])
            nst = [0, 0]
            for g in range(ng):
                cols = slice(g * 512, (g + 1) * 512)
                rows = slice(g * 512, (g + 1) * 512)
                for hd in range(2):
                    bi = hd * 2 + g % 2
                    b.dma("sp", qt[bi][:], RQ[hd, :, :, cols].rearrange("v p t -> p v t"), [], [("rqt", bi)])
                    b.dma("sp", kt[bi][:], RK[hd, :, cols], [], [("rkt", bi)])
                    b.dma("sp", kd[bi][:], RKd[rows, hd * 256:(hd + 1) * 256].rearrange("(s p) c -> p s c", p=128), [], [("rkd", bi)])
                    b.dma("sp", vt[bi][:], RV[rows, hd * 128:(hd + 1) * 128].rearrange("(s p) c -> p s c", p=128), [], [("rvt", bi)])
                    b.dma("sp", gt[bi][:], RG[rows, hd * 128:(hd + 1) * 128].rearrange("(s p) c -> p s c", p=128), [], [("rgt", bi)])
                for sub in range(4):
                    tc_ = slice(sub * 128, (sub + 1) * 128)
                    for hd in range(2):
                        bi = hd * 2 + g % 2
                        s = hd
                        b.mm(psc[s][:], kt[bi][:, tc_], qt[bi][:, 0, tc_], True, True, [("rkt", bi), ("rqt", bi)], [("psc", s)])
                        b.tt("dve", Pm[s][:], psc[s][:], dm[:, hd, :], ALU.mult, [("psc", s), "dm"], [("Pm", s)])
                        k0 = nst[hd] % 2
                        b.mm(pso[s][:], Pm[s][:], vt[bi][:, sub, :], True, False, [("Pm", s), ("rvt", bi)], [("pso", s)])
                        b.mm(pso[s][:], qt[bi][:, 1, tc_], stb[hd][k0][:], False, False, [("rqt", bi), ("stb", hd, k0)], [("pso", s)])
                        b.mm(pkv[s][:], kd[bi][:, sub, 0:128], vt[bi][:, sub, :], True, True, [("rkd", bi), ("rvt", bi)], [("pkv", s)])
                        b.stt("dve", stf[hd][:], stf[hd][:], g64[:, hd:hd + 1], pkv[s][:], ALU.mult, ALU.add, [("stf", hd), "g64", ("pkv", s)], [("stf", hd)])
                        b.copy("act", stb[hd][1 - k0][:], stf[hd][:], [("stf", hd)], [("stb", hd, 1 - k0)])
                        b.mm(pso[s][:], qt[bi][:, 2, tc_], stb[hd][1 - k0][:], False, True, [("rqt", bi), ("stb", hd, 1 - k0)], [("pso", s)])
                        b.mm(pkv[s][:], kd[bi][:, sub, 128:256], vt[bi][:, sub, :], True, True, [("rkd", bi), ("rvt", bi)], [("pkv", s)])
                        b.stt("dve", stf[hd][:], stf[hd][:], g64[:, hd:hd + 1], pkv[s][:], ALU.mult, ALU.add, [("stf", hd), "g64", ("pkv", s)], [("stf", hd)])
                        b.copy("act", stb[hd][k0][:], stf[hd][:], [("stf", hd)], [("stb", hd, k0)])
                        b.copy("act", of[s][:], pso[s][:], [("pso", s)], [("of", s)])
                        S.op("dve", lambda e, s=s: e.bn_stats(out=st_[s][:], in_=of[s][:]), [("of", s)], [("rst", s)])
                        S.op("dve", lambda e, s=s: e.bn_aggr(out=mv[s][:], in_=st_[s][:]), [("rst", s)], [("rmv", s)])
                        b.act(rs[s][:], mv[s][:, 1:2], AF.Ln, [("rmv", s), "epsb"], [("rrs", s)], bias=epsb[:])
                        b.act(rs[s][:], rs[s][:], AF.Exp, [("rrs", s)], [("rrs", s)], scale=-0.5)
                        b.tt("pool", gg[s][:], gt[bi][:, sub, :], gng[:, hd * 128:(hd + 1) * 128], ALU.mult, [("rgt", bi), "gng"], [("gg", s)])
                        b.ts("dve", of[s][:], of[s][:], mv[s][:, 0:1], rs[s][:], ALU.subtract, ALU.mult, [("of", s), ("rmv", s), ("rrs", s)], [("of", s)])
                        b.tt("dve", oo[bi][:, sub, :], of[s][:], gg[s][:], ALU.mult, [("of", s), ("gg", s)], [("oo", bi, sub)])
                for hd in range(2):
                    bi = hd * 2 + g % 2
                    b.dma("sp", att[rows, 256 + hd * 128:256 + (hd + 1) * 128].rearrange("(s p) c -> p s c", p=128), oo[bi][:],
                          [("oo", bi, sub) for sub in range(4)], [("att_r", hd, g)])
        outs = ["mod_d"] + [("att_sb", h, qi) for h in range(2 * npr) for qi in range(nq)] + [("att_r", hd, g) for hd in range(2) for g in range(ng)]
        if b.fused:
            S.barrier()
            return outs
        return b.finish(outs)


def attn0_consts(hh):
    pos = np.arange(128)
    same = (pos[:, None] // 64) == (pos[None, :] // 64)
    dmm = np.zeros((2, 128, 128), np.float32)
    pat = np.zeros((4, 128), np.float32)
    kdec = np.zeros((128, 4), np.float32)
    g64 = np.zeros((128, 2), np.float32)
    for hd in range(2):
        hr = 2 * hh + hd
        lg = np.log1p(-np.exp2(-5.0 - hr))
        dmm[hd] = np.where(same, np.exp(lg * np.abs(pos[:, None] - pos[None, :])), 0.0) * 128 ** -0.5
        for par in range(2):
            inpar = (pos // 64) == par
            pat[hd * 2 + par] = np.where(inpar, np.exp(lg * (pos % 64 + 1.0)), 0.0)
            kdec[:, hd * 2 + par] = np.where(inpar, np.exp(lg * (63 - pos % 64)), 0.0) * 128 ** -0.5
        g64[:, hd] = np.exp(lg * 64)
    s_ = np.arange(128)[:, None]
    t_ = np.arange(512)[None, :]
    maskd = np.stack([(128 * j + s_ < t_) for j in range(4)]).astype(np.float32)
    return dict(ident=np.eye(128, dtype=np.float32), triI=np.tril(np.ones((128, 128), np.float32)),
                maskd=maskd, dm=dmm, pat=pat, kdec=kdec, g64=g64)


LAMBDA_INIT = 0.8 - 0.6 * float(np.exp(-0.3 * 1))
ACT_PSUM = True


def build_attn1(ng=NG, nq=NG, nhb=2, b=None):
    b = b or B()
    b.no_act_copy = True
    S = b.S
    x = b.din("x", [S_LEN, D], F32)
    c_row = b.din("c_row", [D], F32)
    w_ada = b.din("w_ada", [D, 6 * D], F32)
    b_ada = b.din("b_ada", [6 * D], F32)
    w_in = b.din("w_in", [D, 1536], F32)
    lamv = b.din("lamv", [4, 64], F32)
    subg = b.din("subg", [128], F32)
    ident_d = b.din("ident", [128, 128], F32)
    bdiag_d = b.din("bdiag", [4, 4, 128, 512], F32)
    posb_d = b.din("posb", [128, 4, 64], F32)
    gtab_d = b.din("gtab", [4, 128, 512], F32)
    att = b.dout("att", [S_LEN, 512], BF16)
    mod_o = b.dout("mod", [6 * D], F32)
    modS = b.dscr("modS1", [6 * D], F32)
    b.shared["modS"] = modS
    b.shared["att"] = att
    lamS = b.dscr("lamS", [1], F32)
    QD = b.dscr("QD", [4, 128, S_LEN], BF16)
    KD = b.dscr("KD", [4, 128, S_LEN], BF16)
    VD = b.dscr("VD", [S_LEN, 4 * 129], BF16)

    with ExitStack() as es0:
        ident = b.sb(es0, "ident", [128, 128], BF16)
        neglam = b.sb(es0, "neglam", [128, 1], F32)
        gsub = b.sb(es0, "gsub", [128, 128], F32)
        epsb = b.sb(es0, "epsb", [128, 1], F32)
        b.dma("pool", ident[:], ident_d, [], ["ident"])
        b.memset("dve", epsb[:], EPS, ["epsb"])
        emit_mod(b, es0, c_row, w_ada, b_ada, mod_o, modS)
        with ExitStack() as es:
            lv = b.sb(es, "lv", [1, 4, 64], F32)
            lp = b.sb(es, "lp", [1, 2, 64], F32)
            ls = b.sb(es, "ls", [1, 2], F32)
            ll = b.sb(es, "ll", [1, 1], F32)
            b.dma("sp", lv[:], lamv.rearrange("(o a) n -> o a n", o=1), [], ["lv"])
            b.tt("dve", lp[:, 0, :], lv[:, 0, :], lv[:, 1, :], ALU.mult, ["lv"], ["lp0"])
            b.tt("dve", lp[:, 1, :], lv[:, 2, :], lv[:, 3, :], ALU.mult, ["lv"], ["lp1"])
            b.red("dve", ls[:], lp[:], "sum", ["lp0", "lp1"], ["ls"])
            b.act(ls[:], ls[:], AF.Exp, ["ls"], ["ls"])
            b.tt("dve", ll[:], ls[:, 1:2], ls[:, 0:1], ALU.subtract, ["ls"], ["ll"])
            b.ts("dve", ll[:], ll[:], -LAMBDA_INIT, None, ALU.add, None, ["ll"], ["ll"])
            b.dma("sp", lamS.rearrange("(o n) -> o n", o=1), ll[:], ["ll"], ["lamS"])
            b.dma("sp", neglam[:], lamS.partition_broadcast(128), ["lamS"], ["neglam"])
            b.dma("sp", gsub[:], subg.partition_broadcast(128), [], ["gsub"])
            b.ts("dve", gsub[:], gsub[:], 1.0 - LAMBDA_INIT, None, ALU.mult, None, ["gsub"], ["gsub"])
        S.barrier()

        with ExitStack() as es:
            sc1p = b.sb(es, "sc1p", [128, D], F32)
            sh1 = b.sb(es, "sh1", [128, D], F32)
            wcs = [b.sb(es, f"wc{h}", [128, 8, 512], BF16) for h in range(3)]
            xt = [b.sb(es, f"xt{i}", [128, D], F32) for i in range(2)]
            uf = [b.sb(es, f"uf{i}", [128, D], F32) for i in range(2)]
            ub = [b.sb(es, f"ub{i}", [128, D], BF16) for i in range(2)]
            uTs = [b.sb(es, f"uT{i}", [128, 8, 512], BF16) for i in range(2)]
            fst = [b.sb(es, f"fst{i}", [128, 512], BF16) for i in range(4)]
            vst = [b.sb(es, f"vst{i}", [128, 4, 4, 129], BF16) for i in range(2)]
            pT = [b.ps(es, f"pT{i}", [128, 8, 128], BF16) for i in range(2)]
            pf = [b.ps(es, f"pf{i}", [128, 512], F32) for i in range(2)]
            pt = [b.ps(es, f"pt{i}", [128, 512], F32) for i in range(2)]
            b.dma("sp", sh1[:], modS[0:D].partition_broadcast(128), ["mod_s"], ["sh1"])
            b.dma("sp", sc1p[:], modS[D:2 * D].partition_broadcast(128), ["mod_s"], ["sc1p"])
            b.ts("dve", sc1p[:], sc1p[:], 1.0, None, ALU.add, None, ["sc1p"], ["sc1p"])
            for h in range(3):
                b.dma("pool", wcs[h][:], w_in[:, h * 512:(h + 1) * 512].rearrange("(kc p) n -> p kc n", p=128), [], ["wc"] if h == 0 else [("wc", h)])
            S.op("sp", lambda e: None, reads=[("wc", 1), ("wc", 2)], writes=["wc"])
            for i in range(2):
                b.memset("dve", vst[i][:], 1.0, [("vst", i)])
            nf = 0
            emit_uT_group(b, 0, x, sc1p, sh1, ident, xt, uf, ub, pT, uTs[0], 0)
            for g in range(ng):
                cols = slice(g * 512, (g + 1) * 512)
                if g + 1 < ng:
                    emit_uT_group(b, g + 1, x, sc1p, sh1, ident, xt, uf, ub, pT, uTs[(g + 1) % 2], (g + 1) % 2)
                uT = uTs[g % 2]
                uTk = [("uT", g % 2, s_) for s_ in range(4)]
                for fm in range(8):
                    pi = fm % 2
                    wt = wcs[fm // 4]
                    c0 = (fm % 4) * 128
                    for kc in range(8):
                        b.mm(pf[pi][:], wt[:, kc, c0:c0 + 128], uT[:, kc, :], kc == 0, kc == 7, uTk + ["wc"], [("pf", pi)])
                    si = nf % 4
                    nf += 1
                    b.copy("dve", fst[si][:], pf[pi][:], [("pf", pi)], [("fst", si)])
                    dst = QD[fm, :, cols] if fm < 4 else KD[fm - 4, :, cols]
                    b.dma("sp", dst, fst[si][:], [("fst", si)], [("FM", fm, g)])
                ti = g % 2
                for sub in range(4):
                    pi = sub % 2
                    for kc in range(8):
                        b.mm(pt[pi][:], uT[:, kc, sub * 128:(sub + 1) * 128], wcs[2][:, kc, :], kc == 0, kc == 7, uTk + ["wc"], [("pt", pi)])
                    b.copy("dve", vst[ti][:, sub, :, 0:128], pt[pi][:].rearrange("p (h d) -> p h d", h=4), [("pt", pi), ("vst", ti)], [("vst", ti, sub)])
                rows = slice(g * 512, (g + 1) * 512)
                b.dma("sp", VD[rows, :].rearrange("(s p) c -> p s c", p=128), vst[ti][:].rearrange("p s h d -> p s (h d)"),
                      [("vst", ti, sub) for sub in range(4)], [("vst", ti)])
        S.barrier()

        with ExitStack() as es:
            QA = b.sb(es, "QA", [128, 2, S_LEN], BF16)
            KA = b.sb(es, "KA", [128, 2, S_LEN], BF16)
            Vt = b.sb(es, "Vt", [128, NB, 128], BF16)
            bdg = b.sb(es, "bdg", [128, 4, 512], F32)
            posb = b.sb(es, "posb", [128, 4, 64], F32)
            onec = b.sb(es, "onec", [128, 1], BF16)
            identf = b.sb(es, "identf", [128, 128], F32)
            T_ = [b.sb(es, f"T{i}", [128, 512], F32) for i in range(4)]
            P_ = [b.sb(es, f"P{i}", [128, 512], BF16) for i in range(4)]
            Lacc = [b.sb(es, f"Lacc{i}", [128, 512], F32) for i in range(2)]
            Lacd = [b.sb(es, f"Lacd{i}", [128, 512], F32) for i in range(2)]
            Lhi = [b.sb(es, f"Lhi{i}", [128, 512], BF16) for i in range(2)]
            Llo = [b.sb(es, f"Llo{i}", [128, 512], BF16) for i in range(2)]
            OTs = [b.sb(es, f"OTs{i}", [128, 512], F32) for i in range(2)]
            rec = [b.sb(es, f"rec{i}", [128, 4], F32) for i in range(2)]
            On = [b.sb(es, f"On{i}", [128, 4, 128], F32) for i in range(2)]
            aa = b.sb(es, "aa", [128, 4, 128], F32)
            sq = b.sb(es, "sq", [128, 4, 128], F32)
            ssum = b.sb(es, "ssum", [128, 4], F32)
            ost = [b.sb(es, f"ost{i}", [128, 4, 128], BF16) for i in range(2)]
            pz = [b.ps(es, f"pz{i}", [128, 512], F32) for i in range(4)]
            pOT = [b.ps(es, f"pOT{i}", [128, 512], F32) for i in range(2)]
            ptr = b.ps(es, "ptr", [128, 4, 128], F32)
            pl = b.ps(es, "pl", [128, 8], F32)

            b.dma("sp", posb[:], posb_d, [], ["posb"])
            b.dma("sp", identf[:], ident_d, [], ["identf"])
            b.memset("dve", onec[:], 1.0, ["onec"])
            nqc = nq * 512
            LaccD = [b.sb(es, f"LaccD{i}", [128, 512], F32) for i in range(2)]
            OD = [b.sb(es, f"OD{i}", [128, 512], F32) for i in range(2)]
            gt = b.sb(es, "gt", [128, 512], F32)
            nout = 0
            for h in range(2 * nhb):
                b.dma("sp", QA[:, 0, 0:nqc], QD[h, :, 0:nqc], [], [("QA", 0)])
                b.dma("sp", KA[:, 0, 0:nqc], KD[h, :, 0:nqc], [], [("KA", 0)])
                b.dma("sp", gt[:], gtab_d[h], [], ["gt"])
                b.dma("sp", bdg[:], bdiag_d[h].rearrange("j p t -> p j t"), [], ["bdg"])
                for i8 in range(8):
                    if i8 * 1024 >= nqc:
                        continue
                    b.dma("sp", Vt[:, i8 * 8:(i8 + 1) * 8, :], VD[i8 * 1024:(i8 + 1) * 1024, h * 129:h * 129 + 128].rearrange("(n p) d -> p n d", p=128),
                          [], [("Vt", i8)])
                vtk = [("Vt", i8) for i8 in range(8)]
                for qi in range(nq):
                    t0 = qi * 512
                    nkb = 4 * qi + 4
                    def stage_a(step):
                        kb = nkb - 1 - step
                        j = kb - 4 * qi
                        par = step % 2
                        for m in range(2):
                            s_ = m * 2 + par
                            ps_ = slice(m * 64, (m + 1) * 64)
                            b.mm(pz[s_][:], KA[ps_, 0, kb * 128:(kb + 1) * 128], QA[ps_, 0, t0:t0 + 512], True, True,
                                 [("KA", 0), ("QA", 0)], [("pz", s_)])
                        for m in range(2):
                            s_ = m * 2 + par
                            if j >= 0:
                                b.stt("dve", T_[s_][:], pz[s_][:], 0.125, bdg[:, j, :], ALU.mult, ALU.add, [("pz", s_), "bdg"], [("T", s_)])
                            elif not ACT_PSUM:
                                b.copy("dve", T_[s_][:], pz[s_][:], [("pz", s_)], [("T", s_)])
                        for m in range(2):
                            s_ = m * 2 + par
                            if j >= 0:
                                b.act(P_[s_][:], T_[s_][:], AF.Exp, [("T", s_)], [("P", s_)])
                            elif ACT_PSUM:
                                off = 4 * qi - kb
                                b.act(P_[s_][:], pz[s_][:], AF.Exp, [("pz", s_), "posb"], [("P", s_)], bias=posb[:, h, off:off + 1], scale=0.125)
                            else:
                                off = 4 * qi - kb
                                b.act(P_[s_][:], T_[s_][:], AF.Exp, [("T", s_), "posb"], [("P", s_)], bias=posb[:, h, off:off + 1], scale=0.125)

                    def stage_b(step):
                        kb = nkb - 1 - step
                        par = step % 2
                        for m in range(2):
                            s_ = m * 2 + par
                            if step < 4:
                                b.mm(pOT[m][:], Vt[:, kb, :], P_[s_][:], step == 0, step == 3, [("P", s_)] + vtk, [("pOT", m)])
                                if step == 0:
                                    b.copy("pool", LaccD[m][:], P_[s_][:], [("P", s_)], [("LaccD", m)])
                                else:
                                    b.tt("pool", LaccD[m][:], LaccD[m][:], P_[s_][:], ALU.add, [("LaccD", m), ("P", s_)], [("LaccD", m)])
                                if step == 3:
                                    b.copy("dve", OD[m][:], pOT[m][:], [("pOT", m)], [("OD", m)])
                                continue
                            b.mm(pOT[m][:], Vt[:, kb, :], P_[s_][:], step == 4, step == nkb - 1, [("P", s_)] + vtk, [("pOT", m)])
                            if step == 4:
                                b.copy("pool", Lacc[m][:], P_[s_][:], [("P", s_)], [("Lacc", m)])
                            elif step == 5:
                                b.copy("dve", Lacd[m][:], P_[s_][:], [("P", s_)], [("Lacd", m)])
                            elif step % 3 != 0:
                                b.tt("dve", Lacd[m][:], Lacd[m][:], P_[s_][:], ALU.add, [("Lacd", m), ("P", s_)], [("Lacd", m)])
                            else:
                                b.tt("pool", Lacc[m][:], Lacc[m][:], P_[s_][:], ALU.add, [("Lacc", m), ("P", s_)], [("Lacc", m)])

                    stage_a(0)
                    for step in range(nkb):
                        if step + 1 < nkb:
                            stage_a(step + 1)
                        stage_b(step)
                    oi = nout % 2
                    nout += 1
                    for m in range(2):
                        if qi >= 1:
                            b.tt("pool", Lacc[m][:], Lacc[m][:], Lacd[m][:], ALU.add, [("Lacc", m), ("Lacd", m)], [("Lacc", m)])
                            b.tt("pool", Lacc[m][:], Lacc[m][:], gt[:], ALU.mult, [("Lacc", m), "gt"], [("Lacc", m)])
                            b.tt("pool", LaccD[m][:], LaccD[m][:], Lacc[m][:], ALU.add, [("LaccD", m), ("Lacc", m)], [("LaccD", m)])
                            b.tt("dve", OTs[m][:], pOT[m][:], gt[:], ALU.mult, [("pOT", m), "gt"], [("OTs", m)])
                            b.tt("dve", OD[m][:], OD[m][:], OTs[m][:], ALU.add, [("OD", m), ("OTs", m)], [("OD", m)])
                        b.copy("pool", Lhi[m][:], LaccD[m][:], [("LaccD", m)], [("Lhi", m)])
                        b.tt("pool", Llo[m][:], LaccD[m][:], Lhi[m][:], ALU.subtract, [("LaccD", m), ("Lhi", m)], [("Llo", m)])
                        for sub in range(4):
                            c_ = m * 4 + sub
                            b.mm(pl[:, c_:c_ + 1], Lhi[m][:, sub * 128:(sub + 1) * 128], onec[:], True, False, [("Lhi", m), "onec"], [("pl", c_), "plbank"])
                            b.mm(pl[:, c_:c_ + 1], Llo[m][:, sub * 128:(sub + 1) * 128], onec[:], False, True, [("Llo", m), "onec"], [("pl", c_), "plbank"])
                        S.op("dve", lambda e, m=m: e.reciprocal(out=rec[m][:], in_=pl[:, m * 4:(m + 1) * 4]),
                             [("pl", m * 4 + sub) for sub in range(4)] + ["plbank"], [("rec", m)])
                        for sub in range(4):
                            b.tr(ptr[:, sub, :], OD[m][:, sub * 128:(sub + 1) * 128], identf[:], [("OD", m), "identf"], ["ptr"])
                        b.tt("dve", On[m][:], ptr[:], rec[m][:].unsqueeze(2).to_broadcast([128, 4, 128]), ALU.mult, ["ptr", ("rec", m)], [("On", m)])
                    b.stt("dve", aa[:], On[1][:], neglam[:, 0:1], On[0][:], ALU.mult, ALU.add, [("On", 0), ("On", 1), "neglam"], ["aa"])
                    b.tt("pool", sq[:], aa[:], aa[:], ALU.mult, ["aa"], ["sq"])
                    b.red("dve", ssum[:], sq[:], "sum", ["sq"], ["ssum"])
                    b.act(ssum[:], ssum[:], AF.Ln, ["ssum", "epsb"], ["ssum"], bias=epsb[:], scale=1.0 / 128.0)
                    b.act(ssum[:], ssum[:], AF.Exp, ["ssum"], ["ssum"], scale=-0.5)
                    b.tt("dve", aa[:], aa[:], ssum[:].unsqueeze(2).to_broadcast([128, 4, 128]), ALU.mult, ["aa", "ssum"], ["aa"])
                    b.tt("pool", ost[oi][:], aa[:], gsub[:].unsqueeze(1).to_broadcast([128, 4, 128]), ALU.mult, ["aa", "gsub"], [("ost", oi)])
                    b.dma("sp", att[t0:t0 + 512, h * 128:(h + 1) * 128].rearrange("(s p) c -> p s c", p=128), ost[oi][:],
                          [("ost", oi)], [("att", h, qi)])
        outs = ["mod_d"] + [("att", h, qi) for h in range(2 * nhb) for qi in range(nq)]
        if b.fused:
            S.barrier()
            return outs
        return b.finish(outs)


def attn1_consts(hh):
    bd = np.zeros((4, 4, 128, 512), np.float32)
    posb = np.zeros((128, 4, 64), np.float32)
    gtab = np.zeros((4, 128, 512), np.float32)
    s_ = np.arange(128)[:, None].astype(np.float64)
    t_ = np.arange(512)[None, :].astype(np.float64)
    for h in range(4):
        gh = 4 * hh + h
        slope = 2.0 ** (-(gh + 1.0))
        gtab[h] = np.exp(-slope * np.arange(512))[None, :]
        for j in range(4):
            s_abs = 128 * j + s_
            allowed = (s_abs // 64) <= (t_ // 64)
            bd[h, j] = np.where(allowed, -slope * np.abs(t_ - s_abs), -30000.0)
        for off in range(64):
            posb[:, h, off] = slope * (np.arange(128) - 128.0 * off)
    return dict(ident=np.eye(128, dtype=np.float32), bdiag=bd, posb=posb, gtab=gtab)


def _run(nc, in_maps):
    res = run_bass_kernel_spmd(nc, in_maps, core_ids=list(range(NCORES)))
    return res.results


def _post_inputs(l, xfull, att_full, mods, w_out, inp):
    lnp = np.ascontiguousarray(np.stack([inp["ln1_g"][l], inp["ln1_b"][l], inp["ln2_g"][l], inp["ln2_b"][l]]).astype(np.float32))
    wr = np.ascontiguousarray(np.concatenate([inp["moe_w_group"][l], inp["moe_w_router"][l]], axis=1))
    br = np.ascontiguousarray(np.concatenate([inp["moe_b_group"][l], inp["moe_b_router"][l]]))
    cs = post_consts()
    maps = []
    for c in range(NCORES):
        b_, hh = c // 2, c % 2
        rows = slice(hh * TOK, (hh + 1) * TOK)
        d = dict(xs=np.ascontiguousarray(xfull[b_, rows]), att=np.ascontiguousarray(att_full[b_][rows]), mod=mods[b_],
                 w_out=w_out, lnp=lnp, wr=wr, br=br, w1=inp["moe_w1"][l], w3=inp["moe_w3"][l], w2=inp["moe_w2"][l])
        d.update(cs)
        maps.append(d)
    return maps


def kernel_unfused(**inp):
    inp = {k: np.asarray(v) for k, v in inp.items()}
    x = inp["x"]
    Bn = x.shape[0]
    w = inp["even_w_in"][0]
    maps = []
    for c in range(NCORES):
        b_, hh = c // 2, c % 2
        a = slice(hh * 256, (hh + 1) * 256)
        sq, sk, sv = w[:, 0:512], w[:, 512:1024], w[:, 1024:1536]
        rq, rk, rv, rg = w[:, 1536:2048], w[:, 2048:2560], w[:, 2560:3072], w[:, 3072:3584]
        w_in = np.ascontiguousarray(np.concatenate([sq[:, a], sk[:, a], rq[:, a], rk[:, a], sv[:, a], rk[:, a], rv[:, a], rg[:, a]], axis=1))
        d = dict(x=np.ascontiguousarray(x[b_]), c_row=np.ascontiguousarray(inp["c"][b_]), w_ada=inp["w_ada"][0], b_ada=inp["b_ada"][0],
                 w_in=w_in, gn_g=np.ascontiguousarray(inp["ret_gn_g"][0][a]))
        d.update(attn0_consts(hh))
        maps.append(d)
    r = _run(build_attn0(), maps)
    att_full, mods = [], []
    for b_ in range(Bn):
        a0, a1 = np.asarray(r[2 * b_]["att"]), np.asarray(r[2 * b_ + 1]["att"])
        att_full.append(np.concatenate([a0[:, :256], a1[:, :256], a0[:, 256:], a1[:, 256:]], axis=1))
        mods.append(np.asarray(r[2 * b_]["mod"]))
    post_nc = build_post()
    r = _run(post_nc, _post_inputs(0, x, att_full, mods, inp["even_w_out"][0], inp))
    x1 = np.stack([np.concatenate([np.asarray(r[2 * b_]["xo"]), np.asarray(r[2 * b_ + 1]["xo"])], axis=0) for b_ in range(Bn)])
    w = inp["odd_w_in"][0]
    lamv = np.ascontiguousarray(np.stack([inp["lambda_q1"][0], inp["lambda_k1"][0], inp["lambda_q2"][0], inp["lambda_k2"][0]]))
    maps = []
    for c in range(NCORES):
        b_, hh = c // 2, c % 2
        a = slice(hh * 512, (hh + 1) * 512)
        w_in = np.ascontiguousarray(np.concatenate([w[:, 0:1024][:, a], w[:, 1024:2048][:, a], w[:, 2048:3072][:, a]], axis=1))
        d = dict(x=np.ascontiguousarray(x1[b_]), c_row=np.ascontiguousarray(inp["c"][b_]), w_ada=inp["w_ada"][1], b_ada=inp["b_ada"][1],
                 w_in=w_in, lamv=lamv, subg=np.ascontiguousarray(inp["diff_subln_g"][0]))
        d.update(attn1_consts(hh))
        maps.append(d)
    r = _run(build_attn1(), maps)
    att_full, mods = [], []
    for b_ in range(Bn):
        att_full.append(np.concatenate([np.asarray(r[2 * b_]["att"]), np.asarray(r[2 * b_ + 1]["att"])], axis=1))
        mods.append(np.asarray(r[2 * b_]["mod"]))
    r = _run(build_post(), _post_inputs(1, x1, att_full, mods, inp["odd_w_out"][0], inp))
    out = np.stack([np.concatenate([np.asarray(r[2 * b_]["xo"]), np.asarray(r[2 * b_ + 1]["xo"])], axis=0) for b_ in range(Bn)])
    return out.astype(np.float32)


PAIRS = [[0, 1], [2, 3], [4, 5], [6, 7]]


def build_fused():
    b = B()
    b.fused = True
    b.no_act_copy = True
    S = b.S
    G0 = b.dscr("G0", [2 * S_LEN, 512], BF16)
    G1 = b.dscr("G1", [2 * S_LEN, 512], BF16)
    X1h = b.dscr("X1h", [TOK, D], F32)
    X1f = b.dscr("X1f", [S_LEN, D], F32)
    gidx = b.nc.dram_tensor("gidx", [128, 2, NT], I32, kind="ExternalInput").ap()
    out = b.nc.dram_tensor("out", [TOK, D], F32, kind="ExternalOutput").ap()

    agn = [0]

    def allgather(src, dst, R, C, dt, esz):
        rows = (2 * 1024 * 1024) // (C * esz)
        nch = R // rows
        agn[0] += 1
        ss = [b.nc.dram_tensor(f"ag{agn[0]}_s{i}", [rows, C], dt, kind="Internal").ap() for i in range(nch)]
        gg = [b.nc.dram_tensor(f"ag{agn[0]}_g{i}", [2 * rows, C], dt, kind="Internal").ap() for i in range(nch)]
        for i in range(nch):
            b.dma("sp", ss[i][:, :], src[i * rows:(i + 1) * rows, :], [], [])
        S.barrier()
        for i in range(nch):
            S.cc(lambda e, i=i: e.collective_compute("AllGather", ALU.bypass, replica_groups=PAIRS, ins=[ss[i][:, :]], outs=[gg[i][:, :]]), [], [])
        S.barrier()
        for i in range(nch):
            for r in range(2):
                b.dma("sp", dst[r * R + i * rows:r * R + (i + 1) * rows, :], gg[i][r * rows:(r + 1) * rows, :], [], [])
        S.barrier()

    b.pfx, b.ovr = "a0_", {}
    build_attn0(b=b)
    att0, mod0 = b.shared["att"], b.shared["modS"]
    allgather(att0, G0, S_LEN, 512, BF16, 2)
    b.pfx, b.ovr = "p0_", {"mod": mod0, "xo": X1h}
    build_post(b=b, att_gather=(G0, gidx))
    allgather(X1h, X1f, TOK, D, F32, 4)
    b.pfx, b.ovr = "a1_", {"x": X1f}
    build_attn1(b=b)
    att1, mod1 = b.shared["att"], b.shared["modS"]
    allgather(att1, G1, S_LEN, 512, BF16, 2)
    b.pfx, b.ovr = "p1_", {"mod": mod1, "xs": X1h, "xo": out}
    build_post(b=b, att_gather=(G1, gidx))
    return b.finish([])


def kernel(**inp):
    inp = {k: np.asarray(v) for k, v in inp.items()}
    x = inp["x"]
    Bn = x.shape[0]
    w0 = inp["even_w_in"][0]
    w1_ = inp["odd_w_in"][0]
    lamv = np.ascontiguousarray(np.stack([inp["lambda_q1"][0], inp["lambda_k1"][0], inp["lambda_q2"][0], inp["lambda_k2"][0]]))
    wo0 = inp["even_w_out"][0]
    wo0p = np.ascontiguousarray(np.concatenate([wo0[0:256], wo0[512:768], wo0[256:512], wo0[768:1024]], axis=0))
    pcs = post_consts()

    def post_in(l, w_out):
        return dict(w_out=w_out,
                    lnp=np.ascontiguousarray(np.stack([inp["ln1_g"][l], inp["ln1_b"][l], inp["ln2_g"][l], inp["ln2_b"][l]]).astype(np.float32)),
                    wr=np.ascontiguousarray(np.concatenate([inp["moe_w_group"][l], inp["moe_w_router"][l]], axis=1)),
                    br=np.ascontiguousarray(np.concatenate([inp["moe_b_group"][l], inp["moe_b_router"][l]])),
                    w1=inp["moe_w1"][l], w3=inp["moe_w3"][l], w2=inp["moe_w2"][l], tri=pcs["tri"], eoff=pcs["eoff"])
    p0, p1 = post_in(0, wo0p), post_in(1, inp["odd_w_out"][0])
    maps = []
    for c in range(NCORES):
        b_, hh = c // 2, c % 2
        a = slice(hh * 256, (hh + 1) * 256)
        sq, sk, sv = w0[:, 0:512], w0[:, 512:1024], w0[:, 1024:1536]
        rq, rk, rv, rg = w0[:, 1536:2048], w0[:, 2048:2560], w0[:, 2560:3072], w0[:, 3072:3584]
        w_in0 = np.ascontiguousarray(np.concatenate([sq[:, a], sk[:, a], rq[:, a], rk[:, a], sv[:, a], rk[:, a], rv[:, a], rg[:, a]], axis=1))
        a2 = slice(hh * 512, (hh + 1) * 512)
        w_in1 = np.ascontiguousarray(np.concatenate([w1_[:, 0:1024][:, a2], w1_[:, 1024:2048][:, a2], w1_[:, 2048:3072][:, a2]], axis=1))
        c0 = attn0_consts(hh)
        c1 = attn1_consts(hh)
        p_ = np.arange(128)[:, None, None]
        r_ = np.arange(2)[None, :, None]
        t_ = np.arange(NT)[None, None, :]
        gidx = (r_ * S_LEN + hh * TOK + t_ * 128 + p_).astype(np.int32)
        d = {"ident": c0["ident"], "gidx": np.ascontiguousarray(gidx)}
        d.update({"a0_x": np.ascontiguousarray(x[b_]), "a0_c_row": np.ascontiguousarray(inp["c"][b_]), "a0_w_ada": inp["w_ada"][0],
                  "a0_b_ada": inp["b_ada"][0], "a0_w_in": w_in0, "a0_gn_g": np.ascontiguousarray(inp["ret_gn_g"][0][a])})
        d.update({"a0_" + k: v for k, v in c0.items() if k != "ident"})
        d.update({"p0_xs": np.ascontiguousarray(x[b_, hh * TOK:(hh + 1) * TOK])})
        d.update({"p0_" + k: v for k, v in p0.items()})
        d.update({"a1_c_row": np.ascontiguousarray(inp["c"][b_]), "a1_w_ada": inp["w_ada"][1], "a1_b_ada": inp["b_ada"][1],
                  "a1_w_in": w_in1, "a1_lamv": lamv, "a1_subg": np.ascontiguousarray(inp["diff_subln_g"][0])})
        d.update({"a1_" + k: v for k, v in c1.items() if k != "ident"})
        d.update({"p1_" + k: v for k, v in p1.items()})
        maps.append(d)
    r = _run(build_fused(), maps)
    out = np.stack([np.concatenate([np.asarray(r[2 * b_]["out"]), np.asarray(r[2 * b_ + 1]["out"])], axis=0) for b_ in range(Bn)])
    return out.astype(np.float32)
```

```python
from contextlib import ExitStack
import numpy as np
import ml_dtypes
import concourse.bass as bass
import concourse.mybir as mybir
from concourse.bass_utils import run_bass_kernel_spmd

F32 = mybir.dt.float32
BF16 = mybir.dt.bfloat16
I32 = mybir.dt.int32
AF = mybir.ActivationFunctionType
ALU = mybir.AluOpType
AX = mybir.AxisListType

D = 1024
S_LEN = 8192
NCORES = 8
ALPHA = (2.0 * 2) ** 0.25
EPS = 1e-5
NE = 32
CAP = 512
TOK = 4096
NT = TOK // 128
BIG = 1.0e30


class Sched:
    NDMA = 8
    ENGS = ("pe", "act", "dve", "pool", "sp")

    def __init__(self, nc, same_engine_sync=True):
        self.nc = nc
        self.ops = []
        self.state = {}
        self.same = same_engine_sync
        self.last = {}
        self.dma_since = set()

    def op(self, eng, fn, reads=(), writes=(), dma=False, nosame=False, extra=()):
        oid = len(self.ops)
        deps = set(extra)
        for k in reads:
            st = self.state.get(k)
            if st is not None and st[0] is not None:
                deps.add(st[0])
        for k in writes:
            st = self.state.get(k)
            if st is not None:
                if st[0] is not None:
                    deps.add(st[0])
                deps.update(st[1].values())
                deps.update(st[2])
        deps.discard(oid)
        self.ops.append(dict(eng=eng, fn=fn, deps=deps, dma=dma, nosame=nosame))
        for k in reads:
            st = self.state.setdefault(k, [None, {}, set()])
            if dma:
                st[2].add(oid)
            else:
                st[1][eng] = oid
        for k in writes:
            self.state[k] = [oid, {}, set()]
        if dma:
            self.dma_since.add(oid)
        else:
            self.last[eng] = oid
        return oid

    def dma(self, eng, fn, reads=(), writes=()):
        return self.op(eng, fn, reads, writes, dma=True)

    def cc(self, fn, reads=(), writes=()):
        oid = self.op("pool", fn, reads, writes, dma=True)
        self.ops[oid]["cc"] = True
        return oid

    def barrier(self):
        deps = set(self.last.values()) | self.dma_since
        self.dma_since = set()
        for e in self.ENGS:
            self.op(e, lambda eng: None, extra=deps, nosame=False)
        self.state = {}

    def finalize(self, sems):
        ops = self.ops

        def skip_same(o, po):
            if po["dma"] or o["dma"] or po["eng"] != o["eng"]:
                return False
            return o["eng"] in ("pe", "sp") or not self.same or o["nosame"]

        signal = [False] * len(ops)
        for o in ops:
            for p in o["deps"]:
                po = ops[p]
                if po["dma"] or skip_same(o, po):
                    continue
                signal[p] = True
        cnt = {e: 0 for e in self.ENGS}
        dcnt = {}
        event = [None] * len(ops)
        for i, o in enumerate(ops):
            if o.get("cc"):
                ncc = dcnt.get("cc", 0) + 1
                dcnt["cc"] = ncc
                event[i] = ("cc", ncc)
                o["prev"] = ("cc", ncc - 1) if ncc > 1 else None
            elif o["dma"]:
                e = o["eng"]
                n = dcnt.get(e, 0)
                dcnt[e] = n + 1
                j, m = n % self.NDMA, n // self.NDMA
                event[i] = (("dma", e, j), 16 * (m + 1))
                o["prev"] = (("dma", e, j), 16 * m) if m > 0 else None
            elif signal[i]:
                cnt[o["eng"]] += 1
                event[i] = (o["eng"], cnt[o["eng"]])
        seen = {e: {} for e in self.ENGS}
        per_eng = {e: [] for e in self.ENGS}
        nwaits = 0
        for i, o in enumerate(ops):
            e = o["eng"]
            need = {}
            for p in o["deps"]:
                po = ops[p]
                if skip_same(o, po):
                    continue
                ev = event[p]
                if ev is None:
                    continue
                if need.get(ev[0], 0) < ev[1]:
                    need[ev[0]] = ev[1]
            if o["dma"] and o["prev"] is not None:
                k, v = o["prev"]
                if need.get(k, 0) < v:
                    need[k] = v
            waits = []
            for k, v in need.items():
                if seen[e].get(k, 0) >= v:
                    continue
                seen[e][k] = v
                waits.append((k, v))
            nwaits += len(waits)
            per_eng[e].append((waits, o["fn"], event[i]))
        self.per_eng = per_eng
        self.sems = sems
        self.stats = dict(n_ops=len(ops), n_waits=nwaits, per_eng={e: len(v) for e, v in per_eng.items()})
        return per_eng

    def replay(self, ename, eng):
        sems = self.sems
        for waits, fn, ev in self.per_eng[ename]:
            for k, v in waits:
                eng.wait_ge(sems[k], v)
            ins = fn(eng)
            if ev is not None:
                if ins is None:
                    ins = eng.nop()
                ins.then_inc(sems[ev[0]], 16 if isinstance(ev[0], tuple) else 1)


class B:
    def __init__(self):
        self.nc = bass.Bass("TRN2", target_bir_lowering=False)
        self.S = Sched(self.nc)
        self.n = 0
        self.bregs = {}
        self.pfx = ""
        self.ovr = {}
        self.fused = False
        self.shared = {}
        self.no_act_copy = False

    def din(self, name, shape, dt):
        if name in self.ovr:
            return self.ovr[name]
        key = ("in", name, tuple(shape))
        if self.fused and name in ("ident",):
            if key not in self.shared:
                self.shared[key] = self.nc.dram_tensor(name, list(shape), dt, kind="ExternalInput").ap()
            return self.shared[key]
        return self.nc.dram_tensor(self.pfx + name, list(shape), dt, kind="ExternalInput").ap()

    def dout(self, name, shape, dt):
        if name in self.ovr:
            return self.ovr[name]
        if self.fused:
            return self.nc.dram_tensor(self.pfx + name, list(shape), dt, kind="Internal").ap()
        return self.nc.dram_tensor(name, list(shape), dt, kind="ExternalOutput").ap()

    def dscr(self, name, shape, dt):
        return self.nc.dram_tensor(self.pfx + name, list(shape), dt, kind="Internal").ap()

    def sb(self, es, name, shape, dt):
        return es.enter_context(self.nc.sbuf_tensor("sb_" + self.pfx + name, list(shape), dt))

    def ps(self, es, name, shape, dt):
        return es.enter_context(self.nc.psum_tensor("ps_" + self.pfx + name, list(shape), dt))

    def mm(self, out, lhsT, rhs, start, stop, r, w):
        self.S.op("pe", lambda e: e.matmul(out, lhsT=lhsT, rhs=rhs, start=start, stop=stop), r, w)

    def tr(self, out, in_, ident, r, w):
        self.S.op("pe", lambda e: e.transpose(out=out, in_=in_, identity=ident), r, w)

    def act(self, out, in_, func, r, w, bias=None, scale=1.0, accum=None):
        def f(e):
            kw = {}
            if bias is not None:
                kw["bias"] = bias
            if accum is not None:
                kw["accum_out"] = accum
            return e.activation(out=out, in_=in_, func=func, scale=scale, **kw)
        self.S.op("act", f, r, w)

    def tt(self, eng, out, in0, in1, op, r, w):
        self.S.op(eng, lambda e: e.tensor_tensor(out=out, in0=in0, in1=in1, op=op), r, w)

    def ts(self, eng, out, in0, s1, s2, op0, op1, r, w):
        if s2 is None:
            self.S.op(eng, lambda e: e.tensor_scalar(out=out, in0=in0, scalar1=s1, scalar2=None, op0=op0), r, w)
        else:
            self.S.op(eng, lambda e: e.tensor_scalar(out=out, in0=in0, scalar1=s1, scalar2=s2, op0=op0, op1=op1), r, w)

    def stt(self, eng, out, in0, scalar, in1, op0, op1, r, w):
        self.S.op(eng, lambda e: e.scalar_tensor_tensor(out=out, in0=in0, scalar=scalar, in1=in1, op0=op0, op1=op1), r, w)

    def copy(self, eng, out, in_, r, w):
        if eng == "act" and getattr(self, "no_act_copy", False):
            eng = "dve"
        if eng == "act":
            self.S.op("act", lambda e: e.activation(out=out, in_=in_, func=AF.Identity), r, w)
        else:
            self.S.op(eng, lambda e: e.tensor_copy(out=out, in_=in_), r, w)

    def memset(self, eng, out, val, w):
        self.S.op(eng, lambda e: e.memset(out, val), (), w)

    def red(self, eng, out, in_, op, r, w):
        if op == "max":
            self.S.op(eng, lambda e: e.reduce_max(out=out, in_=in_, axis=AX.X), r, w)
        else:
            self.S.op(eng, lambda e: e.reduce_sum(out=out, in_=in_, axis=AX.X), r, w)

    def dma(self, q, out, in_, r, w):
        self.S.dma(q, lambda e: e.dma_start(out=out, in_=in_), r, w)

    def _breg(self, e, bound):
        if bound not in self.bregs:
            self.bregs[bound] = e.to_reg(bound)
        return self.bregs[bound]

    def scatter(self, out_dram, idx, in_sb, bound, r, w):
        self.S.dma("pool", lambda e: e.indirect_dma_start(
            out=out_dram, out_offset=bass.IndirectOffsetOnAxis(ap=idx, axis=0), in_=in_sb, in_offset=None,
            bounds_check=self._breg(e, bound), oob_is_err=False), r, w)

    def gather(self, out_sb, in_dram, idx, bound, r, w):
        self.S.dma("pool", lambda e: e.indirect_dma_start(
            out=out_sb, out_offset=None, in_=in_dram, in_offset=bass.IndirectOffsetOnAxis(ap=idx, axis=0),
            bounds_check=self._breg(e, bound), oob_is_err=False), r, w)

    def layernorm(self, es_bufs, y, out, g_t, b_t, key_in, key_out, tag):
        st, mv, rstd, epsb = es_bufs["st"], es_bufs["mv"], es_bufs["rstd"], es_bufs["epsb"]
        S = self.S
        for c in range(2):
            S.op("dve", lambda e, c=c: e.bn_stats(out=st[:, c, :], in_=y[:, c * 512:(c + 1) * 512]),
                 [key_in], [("st", tag, c)])
        S.op("dve", lambda e: e.bn_aggr(out=mv[:], in_=st[:]), [("st", tag, 0), ("st", tag, 1)], [("mv", tag)])
        self.act(rstd[:], mv[:, 1:2], AF.Ln, [("mv", tag), "epsb"], [("rstd", tag)], bias=epsb[:])
        self.act(rstd[:], rstd[:], AF.Exp, [("rstd", tag)], [("rstd", tag)], scale=-0.5)
        self.ts("dve", out, y, mv[:, 0:1], rstd[:], ALU.subtract, ALU.mult, [key_in, ("mv", tag), ("rstd", tag)], [key_out])
        self.tt("pool", out, out, g_t, ALU.mult, [key_out, "lnp"], [key_out])
        self.tt("pool", out, out, b_t, ALU.add, [key_out, "lnp"], [key_out])

    def finish(self, out_keys):
        S = self.S
        nc = self.nc
        S.op("sp", lambda e: None, reads=list(out_keys))
        with ExitStack() as es:
            names = ["pe", "act", "dve", "pool", "sp", "cc"] + [("dma", q, j) for q in ("sp", "act", "pool") for j in range(Sched.NDMA)]
            sems = {n: es.enter_context(nc.semaphore("s_" + (n if isinstance(n, str) else "_".join(map(str, n))))) for n in names}
            S.finalize(sems)
            with nc.Block() as block:
                @block.tensor
                def _(e):
                    S.replay("pe", e)

                @block.scalar
                def _(e):
                    S.replay("act", e)

                @block.vector
                def _(e):
                    S.replay("dve", e)

                @block.gpsimd
                def _(e):
                    S.replay("pool", e)

                @block.sync
                def _(e):
                    S.replay("sp", e)
        return nc


def build_post(b=None, att_gather=None):
    b = b or B()
    S = b.S
    xs = b.din("xs", [TOK, D], F32)
    att = b.din("att", [TOK, D], BF16) if att_gather is None else None
    mod = b.din("mod", [6 * D], F32)
    w_out = b.din("w_out", [D, D], F32)
    lnp = b.din("lnp", [4, D], F32)
    wr = b.din("wr", [D, 36], F32)
    br = b.din("br", [36], F32)
    w1 = b.din("w1", [NE, D, 512], F32)
    w3 = b.din("w3", [NE, D, 512], F32)
    w2 = b.din("w2", [NE, 512, D], F32)
    ident_d = b.din("ident", [128, 128], F32)
    tri_d = b.din("tri", [128, 128], F32)
    eoff_d = b.din("eoff", [NE], F32)
    xo = b.dout("xo", [TOK, D], F32)
    X1 = b.dscr("X1", [TOK, D], F32)
    U = b.dscr("U", [TOK, D], BF16)
    US = b.dscr("US", [NE * CAP, D], BF16)
    YS = b.dscr("YS", [NE * CAP, D], F32)

    with ExitStack() as es0:
        ident = b.sb(es0, "ident", [128, 128], BF16)
        tri = b.sb(es0, "tri", [128, 128], BF16)
        ones = b.sb(es0, "ones", [128, 128], BF16)
        epsb = b.sb(es0, "epsb", [128, 1], F32)
        g1p = b.sb(es0, "g1p", [128, D], F32)
        sh2 = b.sb(es0, "sh2", [128, D], F32)
        sc2p = b.sb(es0, "sc2p", [128, D], F32)
        g2p = b.sb(es0, "g2p", [128, D], F32)
        lnt = b.sb(es0, "lnt", [128, 4, D], F32)
        st = b.sb(es0, "st", [128, 2, 6], F32)
        mv = b.sb(es0, "mv", [128, 2], F32)
        rstd = b.sb(es0, "rstd", [128, 1], F32)
        lnb = dict(st=st, mv=mv, rstd=rstd, epsb=epsb)
        gate1 = b.sb(es0, "gate1", [128, NT], F32)
        gate2 = b.sb(es0, "gate2", [128, NT], F32)
        dest1 = b.sb(es0, "dest1", [128, NT], I32)
        dest2 = b.sb(es0, "dest2", [128, NT], I32)

        b.dma("pool", ident[:], ident_d, [], ["ident"])
        b.dma("pool", tri[:], tri_d, [], ["tri"])
        if att_gather is not None:
            gidx_sb = b.sb(es0, "gidx", [128, 2, NT], I32)
            b.dma("sp", gidx_sb[:], att_gather[1], [], ["gidx"])
            att_gather = (att_gather[0], gidx_sb)
        b.memset("dve", ones[:], 1.0, ["ones"])
        b.memset("dve", epsb[:], EPS, ["epsb"])
        b.dma("sp", g1p[:], mod[2 * D:3 * D].partition_broadcast(128), [], ["g1p"])
        b.dma("sp", sh2[:], mod[3 * D:4 * D].partition_broadcast(128), [], ["sh2"])
        b.dma("sp", sc2p[:], mod[4 * D:5 * D].partition_broadcast(128), [], ["sc2p"])
        b.dma("sp", g2p[:], mod[5 * D:6 * D].partition_broadcast(128), [], ["g2p"])
        for i in range(4):
            b.dma("sp", lnt[:, i, :], lnp[i, :].partition_broadcast(128), [], ["lnp"] if i == 0 else [("lnp", i)])
        S.op("sp", lambda e: None, reads=[("lnp", 1), ("lnp", 2), ("lnp", 3)], writes=["lnp"])
        for t_, k_ in ((g1p, "g1p"), (sc2p, "sc2p"), (g2p, "g2p")):
            b.ts("dve", t_[:], t_[:], 1.0, None, ALU.add, None, [k_], [k_])

        esL = ExitStack()
        L_all = b.sb(esL, "L_all", [128, NT, 36], F32)
        with ExitStack() as es:
            wo = b.sb(es, "wo", [128, 8, D], BF16)
            wrt = b.sb(es, "wrt", [128, 8, 36], BF16)
            brt = b.sb(es, "brt", [128, 36], F32)
            xt = [b.sb(es, f"xt{i}", [128, D], F32) for i in range(2)]
            at = [b.sb(es, f"at{i}", [128, D], BF16) for i in range(2)]
            attT = [b.sb(es, f"attT{i}", [128, D], BF16) for i in range(2)]
            tmpA = b.sb(es, "tmpA", [128, D], F32)
            yA = b.sb(es, "yA", [128, D], F32)
            x1 = [b.sb(es, f"x1{i}", [128, D], F32) for i in range(2)]
            u2f = b.sb(es, "u2f", [128, D], F32)
            u2b = [b.sb(es, f"u2b{i}", [128, D], BF16) for i in range(2)]
            u2T = [b.sb(es, f"u2T{i}", [128, D], BF16) for i in range(2)]
            pT = [b.ps(es, f"pT{i}", [128, D], BF16) for i in range(2)]
            pmix = [[b.ps(es, f"pmix{i}{c}", [128, 512], F32) for c in range(2)] for i in range(2)]
            plog = b.ps(es, "plog", [128, 36], F32)

            b.dma("pool", wo[:], w_out.rearrange("(kc p) n -> p kc n", p=128), [], ["wo"])
            b.dma("pool", wrt[:], wr.rearrange("(kc p) n -> p kc n", p=128), [], ["wrt"])
            b.dma("sp", brt[:], br.partition_broadcast(128), [], ["brt"])

            def stage1(t):
                p = t % 2
                rows = slice(t * 128, (t + 1) * 128)
                b.dma("sp", xt[p][:], xs[rows, :], [], [("xt", p)])
                if att_gather is None:
                    b.dma("sp", at[p][:], att[rows, :], [], [("at", p)])
                else:
                    G_, gidx_ = att_gather
                    for r_ in range(2):
                        b.gather(at[p][:, r_ * 512:(r_ + 1) * 512], G_[:, :], gidx_[:, r_, t:t + 1], 2 * S_LEN - 1,
                                 ["gidx"] + ([("at", p)] if r_ == 0 else [("at", p, 0)]), [("at", p, 0)] if r_ == 0 else [("at", p)])
                for kc in range(8):
                    b.tr(pT[0][:, kc * 128:(kc + 1) * 128], at[p][:, kc * 128:(kc + 1) * 128], ident[:], [("at", p), "ident"], [("pT", 0)])
                b.copy("act", attT[p][:], pT[0][:], [("pT", 0)], [("attT", p)])
                for c in range(2):
                    for kc in range(8):
                        b.mm(pmix[p][c][:], attT[p][:, kc * 128:(kc + 1) * 128], wo[:, kc, c * 512:(c + 1) * 512],
                             kc == 0, kc == 7, [("attT", p), "wo"], [("pmix", p, c)])
                for c in range(2):
                    b.tt("dve", tmpA[:, c * 512:(c + 1) * 512], pmix[p][c][:], g1p[:, c * 512:(c + 1) * 512], ALU.mult,
                         [("pmix", p, c), "g1p"], [("tmpA", c)])
                b.stt("dve", yA[:], xt[p][:], ALPHA, tmpA[:], ALU.mult, ALU.add, [("xt", p), ("tmpA", 0), ("tmpA", 1)], ["yA"])
                b.layernorm(lnb, yA[:], x1[p][:], lnt[:, 0, :], lnt[:, 1, :], "yA", ("x1", p), "A")
                b.dma("sp", X1[rows, :], x1[p][:], [("x1", p)], [("X1", t)])
                b.tt("pool", u2f[:], x1[p][:], sc2p[:], ALU.mult, [("x1", p), "sc2p"], ["u2f"])
                b.tt("pool", u2b[p][:], u2f[:], sh2[:], ALU.add, ["u2f", "sh2"], [("u2b", p)])
                b.dma("sp", U[rows, :], u2b[p][:], [("u2b", p)], [("U", t)])

            def stage2(t):
                p = t % 2
                for kc in range(8):
                    b.tr(pT[1][:, kc * 128:(kc + 1) * 128], u2b[p][:, kc * 128:(kc + 1) * 128], ident[:], [("u2b", p), "ident"], [("pT", 1)])
                b.copy("act", u2T[p][:], pT[1][:], [("pT", 1)], [("u2T", p)])
                for kc in range(8):
                    b.mm(plog[:], u2T[p][:, kc * 128:(kc + 1) * 128], wrt[:, kc, :], kc == 0, kc == 7, [("u2T", p), "wrt"], ["plog"])
                b.tt("dve", L_all[:, t, :], plog[:], brt[:], ALU.add, ["plog", "brt"], ["L_all"])


            stage1(0)
            for t in range(NT):
                if t + 1 < NT:
                    stage1(t + 1)
                stage2(t)
        S.barrier()
        if True:
            with ExitStack() as esb:
                def t3(name, last, dt=F32):
                    return b.sb(esb, name, [128, NT, last] if last else [128, NT], dt)
                gmax = t3("gmax", 0)
                ohg = t3("ohg", 4)
                eg = t3("eg", 4)
                sume = t3("sume", 0)
                pgrp = t3("pgrp", 0)
                pen = t3("pen", 4)
                Lm = t3("Lm", 32)
                m1 = t3("m1", 0)
                mask1 = t3("mask1", 32)
                Lm2 = t3("Lm2", 32)
                m2 = t3("m2", 0)
                mask2 = t3("mask2", 32)
                dd = t3("dd", 0)
                s1 = t3("s1", 0)
                s2 = t3("s2", 0)
                A_bf = t3("A_bf", 32, BF16)
                rank = t3("rank", 32)
                tmp3 = t3("tmp3", 32)
                eoff = b.sb(esb, "eoff", [128, NE], F32)
                r1 = t3("r1", 0)
                e1 = t3("e1", 0)
                ov = t3("ov", 0)
                df = t3("df", 0)
                pR = [b.ps(esb, f"pR{i}", [128, 16, 32], F32) for i in range(2)]

                b.dma("sp", eoff[:], eoff_d.partition_broadcast(128), [], ["eoff"])
                Lg = L_all[:, :, 0:4]
                Le = L_all[:, :, 4:36]
                bc4 = lambda a: a.unsqueeze(2).to_broadcast([128, NT, 4])
                bc32 = lambda a: a.unsqueeze(2).to_broadcast([128, NT, 32])
                b.red("dve", gmax[:], Lg, "max", ["L_all"], ["gmax"])
                b.tt("dve", ohg[:], Lg, bc4(gmax[:]), ALU.is_equal, ["L_all", "gmax"], ["ohg"])
                b.tt("dve", eg[:], Lg, bc4(gmax[:]), ALU.subtract, ["L_all", "gmax"], ["eg"])
                b.act(eg[:], eg[:], AF.Exp, ["eg"], ["eg"])
                b.red("dve", sume[:], eg[:], "sum", ["eg"], ["sume"])
                S.op("dve", lambda e: e.reciprocal(out=pgrp[:], in_=sume[:]), ["sume"], ["pgrp"])
                b.ts("dve", pen[:], ohg[:], BIG, -BIG, ALU.mult, ALU.add, ["ohg"], ["pen"])
                b.tt("dve", Lm[:].rearrange("p t (g e) -> p t g e", g=4), Le.rearrange("p t (g e) -> p t g e", g=4),
                     pen[:].unsqueeze(3).to_broadcast([128, NT, 4, 8]), ALU.add, ["L_all", "pen"], ["Lm"])
                b.red("dve", m1[:], Lm[:], "max", ["Lm"], ["m1"])
                b.tt("dve", mask1[:], Lm[:], bc32(m1[:]), ALU.is_equal, ["Lm", "m1"], ["mask1"])
                b.stt("dve", Lm2[:], mask1[:], -BIG, Lm[:], ALU.mult, ALU.add, ["mask1", "Lm"], ["Lm2"])
                b.red("dve", m2[:], Lm2[:], "max", ["Lm2"], ["m2"])
                b.tt("dve", mask2[:], Lm2[:], bc32(m2[:]), ALU.is_equal, ["Lm2", "m2"], ["mask2"])
                b.tt("dve", dd[:], m2[:], m1[:], ALU.subtract, ["m1", "m2"], ["dd"])
                b.act(dd[:], dd[:], AF.Exp, ["dd"], ["dd"])
                b.ts("dve", dd[:], dd[:], 1.0, None, ALU.add, None, ["dd"], ["dd"])
                S.op("dve", lambda e: e.reciprocal(out=s1[:], in_=dd[:]), ["dd"], ["s1"])
                b.ts("dve", s2[:], s1[:], -1.0, 1.0, ALU.mult, ALU.add, ["s1"], ["s2"])
                b.tt("dve", gate1[:], s1[:], pgrp[:], ALU.mult, ["s1", "pgrp"], ["gate1"])
                b.tt("dve", gate2[:], s2[:], pgrp[:], ALU.mult, ["s2", "pgrp"], ["gate2"])
                b.tt("dve", A_bf[:], mask1[:], mask2[:], ALU.add, ["mask1", "mask2"], ["A_bf"])
                for t in range(NT):
                    reg = pR[t // 16][:, t % 16, :]
                    for tp in range(t):
                        b.mm(reg, ones[:], A_bf[:, tp, :], tp == 0, False, ["ones", "A_bf"], [("pR", t)])
                    b.mm(reg, tri[:], A_bf[:, t, :], t == 0, True, ["tri", "A_bf"], [("pR", t)])
                for h in range(2):
                    b.copy("dve", rank[:, h * 16:(h + 1) * 16, :], pR[h][:], [("pR", t) for t in range(h * 16, (h + 1) * 16)], [("rank", h)])
                rk = [("rank", 0), ("rank", 1)]
                for (mk, mkk, dst, gate, gk) in ((mask1, "mask1", dest1, gate1, "gate1"), (mask2, "mask2", dest2, gate2, "gate2")):
                    b.tt("dve", tmp3[:], mk[:], rank[:], ALU.mult, [mkk] + rk, ["tmp3"])
                    b.red("dve", r1[:], tmp3[:], "sum", ["tmp3"], ["r1"])
                    b.tt("dve", tmp3[:], mk[:], eoff[:].unsqueeze(1).to_broadcast([128, NT, 32]), ALU.mult, [mkk, "eoff"], ["tmp3"])
                    b.red("dve", e1[:], tmp3[:], "sum", ["tmp3"], ["e1"])
                    b.ts("dve", ov[:], r1[:], float(CAP), None, ALU.is_ge, None, ["r1"], ["ov"])
                    b.tt("dve", df[:], r1[:], e1[:], ALU.add, ["r1", "e1"], ["df"])
                    b.stt("dve", df[:], ov[:], 1.0e6, df[:], ALU.mult, ALU.add, ["ov", "df"], ["df"])
                    b.copy("dve", dst[:], df[:], ["df"], [gk + "d"])
                    b.ts("dve", ov[:], ov[:], -1.0, 1.0, ALU.mult, ALU.add, ["ov"], ["ov"])
                    b.tt("dve", gate[:], gate[:], ov[:], ALU.mult, [gk, "ov"], [gk])
        S.barrier()
        esL.close()

        with ExitStack() as es:
            ut = [b.sb(es, f"ut{i}", [128, D], BF16) for i in range(4)]
            for t in range(NT):
                p = t % 4
                b.dma("sp", ut[p][:], U[t * 128:(t + 1) * 128, :], [("U", t)], [("ut", p)])
                b.scatter(US[:, :], dest1[:, t:t + 1], ut[p][:, :], NE * CAP - 1, [("ut", p), "gate1d"], ["US"])
                b.scatter(US[:, :], dest2[:, t:t + 1], ut[p][:, :], NE * CAP - 1, [("ut", p), "gate2d"], ["US"])
        S.barrier()

        NJ = CAP // 128
        with ExitStack() as es:
            w1e = [b.sb(es, f"w1e{i}", [128, 8, 512], BF16) for i in range(2)]
            w3e = [b.sb(es, f"w3e{i}", [128, 8, 512], BF16) for i in range(2)]
            w2e = [b.sb(es, f"w2e{i}", [128, 4, D], BF16) for i in range(2)]
            us = [b.sb(es, f"us{i}", [128, D], BF16) for i in range(4)]
            uT = [b.sb(es, f"uT{i}", [128, 8, CAP], BF16) for i in range(2)]
            sil = [b.sb(es, f"sil{i}", [128, CAP], F32) for i in range(2)]
            hT = [b.sb(es, f"hT{i}", [128, 4, CAP], BF16) for i in range(2)]
            ysb = [b.sb(es, f"ysb{i}", [128, D], F32) for i in range(2)]
            pT = [b.ps(es, f"pTe{i}", [128, 8, 128], BF16) for i in range(2)]
            pa = [b.ps(es, f"pa{i}", [128, CAP], F32) for i in range(2)]
            pb = [b.ps(es, f"pb{i}", [128, CAP], F32) for i in range(2)]
            py = [b.ps(es, f"py{i}", [128, 512], F32) for i in range(2)]

            def load_w(e):
                q = e % 2
                b.dma("pool", w1e[q][:], w1[e].rearrange("(kc p) f -> p kc f", p=128), [], [("w1e", q)])
                b.dma("pool", w3e[q][:], w3[e].rearrange("(kc p) f -> p kc f", p=128), [], [("w3e", q)])
                b.dma("pool", w2e[q][:], w2[e].rearrange("(fc p) d -> p fc d", p=128), [], [("w2e", q)])

            load_w(0)
            nus = 0
            npy = 0
            for e in range(NE):
                q = e % 2
                if e + 1 < NE:
                    load_w(e + 1)
                for j in range(NJ):
                    ui = nus % 4
                    pi = nus % 2
                    nus += 1
                    r0 = e * CAP + j * 128
                    b.dma("sp", us[ui][:], US[r0:r0 + 128, :], ["US"], [("us", ui)])
                    for kc in range(8):
                        b.tr(pT[pi][:, kc, :], us[ui][:, kc * 128:(kc + 1) * 128], ident[:], [("us", ui), "ident"], [("pTe", pi)])
                    b.copy("act" if j % 2 == 0 else "dve", uT[q][:, :, j * 128:(j + 1) * 128], pT[pi][:], [("pTe", pi)], [("uT", q, j)])
                uTk = [("uT", q, j) for j in range(NJ)]
                for f in range(4):
                    fi = f % 2
                    for kc in range(8):
                        b.mm(pa[fi][:], w1e[q][:, kc, f * 128:(f + 1) * 128], uT[q][:, kc, :], kc == 0, kc == 7, uTk + [("w1e", q)], [("pa", fi)])
                    for kc in range(8):
                        b.mm(pb[fi][:], w3e[q][:, kc, f * 128:(f + 1) * 128], uT[q][:, kc, :], kc == 0, kc == 7, uTk + [("w3e", q)], [("pb", fi)])
                    if b.no_act_copy:
                        b.copy("dve", sil[fi][:], pa[fi][:], [("pa", fi)], [("sil", fi)])
                        b.act(sil[fi][:], sil[fi][:], AF.Silu, [("sil", fi)], [("sil", fi)])
                    else:
                        b.act(sil[fi][:], pa[fi][:], AF.Silu, [("pa", fi)], [("sil", fi)])
                    b.tt("dve", hT[q][:, f, :], sil[fi][:], pb[fi][:], ALU.mult, [("sil", fi), ("pb", fi)], [("hT", q, f)])
                hk = [("hT", q, f) for f in range(4)]
                for j in range(NJ):
                    yi = j % 2
                    for c in range(2):
                        pi = npy % 2
                        npy += 1
                        for f in range(4):
                            b.mm(py[pi][:], hT[q][:, f, j * 128:(j + 1) * 128], w2e[q][:, f, c * 512:(c + 1) * 512], f == 0, f == 3,
                                 hk + [("w2e", q)], [("py", pi)])
                        b.copy("act" if c == 0 else "dve", ysb[yi][:, c * 512:(c + 1) * 512], py[pi][:], [("py", pi)], [("ysb", yi, c)])
                    r0 = e * CAP + j * 128
                    b.dma("sp", YS[r0:r0 + 128, :], ysb[yi][:], [("ysb", yi, 0), ("ysb", yi, 1)], ["YS"])
        S.barrier()

        with ExitStack() as es:
            y1 = [b.sb(es, f"y1{i}", [128, D], F32) for i in range(2)]
            y2 = [b.sb(es, f"y2{i}", [128, D], F32) for i in range(2)]
            x1t = [b.sb(es, f"x1t{i}", [128, D], F32) for i in range(2)]
            fF = b.sb(es, "fF", [128, D], F32)
            zF = b.sb(es, "zF", [128, D], F32)
            oF = [b.sb(es, f"oF{i}", [128, D], F32) for i in range(2)]
            for i in range(2):
                b.memset("dve", y1[i][:], 0.0, [("y1", i)])
                b.memset("dve", y2[i][:], 0.0, [("y2", i)])
            for t in range(NT):
                p = t % 2
                rows = slice(t * 128, (t + 1) * 128)
                b.gather(y1[p][:, :], YS[:, :], dest1[:, t:t + 1], NE * CAP - 1, ["YS", "gate1d", ("y1", p)], [("y1", p)])
                b.gather(y2[p][:, :], YS[:, :], dest2[:, t:t + 1], NE * CAP - 1, ["YS", "gate2d", ("y2", p)], [("y2", p)])
                b.dma("sp", x1t[p][:], X1[rows, :], [("X1", t)], [("x1t", p)])
                b.ts("dve", fF[:], y1[p][:], gate1[:, t:t + 1], None, ALU.mult, None, [("y1", p), "gate1"], ["fF"])
                b.stt("dve", fF[:], y2[p][:], gate2[:, t:t + 1], fF[:], ALU.mult, ALU.add, [("y2", p), "gate2", "fF"], ["fF"])
                b.tt("pool", fF[:], fF[:], g2p[:], ALU.mult, ["fF", "g2p"], ["fF"])
                b.stt("dve", zF[:], x1t[p][:], ALPHA, fF[:], ALU.mult, ALU.add, [("x1t", p), "fF"], ["zF"])
                b.layernorm(lnb, zF[:], oF[p][:], lnt[:, 2, :], lnt[:, 3, :], "zF", ("oF", p), "F")
                b.dma("sp", xo[rows, :], oF[p][:], [("oF", p)], [("xo", t)])
        outs = [("xo", t) for t in range(NT)]
        if b.fused:
            S.op("sp", lambda e: None, reads=outs)
            S.barrier()
            return outs
        return b.finish(outs)


def post_consts():
    tri = np.triu(np.ones((128, 128), np.float32), 1)
    return dict(ident=np.eye(128, dtype=np.float32), tri=tri,
                eoff=(np.arange(NE) * CAP).astype(np.float32))


def emit_mod(b, es, c_row, w_ada, b_ada, mod_out, mod_scr=None, ps_name="pmod"):
    S = b.S
    with ExitStack() as esm:
        cf = b.sb(esm, "cf", [128, 8], F32)
        sg = b.sb(esm, "sg", [128, 8], F32)
        cb = b.sb(esm, "cb", [128, 8], BF16)
        wa = [b.sb(esm, f"wa{i}", [128, 8, 512], BF16) for i in range(2)]
        bad = b.sb(esm, "bad", [1, 6 * D], F32)
        modr = b.sb(esm, "modr", [1, 6 * D], F32)
        pm = [b.ps(esm, f"{ps_name}{i}", [1, 512], F32) for i in range(2)]
        b.dma("sp", cf[:], c_row.rearrange("(p kc) -> p kc", kc=8), [], ["cf"])
        b.dma("sp", bad[:], b_ada.rearrange("(o n) -> o n", o=1), [], ["bad"])
        b.act(sg[:], cf[:], AF.Exp, ["cf"], ["sg"], scale=-1.0)
        b.ts("dve", sg[:], sg[:], 1.0, None, ALU.add, None, ["sg"], ["sg"])
        S.op("dve", lambda e: e.reciprocal(out=sg[:], in_=sg[:]), ["sg"], ["sg"])
        b.tt("dve", cb[:], cf[:], sg[:], ALU.mult, ["cf", "sg"], ["cb"])
        for g in range(12):
            q = g % 2
            b.dma("pool", wa[q][:], w_ada[:, g * 512:(g + 1) * 512].rearrange("(p kc) n -> p kc n", kc=8), [], [("wa", q)])
            for kc in range(8):
                b.mm(pm[q][:], cb[:, kc:kc + 1], wa[q][:, kc, :], kc == 0, kc == 7, ["cb", ("wa", q)], [("pm", q)])
            b.tt("dve", modr[:, g * 512:(g + 1) * 512], pm[q][:], bad[:, g * 512:(g + 1) * 512], ALU.add, [("pm", q), "bad"], ["modr"])
        b.dma("sp", mod_out.rearrange("(o n) -> o n", o=1), modr[:], ["modr"], ["mod_d"])
        if mod_scr is not None:
            b.dma("sp", mod_scr.rearrange("(o n) -> o n", o=1), modr[:], ["modr"], ["mod_s"])
    S.barrier()


NG = S_LEN // 512
NB = S_LEN // 128


def emit_uT_group(b, g, x, sc1p, sh1, ident, xt, uf, ub, pT, uT, ubuf=0):
    for sub in range(4):
        r0 = g * 512 + sub * 128
        p = sub % 2
        b.dma("sp", xt[p][:], x[r0:r0 + 128, :], [], [("xt", p)])
        b.tt("dve", uf[p][:], xt[p][:], sc1p[:], ALU.mult, [("xt", p), "sc1p"], [("uf", p)])
        b.tt("pool", ub[p][:], uf[p][:], sh1[:], ALU.add, [("uf", p), "sh1"], [("ub", p)])
        for kc in range(8):
            b.tr(pT[p][:, kc, :], ub[p][:, kc * 128:(kc + 1) * 128], ident[:], [("ub", p), "ident"], [("pT", p)])
        b.copy("act" if sub % 2 == 0 else "dve", uT[:, :, sub * 128:(sub + 1) * 128], pT[p][:], [("pT", p)], [("uT", ubuf, sub)])


def build_attn0(phases=(1, 2, 3), dbg=False, ng=NG, skip=(), nq=NG, npr=2, b=None):
    b = b or B()
    if dbg:
        b.dscr = b.dout
    b.no_act_copy = True
    S = b.S
    x = b.din("x", [S_LEN, D], F32)
    c_row = b.din("c_row", [D], F32)
    w_ada = b.din("w_ada", [D, 6 * D], F32)
    b_ada = b.din("b_ada", [6 * D], F32)
    w_in = b.din("w_in", [D, 2048], F32)
    gn_g = b.din("gn_g", [256], F32)
    ident_d = b.din("ident", [128, 128], F32)
    triI_d = b.din("triI", [128, 128], F32)
    maskd_d = b.din("maskd", [4, 128, 512], F32)
    dm_d = b.din("dm", [2, 128, 128], F32)
    pat_d = b.din("pat", [4, 128], F32)
    kdec_d = b.din("kdec", [128, 4], F32)
    g64_d = b.din("g64", [128, 2], F32)
    att = b.dout("att", [S_LEN, 512], BF16)
    mod_o = b.dout("mod", [6 * D], F32)
    QS = b.dscr("QS", [2, 128, S_LEN], BF16)
    KS = b.dscr("KS", [2, 128, S_LEN], BF16)
    VS = b.dscr("VS", [S_LEN, 256], BF16)
    RQ = b.dscr("RQ", [2, 3, 128, S_LEN], BF16)
    RK = b.dscr("RK", [2, 128, S_LEN], BF16)
    RKd = b.dscr("RKd", [S_LEN, 512], BF16)
    RV = b.dscr("RV", [S_LEN, 256], BF16)
    RG = b.dscr("RG", [S_LEN, 256], BF16)

    with ExitStack() as es0:
        ident = b.sb(es0, "ident", [128, 128], BF16)
        b.dma("pool", ident[:], ident_d, [], ["ident"])
        modS = b.dscr("modS", [6 * D], F32)
        b.shared["modS"] = modS
        b.shared["att"] = att
        emit_mod(b, es0, c_row, w_ada, b_ada, mod_o, modS)

        with ExitStack() as es:
            if 1 not in phases:
                return b.finish(["mod_d"])
            sc1p = b.sb(es, "sc1p", [128, D], F32)
            sh1 = b.sb(es, "sh1", [128, D], F32)
            wcs = [b.sb(es, f"wc{h}", [128, 8, 512], BF16) for h in range(4)]

            class _WC:
                def __getitem__(self, key):
                    p_, kc_, cs_ = key
                    h_ = cs_.start // 512
                    return wcs[h_][p_, kc_, cs_.start - h_ * 512:cs_.stop - h_ * 512]
            wc = _WC()
            pat = b.sb(es, "pat", [128, 4, 128], F32)
            kdec = b.sb(es, "kdec", [128, 4], F32)
            xt = [b.sb(es, f"xt{i}", [128, D], F32) for i in range(2)]
            uf = [b.sb(es, f"uf{i}", [128, D], F32) for i in range(2)]
            ub = [b.sb(es, f"ub{i}", [128, D], BF16) for i in range(2)]
            uTs = [b.sb(es, f"uT{i}", [128, 8, 512], BF16) for i in range(2)]
            fst = [b.sb(es, f"fst{i}", [128, 512], BF16) for i in range(4)]
            gtmp = b.sb(es, "gtmp", [128, 256], F32)
            tst = [b.sb(es, f"tst{i}", [128, 4, 1280], BF16) for i in range(2)]
            pT = [b.ps(es, f"pT{i}", [128, 8, 128], BF16) for i in range(2)]
            pf = [b.ps(es, f"pf{i}", [128, 512], F32) for i in range(2)]
            pt = [b.ps(es, f"pt{i}", [128, 512], F32) for i in range(3)]

            b.dma("sp", sh1[:], modS[0:D].partition_broadcast(128), ["mod_s"], ["sh1"])
            b.dma("sp", sc1p[:], modS[D:2 * D].partition_broadcast(128), ["mod_s"], ["sc1p"])
            b.ts("dve", sc1p[:], sc1p[:], 1.0, None, ALU.add, None, ["sc1p"], ["sc1p"])
            for h in range(4):
                b.dma("pool", wcs[h][:], w_in[:, h * 512:(h + 1) * 512].rearrange("(kc p) n -> p kc n", p=128), [], ["wc"] if h == 0 else [("wc", h)])
            S.op("sp", lambda e: None, reads=[("wc", 1), ("wc", 2), ("wc", 3)], writes=["wc"])
            for i in range(4):
                b.dma("sp", pat[:, i, :], pat_d[i, :].partition_broadcast(128), [], ["pat"] if i == 0 else [("pat", i)])
            S.op("sp", lambda e: None, reads=[("pat", 1), ("pat", 2), ("pat", 3)], writes=["pat"])
            b.dma("sp", kdec[:], kdec_d, [], ["kdec"])

            nf = 0
            emit_uT_group(b, 0, x, sc1p, sh1, ident, xt, uf, ub, pT, uTs[0], 0)
            for g in range(ng):
                cols = slice(g * 512, (g + 1) * 512)
                if g + 1 < ng:
                    emit_uT_group(b, g + 1, x, sc1p, sh1, ident, xt, uf, ub, pT, uTs[(g + 1) % 2], (g + 1) % 2)
                uT = uTs[g % 2]
                uTk = [("uT", g % 2, s_) for s_ in range(4)]
                for fm in range(8):
                    if "fm" in skip:
                        break
                    pi = fm % 2
                    for kc in range(8):
                        b.mm(pf[pi][:], wc[:, kc, fm * 128:(fm + 1) * 128], uT[:, kc, :], kc == 0, kc == 7, uTk + ["wc"], [("pf", pi)])
                    if "ev" in skip:
                        continue
                    if fm < 4 or fm >= 6:
                        si = nf % 4
                        nf += 1
                        b.copy("act", fst[si][:], pf[pi][:], [("pf", pi)], [("fst", si)])
                        dst = (QS[fm, :, cols] if fm < 2 else KS[fm - 2, :, cols]) if fm < 4 else RK[fm - 6, :, cols]
                        if "st" not in skip:
                            b.dma("sp", dst, fst[si][:], [("fst", si)], [("FM", fm, g)])
                    else:
                        hd = fm - 4
                        for ver in range(3):
                            si = nf % 4
                            nf += 1
                            if ver == 0:
                                b.copy("act", fst[si][:], pf[pi][:], [("pf", pi)], [("fst", si)])
                            else:
                                b.tt("dve", fst[si][:].rearrange("p (a t) -> p a t", a=4), pf[pi][:].rearrange("p (a t) -> p a t", a=4),
                                     pat[:, hd * 2 + ver - 1, :].unsqueeze(1).to_broadcast([128, 4, 128]), ALU.mult,
                                     [("pf", pi), "pat"], [("fst", si)])
                            if "st" not in skip:
                                b.dma("sp", RQ[hd, ver, :, cols], fst[si][:], [("fst", si)], [("RQ", hd, ver, g)])
                ti = g % 2
                if "tm" in skip:
                    continue
                for sub in range(4):
                    for half in range(2):
                        pi = (sub * 2 + half) % 3
                        for kc in range(8):
                            b.mm(pt[pi][:], uT[:, kc, sub * 128:(sub + 1) * 128], wc[:, kc, 1024 + half * 512:1024 + (half + 1) * 512],
                                 kc == 0, kc == 7, uTk + ["wc"], [("pt", pi)])
                        wk = ("tst", ti, sub, half)
                        if half == 0:
                            b.copy("act", tst[ti][:, sub, 0:256], pt[pi][:, 0:256], [("pt", pi)], [wk])
                            for hd in range(2):
                                for par in range(2):
                                    o0 = 256 + (hd * 2 + par) * 128
                                    b.ts("dve", tst[ti][:, sub, o0:o0 + 128], pt[pi][:, 256 + hd * 128:256 + (hd + 1) * 128],
                                         kdec[:, hd * 2 + par:hd * 2 + par + 1], None, ALU.mult, None, [("pt", pi), "kdec"], [wk + (hd, par)])
                        else:
                            b.copy("dve", tst[ti][:, sub, 768:1024], pt[pi][:, 0:256], [("pt", pi)], [wk])
                            b.copy("dve", gtmp[:], pt[pi][:, 256:512], [("pt", pi)], ["gtmp"])
                            b.act(tst[ti][:, sub, 1024:1280], gtmp[:], AF.Silu, ["gtmp"], [wk + ("g",)])
                rk_ = [("tst", ti, sub, half) for sub in range(4) for half in range(2)] + \
                      [("tst", ti, sub, 0, hd, par) for sub in range(4) for hd in range(2) for par in range(2)] + \
                      [("tst", ti, sub, 1, "g") for sub in range(4)]
                rows = slice(g * 512, (g + 1) * 512)
                for (dst, c0, c1, nm) in ((VS, 0, 256, "VS"), (RKd, 256, 768, "RKd"), (RV, 768, 1024, "RV"), (RG, 1024, 1280, "RG")):
                    b.dma("sp", dst[rows, :].rearrange("(s p) c -> p s c", p=128), tst[ti][:, :, c0:c1], rk_, [(nm, g)])
        S.barrier()

        with ExitStack() as es:
            if 2 not in phases:
                npr = 0
            QT = b.sb(es, "QT", [128, 2, S_LEN], BF16)
            KT = b.sb(es, "KT", [128, 2, S_LEN], BF16)
            Vt = b.sb(es, "Vt", [128, NB, 256], BF16)
            triI = b.sb(es, "triI", [128, 128], BF16)
            onesr = b.sb(es, "onesr", [1, 128], BF16)
            maskd = b.sb(es, "maskd", [128, 4, 512], BF16)
            NS = 4
            E_ = [b.sb(es, f"E{i}", [128, 512], F32) for i in range(6)]
            SP = [b.sb(es, f"SP{i}", [128, 512], BF16) for i in range(6)]
            X_ = [b.sb(es, f"X{i}", [128, 512], F32) for i in range(NS)]
            C_ = [b.sb(es, f"C{i}", [128, 512], F32) for i in range(NS)]
            Z_ = [b.sb(es, f"Z{i}", [128, 512], F32) for i in range(NS)]
            W_ = [b.sb(es, f"W{i}", [128, 512], BF16) for i in range(NS)]
            car = [b.sb(es, f"car{i}", [1, 512], BF16) for i in range(2)]
            ost = [b.sb(es, f"ost{i}", [128, 4, 64], BF16) for i in range(2)]
            pz = [b.ps(es, f"pz{i}", [128, 512], F32) for i in range(NS)]
            pc = [b.ps(es, f"pc{i}", [128, 512], F32) for i in range(2)]
            poT = [b.ps(es, f"poT{i}", [64, 512], F32) for i in range(2)]
            OTs = [b.sb(es, f"OTs{i}", [64, 512], F32) for i in range(2)]
            identf = b.sb(es, "identf", [128, 128], F32)
            b.dma("sp", identf[:], ident_d, [], ["identf"])

            nqc = nq * 512
            for pr in range(2):
                b.dma("sp", QT[:, pr, 0:nqc], QS[pr, :, 0:nqc], [], [("QT", pr)])
                b.dma("sp", KT[:, pr, 0:nqc], KS[pr, :, 0:nqc], [], [("KT", pr)])
            for i8 in range(8):
                if i8 * 1024 >= nqc:
                    b.memset("dve", Vt[0:1, i8 * 8, 0:1], 0.0, [("Vt", i8)] if i8 else ["Vt"])
                    continue
                b.dma("sp", Vt[:, i8 * 8:(i8 + 1) * 8, :], VS[i8 * 1024:(i8 + 1) * 1024, :].rearrange("(n p) c -> p n c", p=128), [], ["Vt"] if i8 == 0 else [("Vt", i8)])
            S.op("sp", lambda e: None, reads=[("Vt", i8) for i8 in range(1, 8)], writes=["Vt"])
            b.dma("pool", triI[:], triI_d, [], ["triI"])
            b.dma("pool", maskd[:], maskd_d.rearrange("j p t -> p j t"), [], ["maskd"])
            b.memset("dve", onesr[:], 1.0, ["onesr"])
            ones2 = b.sb(es, "ones2", [64, 128], BF16)
            car2 = b.sb(es, "car2", [64, 512], BF16)
            b.memset("dve", ones2[:], 1.0, ["ones2"])
            b.memset("dve", car2[:], 0.0, [("car", 0)])
            zer = b.sb(es, "zer", [128, 256], BF16)
            b.memset("dve", zer[:], 0.0, ["zer"])

            for pr in range(npr):
                for qi in range(nq):
                    t0 = qi * 512
                    nkb = 4 * qi + 4
                    def stage_a(step):
                        kb = nkb - 1 - step
                        j = kb - 4 * qi
                        par = step % 2
                        for hp in range(2):
                            s = hp * 2 + par
                            ps_ = slice(hp * 64, (hp + 1) * 64)
                            b.mm(pz[s][:], KT[ps_, pr, kb * 128:(kb + 1) * 128], QT[ps_, pr, t0:t0 + 512], True, True,
                                 [("KT", pr), ("QT", pr)], [("pz", s)])
                        for hp in range(2):
                            s = hp * 2 + par
                            b.copy("dve", Z_[s][:], pz[s][:], [("pz", s)], [("Z", s)])
                        for hp in range(2):
                            s, s3 = hp * 2 + par, hp * 3 + step % 3
                            b.act(E_[s3][:], Z_[s][:], AF.Exp, [("Z", s)], [("E", s3)], scale=0.125)
                        for hp in range(2):
                            s3 = hp * 3 + step % 3
                            b.act(SP[s3][:], E_[s3][:], AF.Ln, [("E", s3)], [("SP", s3)], bias=1.0)
                        if j >= 0:
                            for hp in range(2):
                                s3 = hp * 3 + step % 3
                                b.tt("pool", SP[s3][:], SP[s3][:], maskd[:, j, :], ALU.mult, [("SP", s3), "maskd"], [("SP", s3)])
                                b.tt("pool", E_[s3][:], E_[s3][:], maskd[:, j, :], ALU.mult, [("E", s3), "maskd"], [("E", s3)])

                    def stage_b1(step):
                        par = step % 2
                        for hp in range(2):
                            s3 = hp * 3 + step % 3
                            b.mm(pc[hp][:], triI[:], SP[s3][:], True, step == 0, ["triI", ("SP", s3)], [("pc", hp)])
                        if step > 0:
                            for hp in range(2):
                                r_ = slice(hp * 32, hp * 32 + 1)
                                b.mm(pc[hp][:], ones2[r_, :], car2[r_, :], False, True, ["ones2", ("car", hp)], [("pc", hp)])
                        for hp in range(2):
                            s = hp * 2 + par
                            if step < nkb - 1:
                                b.copy("dve", car2[hp * 32:hp * 32 + 1, :], pc[hp][0:1, :], [("pc", hp)], [("car", hp)])
                            b.copy("dve", C_[s][:], pc[hp][:], [("pc", hp)], [("C", s)])
                            b.act(X_[s][:], C_[s][:], AF.Exp, [("C", s)], [("X", s)], scale=-1.0)

                    def stage_b2(step):
                        kb = nkb - 1 - step
                        par = step % 2
                        for hp in range(2):
                            s, s3 = hp * 2 + par, hp * 3 + step % 3
                            b.tt("pool", W_[s][:], E_[s3][:], X_[s][:], ALU.mult, [("E", s3), ("X", s)], [("W", s)])
                        for hp in range(2):
                            s = hp * 2 + par
                            h = pr * 2 + hp
                            b.mm(poT[hp][:], Vt[:, kb, h * 64:(h + 1) * 64], W_[s][:], step == 0, step == nkb - 1, [("W", s), "Vt"], [("poT", hp)])

                    stage_a(0)
                    stage_a(1)
                    stage_b1(0)
                    for step in range(nkb):
                        if step + 2 < nkb:
                            stage_a(step + 2)
                        if step + 1 < nkb:
                            stage_b1(step + 1)
                        stage_b2(step)
                    for hp in range(2):
                        s = hp
                        h = pr * 2 + hp
                        b.copy("dve", OTs[hp][:], poT[hp][:], [("poT", hp)], [("OTs", hp)])
                        for sub in range(4):
                            b.tr(pc[hp][:, sub * 64:(sub + 1) * 64], OTs[hp][:, sub * 128:(sub + 1) * 128], identf[0:64, 0:64],
                                 [("OTs", hp), "identf"], [("pc", hp)])
                        b.copy("dve", ost[s][:].rearrange("p a c -> p (a c)"), pc[hp][:, 0:256], [("pc", hp)], [("ost", s)])
                        b.dma("sp", att[t0:t0 + 512, h * 64:(h + 1) * 64].rearrange("(s p) c -> p s c", p=128), ost[s][:],
                              [("ost", s)], [("att_sb", h, qi)])
        S.barrier()

        with ExitStack() as es:
            if 3 not in phases:
                return b.finish(["mod_d"] + [("att_sb", h, qi) for h in range(2 * npr) for qi in range(nq)])
            dm = b.sb(es, "dm", [128, 2, 128], F32)
            g64 = b.sb(es, "g64", [128, 2], F32)
            gng = b.sb(es, "gng", [128, 256], F32)
            epsb = b.sb(es, "epsb", [128, 1], F32)
            qt = [b.sb(es, f"rqt{i}", [128, 3, 512], BF16) for i in range(4)]
            kt = [b.sb(es, f"rkt{i}", [128, 512], BF16) for i in range(4)]
            kd = [b.sb(es, f"rkd{i}", [128, 4, 256], BF16) for i in range(4)]
            vt = [b.sb(es, f"rvt{i}", [128, 4, 128], BF16) for i in range(4)]
            gt = [b.sb(es, f"rgt{i}", [128, 4, 128], BF16) for i in range(4)]
            stf = [b.sb(es, f"stf{i}", [128, 128], F32) for i in range(2)]
            stb = [[b.sb(es, f"stb{i}{k}", [128, 128], BF16) for k in range(2)] for i in range(2)]
            Pm = [b.sb(es, f"Pm{i}", [128, 128], BF16) for i in range(2)]
            of = [b.sb(es, f"of{i}", [128, 128], F32) for i in range(2)]
            st_ = [b.sb(es, f"rst{i}", [128, 6], F32) for i in range(2)]
            mv = [b.sb(es, f"rmv{i}", [128, 2], F32) for i in range(2)]
            rs = [b.sb(es, f"rrs{i}", [128, 1], F32) for i in range(2)]
            gg = [b.sb(es, f"gg{i}", [128, 128], F32) for i in range(2)]
            oo = [b.sb(es, f"oo{i}", [128, 4, 128], BF16) for i in range(4)]
            psc = [b.ps(es, f"psc{i}", [128, 128], F32) for i in range(2)]
            pso = [b.ps(es, f"pso{i}", [128, 128], F32) for i in range(2)]
            pkv = [b.ps(es, f"pkv{i}", [128, 128], F32) for i in range(2)]

            b.dma("sp", dm[:], dm_d.rearrange("h s c -> s h c"), [], ["dm"])
            b.dma("sp", g64[:], g64_d, [], ["g64"])
            b.dma("sp", gng[:], gn_g.partition_broadcast(128), [], ["gng"])
            b.memset("dve", epsb[:], EPS, ["epsb"])
            for hd in range(2):
                b.memset("dve", stf[hd][:], 0.0, [("stf", hd)])
                b.memset("dve", stb[hd][0][:], 0.0, [("stb", hd, 0)])
            nst = [0, 0]
            for g in range(ng):
                cols = slice(g * 512, (g + 1) * 512)
                rows = slice(g * 512, (g + 1) * 512)
                for hd in range(2):
                    bi = hd * 2 + g % 2
                    b.dma("sp", qt[bi][:], RQ[hd, :, :, cols].rearrange("v p t -> p v t"), [], [("rqt", bi)])
                    b.dma("sp", kt[bi][:], RK[hd, :, cols], [], [("rkt", bi)])
                    b.dma("sp", kd[bi][:], RKd[rows, hd * 256:(hd + 1) * 256].rearrange("(s p) c -> p s c", p=128), [], [("rkd", bi)])
                    b.dma("sp", vt[bi][:], RV[rows, hd * 128:(hd + 1) * 128].rearrange("(s p) c -> p s c", p=128), [], [("rvt", bi)])
                    b.dma("sp", gt[bi][:], RG[rows, hd * 128:(hd + 1) * 128].rearrange("(s p) c -> p s c", p=128), [], [("rgt", bi)])
                for sub in range(4):
                    tc_ = slice(sub * 128, (sub + 1) * 128)
                    for hd in range(2):
                        bi = hd * 2 + g % 2
                        s = hd
                        b.mm(psc[s][:], kt[bi][:, tc_], qt[bi][:, 0, tc_], True, True, [("rkt", bi), ("rqt", bi)], [("psc", s)])
                        b.tt("dve", Pm[s][:], psc[s][:], dm[:, hd, :], ALU.mult, [("psc", s), "dm"], [("Pm", s)])
                        k0 = nst[hd] % 2
                        b.mm(pso[s][:], Pm[s][:], vt[bi][:, sub, :], True, False, [("Pm", s), ("rvt", bi)], [("pso", s)])
                        b.mm(pso[s][:], qt[bi][:, 1, tc_], stb[hd][k0][:], False, False, [("rqt", bi), ("stb", hd, k0)], [("pso", s)])
                        b.mm(pkv[s][:], kd[bi][:, sub, 0:128], vt[bi][:, sub, :], True, True, [("rkd", bi), ("rvt", bi)], [("pkv", s)])
                        b.stt("dve", stf[hd][:], stf[hd][:], g64[:, hd:hd + 1], pkv[s][:], ALU.mult, ALU.add, [("stf", hd), "g64", ("pkv", s)], [("stf", hd)])
                        b.copy("act", stb[hd][1 - k0][:], stf[hd][:], [("stf", hd)], [("stb", hd, 1 - k0)])
                        b.mm(pso[s][:], qt[bi][:, 2, tc_], stb[hd][1 - k0][:], False, True, [("rqt", bi), ("stb", hd, 1 - k0)], [("pso", s)])
                        b.mm(pkv[s][:], kd[bi][:, sub, 128:256], vt[bi][:, sub, :], True, True, [("rkd", bi), ("rvt", bi)], [("pkv", s)])
                        b.stt("dve", stf[hd][:], stf[hd][:], g64[:, hd:hd + 1], pkv[s][:], ALU.mult, ALU.add, [("stf", hd), "g64", ("pkv", s)], [("stf", hd)])
                        b.copy("act", stb[hd][k0][:], stf[hd][:], [("stf", hd)], [("stb", hd, k0)])
                        b.copy("act", of[s][:], pso[s][:], [("pso", s)], [("of", s)])
                        S.op("dve", lambda e, s=s: e.bn_stats(out=st_[s][:], in_=of[s][:]), [("of", s)], [("rst", s)])
                        S.op("dve", lambda e, s=s: e.bn_aggr(out=mv[s][:], in_=st_[s][:]), [("rst", s)], [("rmv", s)])
                        b.act(rs[s][:], mv[s][:, 1:2], AF.Ln, [("rmv", s), "epsb"], [("rrs", s)], bias=epsb[:])
                        b.act(rs[s][:], rs[s][:], AF.Exp, [("rrs", s)], [("rrs", s)], scale=-0.5)
                        b.tt("pool", gg[s][:], gt[bi][:, sub, :], gng[:, hd * 128:(hd + 1) * 128], ALU.mult, [("rgt", bi), "gng"], [("gg", s)])
                        b.ts("dve", of[s][:], of[s][:], mv[s][:, 0:1], rs[s][:], ALU.subtract, ALU.mult, [("of", s), ("rmv", s), ("rrs", s)], [("of", s)])
                        b.tt("dve", oo[bi][:, sub, :], of[s][:], gg[s][:], ALU.mult, [("of", s), ("gg", s)], [("oo", bi, sub)])
                for hd in range(2):
                    bi = hd * 2 + g % 2
                    b.dma("sp", att[rows, 256 + hd * 128:256 + (hd + 1) * 128].rearrange("(s p) c -> p s c", p=128), oo[bi][:],
                          [("oo", bi, sub) for sub in range(4)], [("att_r", hd, g)])
        outs = ["mod_d"] + [("att_sb", h, qi) for h in range(2 * npr) for qi in range(nq)] + [("att_r", hd, g) for hd in range(2) for g in range(ng)]
        if b.fused:
            S.barrier()
            return outs
        return b.finish(outs)


def attn0_consts(hh):
    pos = np.arange(128)
    same = (pos[:, None] // 64) == (pos[None, :] // 64)
    dmm = np.zeros((2, 128, 128), np.float32)
    pat = np.zeros((4, 128), np.float32)
    kdec = np.zeros((128, 4), np.float32)
    g64 = np.zeros((128, 2), np.float32)
    for hd in range(2):
        hr = 2 * hh + hd
        lg = np.log1p(-np.exp2(-5.0 - hr))
        dmm[hd] = np.where(same, np.exp(lg * np.abs(pos[:, None] - pos[None, :])), 0.0) * 128 ** -0.5
        for par in range(2):
            inpar = (pos // 64) == par
            pat[hd * 2 + par] = np.where(inpar, np.exp(lg * (pos % 64 + 1.0)), 0.0)
            kdec[:, hd * 2 + par] = np.where(inpar, np.exp(lg * (63 - pos % 64)), 0.0) * 128 ** -0.5
        g64[:, hd] = np.exp(lg * 64)
    s_ = np.arange(128)[:, None]
    t_ = np.arange(512)[None, :]
    maskd = np.stack([(128 * j + s_ < t_) for j in range(4)]).astype(np.float32)
    return dict(ident=np.eye(128, dtype=np.float32), triI=np.tril(np.ones((128, 128), np.float32)),
                maskd=maskd, dm=dmm, pat=pat, kdec=kdec, g64=g64)


LAMBDA_INIT = 0.8 - 0.6 * float(np.exp(-0.3 * 1))
ACT_PSUM = True


def build_attn1(ng=NG, nq=NG, nhb=2, b=None):
    b = b or B()
    b.no_act_copy = True
    S = b.S
    x = b.din("x", [S_LEN, D], F32)
    c_row = b.din("c_row", [D], F32)
    w_ada = b.din("w_ada", [D, 6 * D], F32)
    b_ada = b.din("b_ada", [6 * D], F32)
    w_in = b.din("w_in", [D, 1536], F32)
    lamv = b.din("lamv", [4, 64], F32)
    subg = b.din("subg", [128], F32)
    ident_d = b.din("ident", [128, 128], F32)
    bdiag_d = b.din("bdiag", [4, 4, 128, 512], F32)
    posb_d = b.din("posb", [128, 4, 64], F32)
    rrow_d = b.din("rrow", [4, S_LEN], F32)
    att = b.dout("att", [S_LEN, 512], BF16)
    mod_o = b.dout("mod", [6 * D], F32)
    modS = b.dscr("modS1", [6 * D], F32)
    b.shared["modS"] = modS
    b.shared["att"] = att
    lamS = b.dscr("lamS", [1], F32)
    QD = b.dscr("QD", [4, 128, S_LEN], BF16)
    KD = b.dscr("KD", [4, 128, S_LEN], BF16)
    VD = b.dscr("VD", [S_LEN, 4 * 129], BF16)

    with ExitStack() as es0:
        ident = b.sb(es0, "ident", [128, 128], BF16)
        neglam = b.sb(es0, "neglam", [128, 1], F32)
        gsub = b.sb(es0, "gsub", [128, 128], F32)
        epsb = b.sb(es0, "epsb", [128, 1], F32)
        b.dma("pool", ident[:], ident_d, [], ["ident"])
        b.memset("dve", epsb[:], EPS, ["epsb"])
        emit_mod(b, es0, c_row, w_ada, b_ada, mod_o, modS)
        with ExitStack() as es:
            lv = b.sb(es, "lv", [1, 4, 64], F32)
            lp = b.sb(es, "lp", [1, 2, 64], F32)
            ls = b.sb(es, "ls", [1, 2], F32)
            ll = b.sb(es, "ll", [1, 1], F32)
            b.dma("sp", lv[:], lamv.rearrange("(o a) n -> o a n", o=1), [], ["lv"])
            b.tt("dve", lp[:, 0, :], lv[:, 0, :], lv[:, 1, :], ALU.mult, ["lv"], ["lp0"])
            b.tt("dve", lp[:, 1, :], lv[:, 2, :], lv[:, 3, :], ALU.mult, ["lv"], ["lp1"])
            b.red("dve", ls[:], lp[:], "sum", ["lp0", "lp1"], ["ls"])
            b.act(ls[:], ls[:], AF.Exp, ["ls"], ["ls"])
            b.tt("dve", ll[:], ls[:, 1:2], ls[:, 0:1], ALU.subtract, ["ls"], ["ll"])
            b.ts("dve", ll[:], ll[:], -LAMBDA_INIT, None, ALU.add, None, ["ll"], ["ll"])
            b.dma("sp", lamS.rearrange("(o n) -> o n", o=1), ll[:], ["ll"], ["lamS"])
            b.dma("sp", neglam[:], lamS.partition_broadcast(128), ["lamS"], ["neglam"])
            b.dma("sp", gsub[:], subg.partition_broadcast(128), [], ["gsub"])
            b.ts("dve", gsub[:], gsub[:], 1.0 - LAMBDA_INIT, None, ALU.mult, None, ["gsub"], ["gsub"])
        S.barrier()

        with ExitStack() as es:
            sc1p = b.sb(es, "sc1p", [128, D], F32)
            sh1 = b.sb(es, "sh1", [128, D], F32)
            wcs = [b.sb(es, f"wc{h}", [128, 8, 512], BF16) for h in range(3)]
            xt = [b.sb(es, f"xt{i}", [128, D], F32) for i in range(2)]
            uf = [b.sb(es, f"uf{i}", [128, D], F32) for i in range(2)]
            ub = [b.sb(es, f"ub{i}", [128, D], BF16) for i in range(2)]
            uTs = [b.sb(es, f"uT{i}", [128, 8, 512], BF16) for i in range(2)]
            fst = [b.sb(es, f"fst{i}", [128, 512], BF16) for i in range(4)]
            vst = [b.sb(es, f"vst{i}", [128, 4, 4, 129], BF16) for i in range(2)]
            pT = [b.ps(es, f"pT{i}", [128, 8, 128], BF16) for i in range(2)]
            pf = [b.ps(es, f"pf{i}", [128, 512], F32) for i in range(2)]
            pt = [b.ps(es, f"pt{i}", [128, 512], F32) for i in range(2)]
            b.dma("sp", sh1[:], modS[0:D].partition_broadcast(128), ["mod_s"], ["sh1"])
            b.dma("sp", sc1p[:], modS[D:2 * D].partition_broadcast(128), ["mod_s"], ["sc1p"])
            b.ts("dve", sc1p[:], sc1p[:], 1.0, None, ALU.add, None, ["sc1p"], ["sc1p"])
            for h in range(3):
                b.dma("pool", wcs[h][:], w_in[:, h * 512:(h + 1) * 512].rearrange("(kc p) n -> p kc n", p=128), [], ["wc"] if h == 0 else [("wc", h)])
            S.op("sp", lambda e: None, reads=[("wc", 1), ("wc", 2)], writes=["wc"])
            for i in range(2):
                b.memset("dve", vst[i][:], 1.0, [("vst", i)])
            nf = 0
            emit_uT_group(b, 0, x, sc1p, sh1, ident, xt, uf, ub, pT, uTs[0], 0)
            for g in range(ng):
                cols = slice(g * 512, (g + 1) * 512)
                if g + 1 < ng:
                    emit_uT_group(b, g + 1, x, sc1p, sh1, ident, xt, uf, ub, pT, uTs[(g + 1) % 2], (g + 1) % 2)
                uT = uTs[g % 2]
                uTk = [("uT", g % 2, s_) for s_ in range(4)]
                for fm in range(8):
                    pi = fm % 2
                    wt = wcs[fm // 4]
                    c0 = (fm % 4) * 128
                    for kc in range(8):
                        b.mm(pf[pi][:], wt[:, kc, c0:c0 + 128], uT[:, kc, :], kc == 0, kc == 7, uTk + ["wc"], [("pf", pi)])
                    si = nf % 4
                    nf += 1
                    b.copy("dve", fst[si][:], pf[pi][:], [("pf", pi)], [("fst", si)])
                    dst = QD[fm, :, cols] if fm < 4 else KD[fm - 4, :, cols]
                    b.dma("sp", dst, fst[si][:], [("fst", si)], [("FM", fm, g)])
                ti = g % 2
                for sub in range(4):
                    pi = sub % 2
                    for kc in range(8):
                        b.mm(pt[pi][:], uT[:, kc, sub * 128:(sub + 1) * 128], wcs[2][:, kc, :], kc == 0, kc == 7, uTk + ["wc"], [("pt", pi)])
                    b.copy("dve", vst[ti][:, sub, :, 0:128], pt[pi][:].rearrange("p (h d) -> p h d", h=4), [("pt", pi), ("vst", ti)], [("vst", ti, sub)])
                rows = slice(g * 512, (g + 1) * 512)
                b.dma("sp", VD[rows, :].rearrange("(s p) c -> p s c", p=128), vst[ti][:].rearrange("p s h d -> p s (h d)"),
                      [("vst", ti, sub) for sub in range(4)], [("vst", ti)])
        S.barrier()

        with ExitStack() as es:
            QA = b.sb(es, "QA", [128, 2, S_LEN], BF16)
            KA = b.sb(es, "KA", [128, 2, S_LEN], BF16)
            Vt = b.sb(es, "Vt", [128, NB, 128], BF16)
            bdg = b.sb(es, "bdg", [128, 4, 512], F32)
            posb = b.sb(es, "posb", [128, 4, 64], F32)
            onec = b.sb(es, "onec", [128, 1], BF16)
            identf = b.sb(es, "identf", [128, 128], F32)
            T_ = [b.sb(es, f"T{i}", [128, 512], F32) for i in range(4)]
            P_ = [b.sb(es, f"P{i}", [128, 512], BF16) for i in range(4)]
            Lacc = [b.sb(es, f"Lacc{i}", [128, 512], F32) for i in range(2)]
            Lacd = [b.sb(es, f"Lacd{i}", [128, 512], F32) for i in range(2)]
            Lhi = [b.sb(es, f"Lhi{i}", [128, 512], BF16) for i in range(2)]
            Llo = [b.sb(es, f"Llo{i}", [128, 512], BF16) for i in range(2)]
            OTs = [b.sb(es, f"OTs{i}", [128, 512], F32) for i in range(2)]
            rec = [b.sb(es, f"rec{i}", [128, 4], F32) for i in range(2)]
            On = [b.sb(es, f"On{i}", [128, 4, 128], F32) for i in range(2)]
            aa = b.sb(es, "aa", [128, 4, 128], F32)
            sq = b.sb(es, "sq", [128, 4, 128], F32)
            ssum = b.sb(es, "ssum", [128, 4], F32)
            ost = [b.sb(es, f"ost{i}", [128, 4, 128], BF16) for i in range(2)]
            pz = [b.ps(es, f"pz{i}", [128, 512], F32) for i in range(4)]
            pOT = [b.ps(es, f"pOT{i}", [128, 512], F32) for i in range(2)]
            ptr = b.ps(es, "ptr", [128, 4, 128], F32)
            pl = b.ps(es, "pl", [128, 8], F32)

            b.dma("sp", posb[:], posb_d, [], ["posb"])
            b.dma("sp", identf[:], ident_d, [], ["identf"])
            b.memset("dve", onec[:], 1.0, ["onec"])
            nqc = nq * 512
            for m in range(2):
                b.memset("dve", KA[64:65, m, :], 1.0, [("KA1", m)])
            nout = 0
            for h in range(2 * nhb):
                for m in range(2):
                    b.dma("sp", QA[0:64, m, 0:nqc], QD[h, m * 64:(m + 1) * 64, 0:nqc], [], [("QA", m)])
                    b.dma("pool", QA[64:65, m, 0:nqc], rrow_d[h:h + 1, 0:nqc], [], [("QAr", m)])
                    b.dma("sp", KA[0:64, m, 0:nqc], KD[h, m * 64:(m + 1) * 64, 0:nqc], [], [("KA", m)])
                b.dma("sp", bdg[:], bdiag_d[h].rearrange("j p t -> p j t"), [], ["bdg"])
                for i8 in range(8):
                    if i8 * 1024 >= nqc:
                        continue
                    b.dma("sp", Vt[:, i8 * 8:(i8 + 1) * 8, :], VD[i8 * 1024:(i8 + 1) * 1024, h * 129:h * 129 + 128].rearrange("(n p) d -> p n d", p=128),
                          [], [("Vt", i8)])
                vtk = [("Vt", i8) for i8 in range(8)]
                for qi in range(nq):
                    t0 = qi * 512
                    nkb = 4 * qi + 4
                    def stage_a(step):
                        kb = nkb - 1 - step
                        j = kb - 4 * qi
                        par = step % 2
                        for m in range(2):
                            s_ = m * 2 + par
                            b.mm(pz[s_][:], KA[0:65, m, kb * 128:(kb + 1) * 128], QA[0:65, m, t0:t0 + 512], True, True,
                                 [("KA", m), ("KA1", m), ("QA", m), ("QAr", m)], [("pz", s_)])
                        for m in range(2):
                            s_ = m * 2 + par
                            if j >= 0:
                                b.stt("dve", T_[s_][:], pz[s_][:], 0.125, bdg[:, j, :], ALU.mult, ALU.add, [("pz", s_), "bdg"], [("T", s_)])
                            elif not ACT_PSUM:
                                b.copy("dve", T_[s_][:], pz[s_][:], [("pz", s_)], [("T", s_)])
                        for m in range(2):
                            s_ = m * 2 + par
                            if j >= 0:
                                b.act(P_[s_][:], T_[s_][:], AF.Exp, [("T", s_)], [("P", s_)])
                            elif ACT_PSUM:
                                off = 4 * qi - kb
                                b.act(P_[s_][:], pz[s_][:], AF.Exp, [("pz", s_), "posb"], [("P", s_)], bias=posb[:, h, off:off + 1], scale=0.125)
                            else:
                                off = 4 * qi - kb
                                b.act(P_[s_][:], T_[s_][:], AF.Exp, [("T", s_), "posb"], [("P", s_)], bias=posb[:, h, off:off + 1], scale=0.125)

                    def stage_b(step):
                        kb = nkb - 1 - step
                        par = step % 2
                        for m in range(2):
                            s_ = m * 2 + par
                            b.mm(pOT[m][:], Vt[:, kb, :], P_[s_][:], step == 0, step == nkb - 1, [("P", s_)] + vtk, [("pOT", m)])
                            if step == 0:
                                b.copy("pool", Lacc[m][:], P_[s_][:], [("P", s_)], [("Lacc", m)])
                            elif step == 1:
                                b.copy("dve", Lacd[m][:], P_[s_][:], [("P", s_)], [("Lacd", m)])
                            elif step % 3 != 0:
                                b.tt("dve", Lacd[m][:], Lacd[m][:], P_[s_][:], ALU.add, [("Lacd", m), ("P", s_)], [("Lacd", m)])
                            else:
                                b.tt("pool", Lacc[m][:], Lacc[m][:], P_[s_][:], ALU.add, [("Lacc", m), ("P", s_)], [("Lacc", m)])

                    stage_a(0)
                    for step in range(nkb):
                        if step + 1 < nkb:
                            stage_a(step + 1)
                        stage_b(step)
                    oi = nout % 2
                    nout += 1
                    for m in range(2):
                        b.tt("pool", Lacc[m][:], Lacc[m][:], Lacd[m][:], ALU.add, [("Lacc", m), ("Lacd", m)], [("Lacc", m)])
                        b.copy("pool", Lhi[m][:], Lacc[m][:], [("Lacc", m)], [("Lhi", m)])
                        b.tt("pool", Llo[m][:], Lacc[m][:], Lhi[m][:], ALU.subtract, [("Lacc", m), ("Lhi", m)], [("Llo", m)])
                        for sub in range(4):
                            c_ = m * 4 + sub
                            b.mm(pl[:, c_:c_ + 1], Lhi[m][:, sub * 128:(sub + 1) * 128], onec[:], True, False, [("Lhi", m), "onec"], [("pl", c_)])
                            b.mm(pl[:, c_:c_ + 1], Llo[m][:, sub * 128:(sub + 1) * 128], onec[:], False, True, [("Llo", m), "onec"], [("pl", c_)])
                        S.op("dve", lambda e, m=m: e.reciprocal(out=rec[m][:], in_=pl[:, m * 4:(m + 1) * 4]),
                             [("pl", m * 4 + sub) for sub in range(4)], [("rec", m)])
                        b.copy("dve", OTs[m][:], pOT[m][:], [("pOT", m)], [("OTs", m)])
                        for sub in range(4):
                            b.tr(ptr[:, sub, :], OTs[m][:, sub * 128:(sub + 1) * 128], identf[:], [("OTs", m), "identf"], ["ptr"])
                        b.tt("dve", On[m][:], ptr[:], rec[m][:].unsqueeze(2).to_broadcast([128, 4, 128]), ALU.mult, ["ptr", ("rec", m)], [("On", m)])
                    b.stt("dve", aa[:], On[1][:], neglam[:, 0:1], On[0][:], ALU.mult, ALU.add, [("On", 0), ("On", 1), "neglam"], ["aa"])
                    b.tt("pool", sq[:], aa[:], aa[:], ALU.mult, ["aa"], ["sq"])
                    b.red("dve", ssum[:], sq[:], "sum", ["sq"], ["ssum"])
                    b.act(ssum[:], ssum[:], AF.Ln, ["ssum", "epsb"], ["ssum"], bias=epsb[:], scale=1.0 / 128.0)
                    b.act(ssum[:], ssum[:], AF.Exp, ["ssum"], ["ssum"], scale=-0.5)
                    b.tt("dve", aa[:], aa[:], ssum[:].unsqueeze(2).to_broadcast([128, 4, 128]), ALU.mult, ["aa", "ssum"], ["aa"])
                    b.tt("pool", ost[oi][:], aa[:], gsub[:].unsqueeze(1).to_broadcast([128, 4, 128]), ALU.mult, ["aa", "gsub"], [("ost", oi)])
                    b.dma("sp", att[t0:t0 + 512, h * 128:(h + 1) * 128].rearrange("(s p) c -> p s c", p=128), ost[oi][:],
                          [("ost", oi)], [("att", h, qi)])
        outs = ["mod_d"] + [("att", h, qi) for h in range(2 * nhb) for qi in range(nq)]
        if b.fused:
            S.barrier()
            return outs
        return b.finish(outs)


def attn1_consts(hh):
    bd = np.zeros((4, 4, 128, 512), np.float32)
    posb = np.zeros((128, 4, 64), np.float32)
    rrow = np.zeros((4, S_LEN), np.float32)
    s_ = np.arange(128)[:, None].astype(np.float64)
    t_ = np.arange(512)[None, :].astype(np.float64)
    for h in range(4):
        gh = 4 * hh + h
        slope = 2.0 ** (-(gh + 1.0))
        rrow[h] = (-8.0 * slope * (np.arange(S_LEN) % 512)).astype(np.float32)
        for j in range(4):
            s_abs = 128 * j + s_
            allowed = (s_abs // 64) <= (t_ // 64)
            bd[h, j] = np.where(allowed, -slope * np.abs(t_ - s_abs) + slope * t_, -30000.0)
        for off in range(64):
            posb[:, h, off] = slope * (np.arange(128) - 128.0 * off)
    return dict(ident=np.eye(128, dtype=np.float32), bdiag=bd, posb=posb, rrow=rrow)


def _run(nc, in_maps):
    res = run_bass_kernel_spmd(nc, in_maps, core_ids=list(range(NCORES)))
    return res.results


def _post_inputs(l, xfull, att_full, mods, w_out, inp):
    lnp = np.ascontiguousarray(np.stack([inp["ln1_g"][l], inp["ln1_b"][l], inp["ln2_g"][l], inp["ln2_b"][l]]).astype(np.float32))
    wr = np.ascontiguousarray(np.concatenate([inp["moe_w_group"][l], inp["moe_w_router"][l]], axis=1))
    br = np.ascontiguousarray(np.concatenate([inp["moe_b_group"][l], inp["moe_b_router"][l]]))
    cs = post_consts()
    maps = []
    for c in range(NCORES):
        b_, hh = c // 2, c % 2
        rows = slice(hh * TOK, (hh + 1) * TOK)
        d = dict(xs=np.ascontiguousarray(xfull[b_, rows]), att=np.ascontiguousarray(att_full[b_][rows]), mod=mods[b_],
                 w_out=w_out, lnp=lnp, wr=wr, br=br, w1=inp["moe_w1"][l], w3=inp["moe_w3"][l], w2=inp["moe_w2"][l])
        d.update(cs)
        maps.append(d)
    return maps


def kernel_unfused(**inp):
    inp = {k: np.asarray(v) for k, v in inp.items()}
    x = inp["x"]
    Bn = x.shape[0]
    w = inp["even_w_in"][0]
    maps = []
    for c in range(NCORES):
        b_, hh = c // 2, c % 2
        a = slice(hh * 256, (hh + 1) * 256)
        sq, sk, sv = w[:, 0:512], w[:, 512:1024], w[:, 1024:1536]
        rq, rk, rv, rg = w[:, 1536:2048], w[:, 2048:2560], w[:, 2560:3072], w[:, 3072:3584]
        w_in = np.ascontiguousarray(np.concatenate([sq[:, a], sk[:, a], rq[:, a], rk[:, a], sv[:, a], rk[:, a], rv[:, a], rg[:, a]], axis=1))
        d = dict(x=np.ascontiguousarray(x[b_]), c_row=np.ascontiguousarray(inp["c"][b_]), w_ada=inp["w_ada"][0], b_ada=inp["b_ada"][0],
                 w_in=w_in, gn_g=np.ascontiguousarray(inp["ret_gn_g"][0][a]))
        d.update(attn0_consts(hh))
        maps.append(d)
    r = _run(build_attn0(), maps)
    att_full, mods = [], []
    for b_ in range(Bn):
        a0, a1 = np.asarray(r[2 * b_]["att"]), np.asarray(r[2 * b_ + 1]["att"])
        att_full.append(np.concatenate([a0[:, :256], a1[:, :256], a0[:, 256:], a1[:, 256:]], axis=1))
        mods.append(np.asarray(r[2 * b_]["mod"]))
    post_nc = build_post()
    r = _run(post_nc, _post_inputs(0, x, att_full, mods, inp["even_w_out"][0], inp))
    x1 = np.stack([np.concatenate([np.asarray(r[2 * b_]["xo"]), np.asarray(r[2 * b_ + 1]["xo"])], axis=0) for b_ in range(Bn)])
    w = inp["odd_w_in"][0]
    lamv = np.ascontiguousarray(np.stack([inp["lambda_q1"][0], inp["lambda_k1"][0], inp["lambda_q2"][0], inp["lambda_k2"][0]]))
    maps = []
    for c in range(NCORES):
        b_, hh = c // 2, c % 2
        a = slice(hh * 512, (hh + 1) * 512)
        w_in = np.ascontiguousarray(np.concatenate([w[:, 0:1024][:, a], w[:, 1024:2048][:, a], w[:, 2048:3072][:, a]], axis=1))
        d = dict(x=np.ascontiguousarray(x1[b_]), c_row=np.ascontiguousarray(inp["c"][b_]), w_ada=inp["w_ada"][1], b_ada=inp["b_ada"][1],
                 w_in=w_in, lamv=lamv, subg=np.ascontiguousarray(inp["diff_subln_g"][0]))
        d.update(attn1_consts(hh))
        maps.append(d)
    r = _run(build_attn1(), maps)
    att_full, mods = [], []
    for b_ in range(Bn):
        att_full.append(np.concatenate([np.asarray(r[2 * b_]["att"]), np.asarray(r[2 * b_ + 1]["att"])], axis=1))
        mods.append(np.asarray(r[2 * b_]["mod"]))
    r = _run(build_post(), _post_inputs(1, x1, att_full, mods, inp["odd_w_out"][0], inp))
    out = np.stack([np.concatenate([np.asarray(r[2 * b_]["xo"]), np.asarray(r[2 * b_ + 1]["xo"])], axis=0) for b_ in range(Bn)])
    return out.astype(np.float32)


PAIRS = [[0, 1], [2, 3], [4, 5], [6, 7]]


def build_fused():
    b = B()
    b.fused = True
    b.no_act_copy = True
    S = b.S
    G0 = b.dscr("G0", [2 * S_LEN, 512], BF16)
    G1 = b.dscr("G1", [2 * S_LEN, 512], BF16)
    X1h = b.dscr("X1h", [TOK, D], F32)
    X1f = b.dscr("X1f", [S_LEN, D], F32)
    gidx = b.nc.dram_tensor("gidx", [128, 2, NT], I32, kind="ExternalInput").ap()
    out = b.nc.dram_tensor("out", [TOK, D], F32, kind="ExternalOutput").ap()

    agn = [0]

    def allgather(src, dst, R, C, dt, esz):
        rows = (2 * 1024 * 1024) // (C * esz)
        nch = R // rows
        agn[0] += 1
        ss = [b.nc.dram_tensor(f"ag{agn[0]}_s{i}", [rows, C], dt, kind="Internal").ap() for i in range(nch)]
        gg = [b.nc.dram_tensor(f"ag{agn[0]}_g{i}", [2 * rows, C], dt, kind="Internal").ap() for i in range(nch)]
        for i in range(nch):
            b.dma("sp", ss[i][:, :], src[i * rows:(i + 1) * rows, :], [], [])
        S.barrier()
        for i in range(nch):
            S.cc(lambda e, i=i: e.collective_compute("AllGather", ALU.bypass, replica_groups=PAIRS, ins=[ss[i][:, :]], outs=[gg[i][:, :]]), [], [])
        S.barrier()
        for i in range(nch):
            for r in range(2):
                b.dma("sp", dst[r * R + i * rows:r * R + (i + 1) * rows, :], gg[i][r * rows:(r + 1) * rows, :], [], [])
        S.barrier()

    b.pfx, b.ovr = "a0_", {}
    build_attn0(b=b)
    att0, mod0 = b.shared["att"], b.shared["modS"]
    allgather(att0, G0, S_LEN, 512, BF16, 2)
    b.pfx, b.ovr = "p0_", {"mod": mod0, "xo": X1h}
    build_post(b=b, att_gather=(G0, gidx))
    allgather(X1h, X1f, TOK, D, F32, 4)
    b.pfx, b.ovr = "a1_", {"x": X1f}
    build_attn1(b=b)
    att1, mod1 = b.shared["att"], b.shared["modS"]
    allgather(att1, G1, S_LEN, 512, BF16, 2)
    b.pfx, b.ovr = "p1_", {"mod": mod1, "xs": X1h, "xo": out}
    build_post(b=b, att_gather=(G1, gidx))
    return b.finish([])


def kernel(**inp):
    inp = {k: np.asarray(v) for k, v in inp.items()}
    x = inp["x"]
    Bn = x.shape[0]
    w0 = inp["even_w_in"][0]
    w1_ = inp["odd_w_in"][0]
    lamv = np.ascontiguousarray(np.stack([inp["lambda_q1"][0], inp["lambda_k1"][0], inp["lambda_q2"][0], inp["lambda_k2"][0]]))
    wo0 = inp["even_w_out"][0]
    wo0p = np.ascontiguousarray(np.concatenate([wo0[0:256], wo0[512:768], wo0[256:512], wo0[768:1024]], axis=0))
    pcs = post_consts()

    def post_in(l, w_out):
        return dict(w_out=w_out,
                    lnp=np.ascontiguousarray(np.stack([inp["ln1_g"][l], inp["ln1_b"][l], inp["ln2_g"][l], inp["ln2_b"][l]]).astype(np.float32)),
                    wr=np.ascontiguousarray(np.concatenate([inp["moe_w_group"][l], inp["moe_w_router"][l]], axis=1)),
                    br=np.ascontiguousarray(np.concatenate([inp["moe_b_group"][l], inp["moe_b_router"][l]])),
                    w1=inp["moe_w1"][l], w3=inp["moe_w3"][l], w2=inp["moe_w2"][l], tri=pcs["tri"], eoff=pcs["eoff"])
    p0, p1 = post_in(0, wo0p), post_in(1, inp["odd_w_out"][0])
    maps = []
    for c in range(NCORES):
        b_, hh = c // 2, c % 2
        a = slice(hh * 256, (hh + 1) * 256)
        sq, sk, sv = w0[:, 0:512], w0[:, 512:1024], w0[:, 1024:1536]
        rq, rk, rv, rg = w0[:, 1536:2048], w0[:, 2048:2560], w0[:, 2560:3072], w0[:, 3072:3584]
        w_in0 = np.ascontiguousarray(np.concatenate([sq[:, a], sk[:, a], rq[:, a], rk[:, a], sv[:, a], rk[:, a], rv[:, a], rg[:, a]], axis=1))
        a2 = slice(hh * 512, (hh + 1) * 512)
        w_in1 = np.ascontiguousarray(np.concatenate([w1_[:, 0:1024][:, a2], w1_[:, 1024:2048][:, a2], w1_[:, 2048:3072][:, a2]], axis=1))
        c0 = attn0_consts(hh)
        c1 = attn1_consts(hh)
        p_ = np.arange(128)[:, None, None]
        r_ = np.arange(2)[None, :, None]
        t_ = np.arange(NT)[None, None, :]
        gidx = (r_ * S_LEN + hh * TOK + t_ * 128 + p_).astype(np.int32)
        d = {"ident": c0["ident"], "gidx": np.ascontiguousarray(gidx)}
        d.update({"a0_x": np.ascontiguousarray(x[b_]), "a0_c_row": np.ascontiguousarray(inp["c"][b_]), "a0_w_ada": inp["w_ada"][0],
                  "a0_b_ada": inp["b_ada"][0], "a0_w_in": w_in0, "a0_gn_g": np.ascontiguousarray(inp["ret_gn_g"][0][a])})
        d.update({"a0_" + k: v for k, v in c0.items() if k != "ident"})
        d.update({"p0_xs": np.ascontiguousarray(x[b_, hh * TOK:(hh + 1) * TOK])})
        d.update({"p0_" + k: v for k, v in p0.items()})
        d.update({"a1_c_row": np.ascontiguousarray(inp["c"][b_]), "a1_w_ada": inp["w_ada"][1], "a1_b_ada": inp["b_ada"][1],
                  "a1_w_in": w_in1, "a1_lamv": lamv, "a1_subg": np.ascontiguousarray(inp["diff_subln_g"][0])})
        d.update({"a1_" + k: v for k, v in c1.items() if k != "ident"})
        d.update({"p1_" + k: v for k, v in p1.items()})
        maps.append(d)
    r = _run(build_fused(), maps)
    out = np.stack([np.concatenate([np.asarray(r[2 * b_]["out"]), np.asarray(r[2 * b_ + 1]["out"])], axis=0) for b_ in range(Bn)])
    return out.astype(np.float32)
```

```python
from contextlib import ExitStack
import numpy as np
import ml_dtypes
import concourse.bass as bass
import concourse.mybir as mybir
from concourse.bass_utils import run_bass_kernel_spmd

F32 = mybir.dt.float32
BF16 = mybir.dt.bfloat16
I32 = mybir.dt.int32
AF = mybir.ActivationFunctionType
ALU = mybir.AluOpType
AX = mybir.AxisListType

D = 1024
S_LEN = 8192
NCORES = 8
ALPHA = (2.0 * 2) ** 0.25
EPS = 1e-5
NE = 32
CAP = 512
TOK = 4096
NT = TOK // 128
BIG = 1.0e30


class Sched:
    NDMA = 8
    ENGS = ("pe", "act", "dve", "pool", "sp")

    def __init__(self, nc, same_engine_sync=True):
        self.nc = nc
        self.ops = []
        self.state = {}
        self.same = same_engine_sync
        self.last = {}
        self.dma_since = set()

    def op(self, eng, fn, reads=(), writes=(), dma=False, nosame=False, extra=()):
        oid = len(self.ops)
        deps = set(extra)
        for k in reads:
            st = self.state.get(k)
            if st is not None and st[0] is not None:
                deps.add(st[0])
        for k in writes:
            st = self.state.get(k)
            if st is not None:
                if st[0] is not None:
                    deps.add(st[0])
                deps.update(st[1].values())
                deps.update(st[2])
        deps.discard(oid)
        self.ops.append(dict(eng=eng, fn=fn, deps=deps, dma=dma, nosame=nosame))
        for k in reads:
            st = self.state.setdefault(k, [None, {}, set()])
            if dma:
                st[2].add(oid)
            else:
                st[1][eng] = oid
        for k in writes:
            self.state[k] = [oid, {}, set()]
        if dma:
            self.dma_since.add(oid)
        else:
            self.last[eng] = oid
        return oid

    def dma(self, eng, fn, reads=(), writes=()):
        return self.op(eng, fn, reads, writes, dma=True)

    def cc(self, fn, reads=(), writes=()):
        oid = self.op("pool", fn, reads, writes, dma=True)
        self.ops[oid]["cc"] = True
        return oid

    def barrier(self):
        deps = set(self.last.values()) | self.dma_since
        self.dma_since = set()
        for e in self.ENGS:
            self.op(e, lambda eng: None, extra=deps, nosame=False)
        self.state = {}

    def finalize(self, sems):
        ops = self.ops

        def skip_same(o, po):
            if po["dma"] or o["dma"] or po["eng"] != o["eng"]:
                return False
            return o["eng"] in ("pe", "sp") or not self.same or o["nosame"]

        signal = [False] * len(ops)
        for o in ops:
            for p in o["deps"]:
                po = ops[p]
                if po["dma"] or skip_same(o, po):
                    continue
                signal[p] = True
        cnt = {e: 0 for e in self.ENGS}
        dcnt = {}
        event = [None] * len(ops)
        for i, o in enumerate(ops):
            if o.get("cc"):
                ncc = dcnt.get("cc", 0) + 1
                dcnt["cc"] = ncc
                event[i] = ("cc", ncc)
                o["prev"] = ("cc", ncc - 1) if ncc > 1 else None
            elif o["dma"]:
                e = o["eng"]
                n = dcnt.get(e, 0)
                dcnt[e] = n + 1
                j, m = n % self.NDMA, n // self.NDMA
                event[i] = (("dma", e, j), 16 * (m + 1))
                o["prev"] = (("dma", e, j), 16 * m) if m > 0 else None
            elif signal[i]:
                cnt[o["eng"]] += 1
                event[i] = (o["eng"], cnt[o["eng"]])
        seen = {e: {} for e in self.ENGS}
        per_eng = {e: [] for e in self.ENGS}
        nwaits = 0
        for i, o in enumerate(ops):
            e = o["eng"]
            need = {}
            for p in o["deps"]:
                po = ops[p]
                if skip_same(o, po):
                    continue
                ev = event[p]
                if ev is None:
                    continue
                if need.get(ev[0], 0) < ev[1]:
                    need[ev[0]] = ev[1]
            if o["dma"] and o["prev"] is not None:
                k, v = o["prev"]
                if need.get(k, 0) < v:
                    need[k] = v
            waits = []
            for k, v in need.items():
                if seen[e].get(k, 0) >= v:
                    continue
                seen[e][k] = v
                waits.append((k, v))
            nwaits += len(waits)
            per_eng[e].append((waits, o["fn"], event[i]))
        self.per_eng = per_eng
        self.sems = sems
        self.stats = dict(n_ops=len(ops), n_waits=nwaits, per_eng={e: len(v) for e, v in per_eng.items()})
        return per_eng

    def replay(self, ename, eng):
        sems = self.sems
        for waits, fn, ev in self.per_eng[ename]:
            for k, v in waits:
                eng.wait_ge(sems[k], v)
            ins = fn(eng)
            if ev is not None:
                if ins is None:
                    ins = eng.nop()
                ins.then_inc(sems[ev[0]], 16 if isinstance(ev[0], tuple) else 1)


class B:
    def __init__(self):
        self.nc = bass.Bass("TRN2", target_bir_lowering=False)
        self.S = Sched(self.nc)
        self.n = 0
        self.bregs = {}
        self.pfx = ""
        self.ovr = {}
        self.fused = False
        self.shared = {}
        self.no_act_copy = False

    def din(self, name, shape, dt):
        if name in self.ovr:
            return self.ovr[name]
        key = ("in", name, tuple(shape))
        if self.fused and name in ("ident",):
            if key not in self.shared:
                self.shared[key] = self.nc.dram_tensor(name, list(shape), dt, kind="ExternalInput").ap()
            return self.shared[key]
        return self.nc.dram_tensor(self.pfx + name, list(shape), dt, kind="ExternalInput").ap()

    def dout(self, name, shape, dt):
        if name in self.ovr:
            return self.ovr[name]
        if self.fused:
            return self.nc.dram_tensor(self.pfx + name, list(shape), dt, kind="Internal").ap()
        return self.nc.dram_tensor(name, list(shape), dt, kind="ExternalOutput").ap()

    def dscr(self, name, shape, dt):
        return self.nc.dram_tensor(self.pfx + name, list(shape), dt, kind="Internal").ap()

    def sb(self, es, name, shape, dt):
        return es.enter_context(self.nc.sbuf_tensor("sb_" + self.pfx + name, list(shape), dt))

    def ps(self, es, name, shape, dt):
        return es.enter_context(self.nc.psum_tensor("ps_" + self.pfx + name, list(shape), dt))

    def mm(self, out, lhsT, rhs, start, stop, r, w):
        self.S.op("pe", lambda e: e.matmul(out, lhsT=lhsT, rhs=rhs, start=start, stop=stop), r, w)

    def tr(self, out, in_, ident, r, w):
        self.S.op("pe", lambda e: e.transpose(out=out, in_=in_, identity=ident), r, w)

    def act(self, out, in_, func, r, w, bias=None, scale=1.0, accum=None):
        def f(e):
            kw = {}
            if bias is not None:
                kw["bias"] = bias
            if accum is not None:
                kw["accum_out"] = accum
            return e.activation(out=out, in_=in_, func=func, scale=scale, **kw)
        self.S.op("act", f, r, w)

    def tt(self, eng, out, in0, in1, op, r, w):
        self.S.op(eng, lambda e: e.tensor_tensor(out=out, in0=in0, in1=in1, op=op), r, w)

    def ts(self, eng, out, in0, s1, s2, op0, op1, r, w):
        if s2 is None:
            self.S.op(eng, lambda e: e.tensor_scalar(out=out, in0=in0, scalar1=s1, scalar2=None, op0=op0), r, w)
        else:
            self.S.op(eng, lambda e: e.tensor_scalar(out=out, in0=in0, scalar1=s1, scalar2=s2, op0=op0, op1=op1), r, w)

    def stt(self, eng, out, in0, scalar, in1, op0, op1, r, w):
        self.S.op(eng, lambda e: e.scalar_tensor_tensor(out=out, in0=in0, scalar=scalar, in1=in1, op0=op0, op1=op1), r, w)

    def copy(self, eng, out, in_, r, w):
        if eng == "act" and getattr(self, "no_act_copy", False):
            eng = "dve"
        if eng == "act":
            self.S.op("act", lambda e: e.activation(out=out, in_=in_, func=AF.Identity), r, w)
        else:
            self.S.op(eng, lambda e: e.tensor_copy(out=out, in_=in_), r, w)

    def memset(self, eng, out, val, w):
        self.S.op(eng, lambda e: e.memset(out, val), (), w)

    def red(self, eng, out, in_, op, r, w):
        if op == "max":
            self.S.op(eng, lambda e: e.reduce_max(out=out, in_=in_, axis=AX.X), r, w)
        else:
            self.S.op(eng, lambda e: e.reduce_sum(out=out, in_=in_, axis=AX.X), r, w)

    def dma(self, q, out, in_, r, w):
        self.S.dma(q, lambda e: e.dma_start(out=out, in_=in_), r, w)

    def _breg(self, e, bound):
        if bound not in self.bregs:
            self.bregs[bound] = e.to_reg(bound)
        return self.bregs[bound]

    def scatter(self, out_dram, idx, in_sb, bound, r, w):
        self.S.dma("pool", lambda e: e.indirect_dma_start(
            out=out_dram, out_offset=bass.IndirectOffsetOnAxis(ap=idx, axis=0), in_=in_sb, in_offset=None,
            bounds_check=self._breg(e, bound), oob_is_err=False), r, w)

    def gather(self, out_sb, in_dram, idx, bound, r, w):
        self.S.dma("pool", lambda e: e.indirect_dma_start(
            out=out_sb, out_offset=None, in_=in_dram, in_offset=bass.IndirectOffsetOnAxis(ap=idx, axis=0),
            bounds_check=self._breg(e, bound), oob_is_err=False), r, w)

    def layernorm(self, es_bufs, y, out, g_t, b_t, key_in, key_out, tag):
        st, mv, rstd, epsb = es_bufs["st"], es_bufs["mv"], es_bufs["rstd"], es_bufs["epsb"]
        S = self.S
        for c in range(2):
            S.op("dve", lambda e, c=c: e.bn_stats(out=st[:, c, :], in_=y[:, c * 512:(c + 1) * 512]),
                 [key_in], [("st", tag, c)])
        S.op("dve", lambda e: e.bn_aggr(out=mv[:], in_=st[:]), [("st", tag, 0), ("st", tag, 1)], [("mv", tag)])
        self.act(rstd[:], mv[:, 1:2], AF.Ln, [("mv", tag), "epsb"], [("rstd", tag)], bias=epsb[:])
        self.act(rstd[:], rstd[:], AF.Exp, [("rstd", tag)], [("rstd", tag)], scale=-0.5)
        self.ts("dve", out, y, mv[:, 0:1], rstd[:], ALU.subtract, ALU.mult, [key_in, ("mv", tag), ("rstd", tag)], [key_out])
        self.tt("pool", out, out, g_t, ALU.mult, [key_out, "lnp"], [key_out])
        self.tt("pool", out, out, b_t, ALU.add, [key_out, "lnp"], [key_out])

    def finish(self, out_keys):
        S = self.S
        nc = self.nc
        S.op("sp", lambda e: None, reads=list(out_keys))
        with ExitStack() as es:
            names = ["pe", "act", "dve", "pool", "sp", "cc"] + [("dma", q, j) for q in ("sp", "act", "pool") for j in range(Sched.NDMA)]
            sems = {n: es.enter_context(nc.semaphore("s_" + (n if isinstance(n, str) else "_".join(map(str, n))))) for n in names}
            S.finalize(sems)
            with nc.Block() as block:
                @block.tensor
                def _(e):
                    S.replay("pe", e)

                @block.scalar
                def _(e):
                    S.replay("act", e)

                @block.vector
                def _(e):
                    S.replay("dve", e)

                @block.gpsimd
                def _(e):
                    S.replay("pool", e)

                @block.sync
                def _(e):
                    S.replay("sp", e)
        return nc


def build_post(b=None, att_gather=None):
    b = b or B()
    S = b.S
    xs = b.din("xs", [TOK, D], F32)
    att = b.din("att", [TOK, D], BF16) if att_gather is None else None
    mod = b.din("mod", [6 * D], F32)
    w_out = b.din("w_out", [D, D], F32)
    lnp = b.din("lnp", [4, D], F32)
    wr = b.din("wr", [D, 36], F32)
    br = b.din("br", [36], F32)
    w1 = b.din("w1", [NE, D, 512], F32)
    w3 = b.din("w3", [NE, D, 512], F32)
    w2 = b.din("w2", [NE, 512, D], F32)
    ident_d = b.din("ident", [128, 128], F32)
    tri_d = b.din("tri", [128, 128], F32)
    eoff_d = b.din("eoff", [NE], F32)
    xo = b.dout("xo", [TOK, D], F32)
    X1 = b.dscr("X1", [TOK, D], F32)
    U = b.dscr("U", [TOK, D], BF16)
    US = b.dscr("US", [NE * CAP, D], BF16)
    YS = b.dscr("YS", [NE * CAP, D], F32)

    with ExitStack() as es0:
        ident = b.sb(es0, "ident", [128, 128], BF16)
        tri = b.sb(es0, "tri", [128, 128], BF16)
        ones = b.sb(es0, "ones", [128, 128], BF16)
        epsb = b.sb(es0, "epsb", [128, 1], F32)
        g1p = b.sb(es0, "g1p", [128, D], F32)
        sh2 = b.sb(es0, "sh2", [128, D], F32)
        sc2p = b.sb(es0, "sc2p", [128, D], F32)
        g2p = b.sb(es0, "g2p", [128, D], F32)
        lnt = b.sb(es0, "lnt", [128, 4, D], F32)
        st = b.sb(es0, "st", [128, 2, 6], F32)
        mv = b.sb(es0, "mv", [128, 2], F32)
        rstd = b.sb(es0, "rstd", [128, 1], F32)
        lnb = dict(st=st, mv=mv, rstd=rstd, epsb=epsb)
        gate1 = b.sb(es0, "gate1", [128, NT], F32)
        gate2 = b.sb(es0, "gate2", [128, NT], F32)
        dest1 = b.sb(es0, "dest1", [128, NT], I32)
        dest2 = b.sb(es0, "dest2", [128, NT], I32)

        b.dma("pool", ident[:], ident_d, [], ["ident"])
        b.dma("pool", tri[:], tri_d, [], ["tri"])
        if att_gather is not None:
            gidx_sb = b.sb(es0, "gidx", [128, 2, NT], I32)
            b.dma("sp", gidx_sb[:], att_gather[1], [], ["gidx"])
            att_gather = (att_gather[0], gidx_sb)
        b.memset("dve", ones[:], 1.0, ["ones"])
        b.memset("dve", epsb[:], EPS, ["epsb"])
        b.dma("sp", g1p[:], mod[2 * D:3 * D].partition_broadcast(128), [], ["g1p"])
        b.dma("sp", sh2[:], mod[3 * D:4 * D].partition_broadcast(128), [], ["sh2"])
        b.dma("sp", sc2p[:], mod[4 * D:5 * D].partition_broadcast(128), [], ["sc2p"])
        b.dma("sp", g2p[:], mod[5 * D:6 * D].partition_broadcast(128), [], ["g2p"])
        for i in range(4):
            b.dma("sp", lnt[:, i, :], lnp[i, :].partition_broadcast(128), [], ["lnp"] if i == 0 else [("lnp", i)])
        S.op("sp", lambda e: None, reads=[("lnp", 1), ("lnp", 2), ("lnp", 3)], writes=["lnp"])
        for t_, k_ in ((g1p, "g1p"), (sc2p, "sc2p"), (g2p, "g2p")):
            b.ts("dve", t_[:], t_[:], 1.0, None, ALU.add, None, [k_], [k_])

        esL = ExitStack()
        L_all = b.sb(esL, "L_all", [128, NT, 36], F32)
        with ExitStack() as es:
            wo = b.sb(es, "wo", [128, 8, D], BF16)
            wrt = b.sb(es, "wrt", [128, 8, 36], BF16)
            brt = b.sb(es, "brt", [128, 36], F32)
            xt = [b.sb(es, f"xt{i}", [128, D], F32) for i in range(2)]
            at = [b.sb(es, f"at{i}", [128, D], BF16) for i in range(2)]
            attT = [b.sb(es, f"attT{i}", [128, D], BF16) for i in range(2)]
            tmpA = b.sb(es, "tmpA", [128, D], F32)
            yA = b.sb(es, "yA", [128, D], F32)
            x1 = [b.sb(es, f"x1{i}", [128, D], F32) for i in range(2)]
            u2f = b.sb(es, "u2f", [128, D], F32)
            u2b = [b.sb(es, f"u2b{i}", [128, D], BF16) for i in range(2)]
            u2T = [b.sb(es, f"u2T{i}", [128, D], BF16) for i in range(2)]
            pT = [b.ps(es, f"pT{i}", [128, D], BF16) for i in range(2)]
            pmix = [[b.ps(es, f"pmix{i}{c}", [128, 512], F32) for c in range(2)] for i in range(2)]
            plog = b.ps(es, "plog", [128, 36], F32)

            b.dma("pool", wo[:], w_out.rearrange("(kc p) n -> p kc n", p=128), [], ["wo"])
            b.dma("pool", wrt[:], wr.rearrange("(kc p) n -> p kc n", p=128), [], ["wrt"])
            b.dma("sp", brt[:], br.partition_broadcast(128), [], ["brt"])

            def stage1(t):
                p = t % 2
                rows = slice(t * 128, (t + 1) * 128)
                b.dma("sp", xt[p][:], xs[rows, :], [], [("xt", p)])
                if att_gather is None:
                    b.dma("sp", at[p][:], att[rows, :], [], [("at", p)])
                else:
                    G_, gidx_ = att_gather
                    for r_ in range(2):
                        b.gather(at[p][:, r_ * 512:(r_ + 1) * 512], G_[:, :], gidx_[:, r_, t:t + 1], 2 * S_LEN - 1,
                                 ["gidx"] + ([("at", p)] if r_ == 0 else [("at", p, 0)]), [("at", p, 0)] if r_ == 0 else [("at", p)])
                for kc in range(8):
                    b.tr(pT[0][:, kc * 128:(kc + 1) * 128], at[p][:, kc * 128:(kc + 1) * 128], ident[:], [("at", p), "ident"], [("pT", 0)])
                b.copy("act", attT[p][:], pT[0][:], [("pT", 0)], [("attT", p)])
                for c in range(2):
                    for kc in range(8):
                        b.mm(pmix[p][c][:], attT[p][:, kc * 128:(kc + 1) * 128], wo[:, kc, c * 512:(c + 1) * 512],
                             kc == 0, kc == 7, [("attT", p), "wo"], [("pmix", p, c)])
                for c in range(2):
                    b.tt("dve", tmpA[:, c * 512:(c + 1) * 512], pmix[p][c][:], g1p[:, c * 512:(c + 1) * 512], ALU.mult,
                         [("pmix", p, c), "g1p"], [("tmpA", c)])
                b.stt("dve", yA[:], xt[p][:], ALPHA, tmpA[:], ALU.mult, ALU.add, [("xt", p), ("tmpA", 0), ("tmpA", 1)], ["yA"])
                b.layernorm(lnb, yA[:], x1[p][:], lnt[:, 0, :], lnt[:, 1, :], "yA", ("x1", p), "A")
                b.dma("sp", X1[rows, :], x1[p][:], [("x1", p)], [("X1", t)])
                b.tt("pool", u2f[:], x1[p][:], sc2p[:], ALU.mult, [("x1", p), "sc2p"], ["u2f"])
                b.tt("pool", u2b[p][:], u2f[:], sh2[:], ALU.add, ["u2f", "sh2"], [("u2b", p)])
                b.dma("sp", U[rows, :], u2b[p][:], [("u2b", p)], [("U", t)])

            def stage2(t):
                p = t % 2
                for kc in range(8):
                    b.tr(pT[1][:, kc * 128:(kc + 1) * 128], u2b[p][:, kc * 128:(kc + 1) * 128], ident[:], [("u2b", p), "ident"], [("pT", 1)])
                b.copy("act", u2T[p][:], pT[1][:], [("pT", 1)], [("u2T", p)])
                for kc in range(8):
                    b.mm(plog[:], u2T[p][:, kc * 128:(kc + 1) * 128], wrt[:, kc, :], kc == 0, kc == 7, [("u2T", p), "wrt"], ["plog"])
                b.tt("dve", L_all[:, t, :], plog[:], brt[:], ALU.add, ["plog", "brt"], ["L_all"])


            stage1(0)
            for t in range(NT):
                if t + 1 < NT:
                    stage1(t + 1)
                stage2(t)
        S.barrier()
        if True:
            with ExitStack() as esb:
                def t3(name, last, dt=F32):
                    return b.sb(esb, name, [128, NT, last] if last else [128, NT], dt)
                gmax = t3("gmax", 0)
                ohg = t3("ohg", 4)
                eg = t3("eg", 4)
                sume = t3("sume", 0)
                pgrp = t3("pgrp", 0)
                pen = t3("pen", 4)
                Lm = t3("Lm", 32)
                m1 = t3("m1", 0)
                mask1 = t3("mask1", 32)
                Lm2 = t3("Lm2", 32)
                m2 = t3("m2", 0)
                mask2 = t3("mask2", 32)
                dd = t3("dd", 0)
                s1 = t3("s1", 0)
                s2 = t3("s2", 0)
                A_bf = t3("A_bf", 32, BF16)
                rank = t3("rank", 32)
                tmp3 = t3("tmp3", 32)
                eoff = b.sb(esb, "eoff", [128, NE], F32)
                r1 = t3("r1", 0)
                e1 = t3("e1", 0)
                ov = t3("ov", 0)
                df = t3("df", 0)
                pR = [b.ps(esb, f"pR{i}", [128, 16, 32], F32) for i in range(2)]

                b.dma("sp", eoff[:], eoff_d.partition_broadcast(128), [], ["eoff"])
                Lg = L_all[:, :, 0:4]
                Le = L_all[:, :, 4:36]
                bc4 = lambda a: a.unsqueeze(2).to_broadcast([128, NT, 4])
                bc32 = lambda a: a.unsqueeze(2).to_broadcast([128, NT, 32])
                b.red("dve", gmax[:], Lg, "max", ["L_all"], ["gmax"])
                b.tt("dve", ohg[:], Lg, bc4(gmax[:]), ALU.is_equal, ["L_all", "gmax"], ["ohg"])
                b.tt("dve", eg[:], Lg, bc4(gmax[:]), ALU.subtract, ["L_all", "gmax"], ["eg"])
                b.act(eg[:], eg[:], AF.Exp, ["eg"], ["eg"])
                b.red("dve", sume[:], eg[:], "sum", ["eg"], ["sume"])
                S.op("dve", lambda e: e.reciprocal(out=pgrp[:], in_=sume[:]), ["sume"], ["pgrp"])
                b.ts("dve", pen[:], ohg[:], BIG, -BIG, ALU.mult, ALU.add, ["ohg"], ["pen"])
                b.tt("dve", Lm[:].rearrange("p t (g e) -> p t g e", g=4), Le.rearrange("p t (g e) -> p t g e", g=4),
                     pen[:].unsqueeze(3).to_broadcast([128, NT, 4, 8]), ALU.add, ["L_all", "pen"], ["Lm"])
                b.red("dve", m1[:], Lm[:], "max", ["Lm"], ["m1"])
                b.tt("dve", mask1[:], Lm[:], bc32(m1[:]), ALU.is_equal, ["Lm", "m1"], ["mask1"])
                b.stt("dve", Lm2[:], mask1[:], -BIG, Lm[:], ALU.mult, ALU.add, ["mask1", "Lm"], ["Lm2"])
                b.red("dve", m2[:], Lm2[:], "max", ["Lm2"], ["m2"])
                b.tt("dve", mask2[:], Lm2[:], bc32(m2[:]), ALU.is_equal, ["Lm2", "m2"], ["mask2"])
                b.tt("dve", dd[:], m2[:], m1[:], ALU.subtract, ["m1", "m2"], ["dd"])
                b.act(dd[:], dd[:], AF.Exp, ["dd"], ["dd"])
                b.ts("dve", dd[:], dd[:], 1.0, None, ALU.add, None, ["dd"], ["dd"])
                S.op("dve", lambda e: e.reciprocal(out=s1[:], in_=dd[:]), ["dd"], ["s1"])
                b.ts("dve", s2[:], s1[:], -1.0, 1.0, ALU.mult, ALU.add, ["s1"], ["s2"])
                b.tt("dve", gate1[:], s1[:], pgrp[:], ALU.mult, ["s1", "pgrp"], ["gate1"])
                b.tt("dve", gate2[:], s2[:], pgrp[:], ALU.mult, ["s2", "pgrp"], ["gate2"])
                b.tt("dve", A_bf[:], mask1[:], mask2[:], ALU.add, ["mask1", "mask2"], ["A_bf"])
                for t in range(NT):
                    reg = pR[t // 16][:, t % 16, :]
                    for tp in range(t):
                        b.mm(reg, ones[:], A_bf[:, tp, :], tp == 0, False, ["ones", "A_bf"], [("pR", t)])
                    b.mm(reg, tri[:], A_bf[:, t, :], t == 0, True, ["tri", "A_bf"], [("pR", t)])
                for h in range(2):
                    b.copy("dve", rank[:, h * 16:(h + 1) * 16, :], pR[h][:], [("pR", t) for t in range(h * 16, (h + 1) * 16)], [("rank", h)])
                rk = [("rank", 0), ("rank", 1)]
                for (mk, mkk, dst, gate, gk) in ((mask1, "mask1", dest1, gate1, "gate1"), (mask2, "mask2", dest2, gate2, "gate2")):
                    b.tt("dve", tmp3[:], mk[:], rank[:], ALU.mult, [mkk] + rk, ["tmp3"])
                    b.red("dve", r1[:], tmp3[:], "sum", ["tmp3"], ["r1"])
                    b.tt("dve", tmp3[:], mk[:], eoff[:].unsqueeze(1).to_broadcast([128, NT, 32]), ALU.mult, [mkk, "eoff"], ["tmp3"])
                    b.red("dve", e1[:], tmp3[:], "sum", ["tmp3"], ["e1"])
                    b.ts("dve", ov[:], r1[:], float(CAP), None, ALU.is_ge, None, ["r1"], ["ov"])
                    b.tt("dve", df[:], r1[:], e1[:], ALU.add, ["r1", "e1"], ["df"])
                    b.stt("dve", df[:], ov[:], 1.0e6, df[:], ALU.mult, ALU.add, ["ov", "df"], ["df"])
                    b.copy("dve", dst[:], df[:], ["df"], [gk + "d"])
                    b.ts("dve", ov[:], ov[:], -1.0, 1.0, ALU.mult, ALU.add, ["ov"], ["ov"])
                    b.tt("dve", gate[:], gate[:], ov[:], ALU.mult, [gk, "ov"], [gk])
        S.barrier()
        esL.close()

        with ExitStack() as es:
            ut = [b.sb(es, f"ut{i}", [128, D], BF16) for i in range(4)]
            for t in range(NT):
                p = t % 4
                b.dma("sp", ut[p][:], U[t * 128:(t + 1) * 128, :], [("U", t)], [("ut", p)])
                b.scatter(US[:, :], dest1[:, t:t + 1], ut[p][:, :], NE * CAP - 1, [("ut", p), "gate1d"], ["US"])
                b.scatter(US[:, :], dest2[:, t:t + 1], ut[p][:, :], NE * CAP - 1, [("ut", p), "gate2d"], ["US"])
        S.barrier()

        NJ = CAP // 128
        with ExitStack() as es:
            w1e = [b.sb(es, f"w1e{i}", [128, 8, 512], BF16) for i in range(2)]
            w3e = [b.sb(es, f"w3e{i}", [128, 8, 512], BF16) for i in range(2)]
            w2e = [b.sb(es, f"w2e{i}", [128, 4, D], BF16) for i in range(2)]
            us = [b.sb(es, f"us{i}", [128, D], BF16) for i in range(4)]
            uT = [b.sb(es, f"uT{i}", [128, 8, CAP], BF16) for i in range(2)]
            sil = [b.sb(es, f"sil{i}", [128, CAP], F32) for i in range(2)]
            hT = [b.sb(es, f"hT{i}", [128, 4, CAP], BF16) for i in range(2)]
            ysb = [b.sb(es, f"ysb{i}", [128, D], F32) for i in range(2)]
            pT = [b.ps(es, f"pTe{i}", [128, 8, 128], BF16) for i in range(2)]
            pa = [b.ps(es, f"pa{i}", [128, CAP], F32) for i in range(2)]
            pb = [b.ps(es, f"pb{i}", [128, CAP], F32) for i in range(2)]
            py = [b.ps(es, f"py{i}", [128, 512], F32) for i in range(2)]

            def load_w(e):
                q = e % 2
                b.dma("pool", w1e[q][:], w1[e].rearrange("(kc p) f -> p kc f", p=128), [], [("w1e", q)])
                b.dma("pool", w3e[q][:], w3[e].rearrange("(kc p) f -> p kc f", p=128), [], [("w3e", q)])
                b.dma("pool", w2e[q][:], w2[e].rearrange("(fc p) d -> p fc d", p=128), [], [("w2e", q)])

            load_w(0)
            nus = 0
            npy = 0
            for e in range(NE):
                q = e % 2
                if e + 1 < NE:
                    load_w(e + 1)
                for j in range(NJ):
                    ui = nus % 4
                    pi = nus % 2
                    nus += 1
                    r0 = e * CAP + j * 128
                    b.dma("sp", us[ui][:], US[r0:r0 + 128, :], ["US"], [("us", ui)])
                    for kc in range(8):
                        b.tr(pT[pi][:, kc, :], us[ui][:, kc * 128:(kc + 1) * 128], ident[:], [("us", ui), "ident"], [("pTe", pi)])
                    b.copy("act" if j % 2 == 0 else "dve", uT[q][:, :, j * 128:(j + 1) * 128], pT[pi][:], [("pTe", pi)], [("uT", q, j)])
                uTk = [("uT", q, j) for j in range(NJ)]
                for f in range(4):
                    fi = f % 2
                    for kc in range(8):
                        b.mm(pa[fi][:], w1e[q][:, kc, f * 128:(f + 1) * 128], uT[q][:, kc, :], kc == 0, kc == 7, uTk + [("w1e", q)], [("pa", fi)])
                    for kc in range(8):
                        b.mm(pb[fi][:], w3e[q][:, kc, f * 128:(f + 1) * 128], uT[q][:, kc, :], kc == 0, kc == 7, uTk + [("w3e", q)], [("pb", fi)])
                    if b.no_act_copy:
                        b.copy("dve", sil[fi][:], pa[fi][:], [("pa", fi)], [("sil", fi)])
                        b.act(sil[fi][:], sil[fi][:], AF.Silu, [("sil", fi)], [("sil", fi)])
                    else:
                        b.act(sil[fi][:], pa[fi][:], AF.Silu, [("pa", fi)], [("sil", fi)])
                    b.tt("dve", hT[q][:, f, :], sil[fi][:], pb[fi][:], ALU.mult, [("sil", fi), ("pb", fi)], [("hT", q, f)])
                hk = [("hT", q, f) for f in range(4)]
                for j in range(NJ):
                    yi = j % 2
                    for c in range(2):
                        pi = npy % 2
                        npy += 1
                        for f in range(4):
                            b.mm(py[pi][:], hT[q][:, f, j * 128:(j + 1) * 128], w2e[q][:, f, c * 512:(c + 1) * 512], f == 0, f == 3,
                                 hk + [("w2e", q)], [("py", pi)])
                        b.copy("act" if c == 0 else "dve", ysb[yi][:, c * 512:(c + 1) * 512], py[pi][:], [("py", pi)], [("ysb", yi, c)])
                    r0 = e * CAP + j * 128
                    b.dma("sp", YS[r0:r0 + 128, :], ysb[yi][:], [("ysb", yi, 0), ("ysb", yi, 1)], ["YS"])
        S.barrier()

        with ExitStack() as es:
            y1 = [b.sb(es, f"y1{i}", [128, D], F32) for i in range(2)]
            y2 = [b.sb(es, f"y2{i}", [128, D], F32) for i in range(2)]
            x1t = [b.sb(es, f"x1t{i}", [128, D], F32) for i in range(2)]
            fF = b.sb(es, "fF", [128, D], F32)
            zF = b.sb(es, "zF", [128, D], F32)
            oF = [b.sb(es, f"oF{i}", [128, D], F32) for i in range(2)]
            for i in range(2):
                b.memset("dve", y1[i][:], 0.0, [("y1", i)])
                b.memset("dve", y2[i][:], 0.0, [("y2", i)])
            for t in range(NT):
                p = t % 2
                rows = slice(t * 128, (t + 1) * 128)
                b.gather(y1[p][:, :], YS[:, :], dest1[:, t:t + 1], NE * CAP - 1, ["YS", "gate1d", ("y1", p)], [("y1", p)])
                b.gather(y2[p][:, :], YS[:, :], dest2[:, t:t + 1], NE * CAP - 1, ["YS", "gate2d", ("y2", p)], [("y2", p)])
                b.dma("sp", x1t[p][:], X1[rows, :], [("X1", t)], [("x1t", p)])
                b.ts("dve", fF[:], y1[p][:], gate1[:, t:t + 1], None, ALU.mult, None, [("y1", p), "gate1"], ["fF"])
                b.stt("dve", fF[:], y2[p][:], gate2[:, t:t + 1], fF[:], ALU.mult, ALU.add, [("y2", p), "gate2", "fF"], ["fF"])
                b.tt("pool", fF[:], fF[:], g2p[:], ALU.mult, ["fF", "g2p"], ["fF"])
                b.stt("dve", zF[:], x1t[p][:], ALPHA, fF[:], ALU.mult, ALU.add, [("x1t", p), "fF"], ["zF"])
                b.layernorm(lnb, zF[:], oF[p][:], lnt[:, 2, :], lnt[:, 3, :], "zF", ("oF", p), "F")
                b.dma("sp", xo[rows, :], oF[p][:], [("oF", p)], [("xo", t)])
        outs = [("xo", t) for t in range(NT)]
        if b.fused:
            S.op("sp", lambda e: None, reads=outs)
            S.barrier()
            return outs
        return b.finish(outs)


def post_consts():
    tri = np.triu(np.ones((128, 128), np.float32), 1)
    return dict(ident=np.eye(128, dtype=np.float32), tri=tri,
                eoff=(np.arange(NE) * CAP).astype(np.float32))


def emit_mod(b, es, c_row, w_ada, b_ada, mod_out, mod_scr=None, ps_name="pmod"):
    S = b.S
    with ExitStack() as esm:
        cf = b.sb(esm, "cf", [128, 8], F32)
        sg = b.sb(esm, "sg", [128, 8], F32)
        cb = b.sb(esm, "cb", [128, 8], BF16)
        wa = [b.sb(esm, f"wa{i}", [128, 8, 512], BF16) for i in range(2)]
        bad = b.sb(esm, "bad", [1, 6 * D], F32)
        modr = b.sb(esm, "modr", [1, 6 * D], F32)
        pm = [b.ps(esm, f"{ps_name}{i}", [1, 512], F32) for i in range(2)]
        b.dma("sp", cf[:], c_row.rearrange("(p kc) -> p kc", kc=8), [], ["cf"])
        b.dma("sp", bad[:], b_ada.rearrange("(o n) -> o n", o=1), [], ["bad"])
        b.act(sg[:], cf[:], AF.Exp, ["cf"], ["sg"], scale=-1.0)
        b.ts("dve", sg[:], sg[:], 1.0, None, ALU.add, None, ["sg"], ["sg"])
        S.op("dve", lambda e: e.reciprocal(out=sg[:], in_=sg[:]), ["sg"], ["sg"])
        b.tt("dve", cb[:], cf[:], sg[:], ALU.mult, ["cf", "sg"], ["cb"])
        for g in range(12):
            q = g % 2
            b.dma("pool", wa[q][:], w_ada[:, g * 512:(g + 1) * 512].rearrange("(p kc) n -> p kc n", kc=8), [], [("wa", q)])
            for kc in range(8):
                b.mm(pm[q][:], cb[:, kc:kc + 1], wa[q][:, kc, :], kc == 0, kc == 7, ["cb", ("wa", q)], [("pm", q)])
            b.tt("dve", modr[:, g * 512:(g + 1) * 512], pm[q][:], bad[:, g * 512:(g + 1) * 512], ALU.add, [("pm", q), "bad"], ["modr"])
        b.dma("sp", mod_out.rearrange("(o n) -> o n", o=1), modr[:], ["modr"], ["mod_d"])
        if mod_scr is not None:
            b.dma("sp", mod_scr.rearrange("(o n) -> o n", o=1), modr[:], ["modr"], ["mod_s"])
    S.barrier()


NG = S_LEN // 512
NB = S_LEN // 128


def emit_uT_group(b, g, x, sc1p, sh1, ident, xt, uf, ub, pT, uT, ubuf=0):
    for sub in range(4):
        r0 = g * 512 + sub * 128
        p = sub % 2
        b.dma("sp", xt[p][:], x[r0:r0 + 128, :], [], [("xt", p)])
        b.tt("dve", uf[p][:], xt[p][:], sc1p[:], ALU.mult, [("xt", p), "sc1p"], [("uf", p)])
        b.tt("pool", ub[p][:], uf[p][:], sh1[:], ALU.add, [("uf", p), "sh1"], [("ub", p)])
        for kc in range(8):
            b.tr(pT[p][:, kc, :], ub[p][:, kc * 128:(kc + 1) * 128], ident[:], [("ub", p), "ident"], [("pT", p)])
        b.copy("act" if sub % 2 == 0 else "dve", uT[:, :, sub * 128:(sub + 1) * 128], pT[p][:], [("pT", p)], [("uT", ubuf, sub)])


def build_attn0(phases=(1, 2, 3), dbg=False, ng=NG, skip=(), nq=NG, npr=2, b=None):
    b = b or B()
    if dbg:
        b.dscr = b.dout
    b.no_act_copy = True
    S = b.S
    x = b.din("x", [S_LEN, D], F32)
    c_row = b.din("c_row", [D], F32)
    w_ada = b.din("w_ada", [D, 6 * D], F32)
    b_ada = b.din("b_ada", [6 * D], F32)
    w_in = b.din("w_in", [D, 2048], F32)
    gn_g = b.din("gn_g", [256], F32)
    ident_d = b.din("ident", [128, 128], F32)
    triI_d = b.din("triI", [128, 128], F32)
    maskd_d = b.din("maskd", [4, 128, 512], F32)
    dm_d = b.din("dm", [2, 128, 128], F32)
    pat_d = b.din("pat", [4, 128], F32)
    kdec_d = b.din("kdec", [128, 4], F32)
    g64_d = b.din("g64", [128, 2], F32)
    att = b.dout("att", [S_LEN, 512], BF16)
    mod_o = b.dout("mod", [6 * D], F32)
    QS = b.dscr("QS", [2, 128, S_LEN], BF16)
    KS = b.dscr("KS", [2, 128, S_LEN], BF16)
    VS = b.dscr("VS", [S_LEN, 256], BF16)
    RQ = b.dscr("RQ", [2, 3, 128, S_LEN], BF16)
    RK = b.dscr("RK", [2, 128, S_LEN], BF16)
    RKd = b.dscr("RKd", [S_LEN, 512], BF16)
    RV = b.dscr("RV", [S_LEN, 256], BF16)
    RG = b.dscr("RG", [S_LEN, 256], BF16)

    with ExitStack() as es0:
        ident = b.sb(es0, "ident", [128, 128], BF16)
        b.dma("pool", ident[:], ident_d, [], ["ident"])
        modS = b.dscr("modS", [6 * D], F32)
        b.shared["modS"] = modS
        b.shared["att"] = att
        emit_mod(b, es0, c_row, w_ada, b_ada, mod_o, modS)

        with ExitStack() as es:
            if 1 not in phases:
                return b.finish(["mod_d"])
            sc1p = b.sb(es, "sc1p", [128, D], F32)
            sh1 = b.sb(es, "sh1", [128, D], F32)
            wcs = [b.sb(es, f"wc{h}", [128, 8, 512], BF16) for h in range(4)]

            class _WC:
                def __getitem__(self, key):
                    p_, kc_, cs_ = key
                    h_ = cs_.start // 512
                    return wcs[h_][p_, kc_, cs_.start - h_ * 512:cs_.stop - h_ * 512]
            wc = _WC()
            pat = b.sb(es, "pat", [128, 4, 128], F32)
            kdec = b.sb(es, "kdec", [128, 4], F32)
            xt = [b.sb(es, f"xt{i}", [128, D], F32) for i in range(2)]
            uf = [b.sb(es, f"uf{i}", [128, D], F32) for i in range(2)]
            ub = [b.sb(es, f"ub{i}", [128, D], BF16) for i in range(2)]
            uTs = [b.sb(es, f"uT{i}", [128, 8, 512], BF16) for i in range(2)]
            fst = [b.sb(es, f"fst{i}", [128, 512], BF16) for i in range(4)]
            gtmp = b.sb(es, "gtmp", [128, 256], F32)
            tst = [b.sb(es, f"tst{i}", [128, 4, 1280], BF16) for i in range(2)]
            pT = [b.ps(es, f"pT{i}", [128, 8, 128], BF16) for i in range(2)]
            pf = [b.ps(es, f"pf{i}", [128, 512], F32) for i in range(2)]
            pt = [b.ps(es, f"pt{i}", [128, 512], F32) for i in range(3)]

            b.dma("sp", sh1[:], modS[0:D].partition_broadcast(128), ["mod_s"], ["sh1"])
            b.dma("sp", sc1p[:], modS[D:2 * D].partition_broadcast(128), ["mod_s"], ["sc1p"])
            b.ts("dve", sc1p[:], sc1p[:], 1.0, None, ALU.add, None, ["sc1p"], ["sc1p"])
            for h in range(4):
                b.dma("pool", wcs[h][:], w_in[:, h * 512:(h + 1) * 512].rearrange("(kc p) n -> p kc n", p=128), [], ["wc"] if h == 0 else [("wc", h)])
            S.op("sp", lambda e: None, reads=[("wc", 1), ("wc", 2), ("wc", 3)], writes=["wc"])
            for i in range(4):
                b.dma("sp", pat[:, i, :], pat_d[i, :].partition_broadcast(128), [], ["pat"] if i == 0 else [("pat", i)])
            S.op("sp", lambda e: None, reads=[("pat", 1), ("pat", 2), ("pat", 3)], writes=["pat"])
            b.dma("sp", kdec[:], kdec_d, [], ["kdec"])

            nf = 0
            emit_uT_group(b, 0, x, sc1p, sh1, ident, xt, uf, ub, pT, uTs[0], 0)
            for g in range(ng):
                cols = slice(g * 512, (g + 1) * 512)
                if g + 1 < ng:
                    emit_uT_group(b, g + 1, x, sc1p, sh1, ident, xt, uf, ub, pT, uTs[(g + 1) % 2], (g + 1) % 2)
                uT = uTs[g % 2]
                uTk = [("uT", g % 2, s_) for s_ in range(4)]
                for fm in range(8):
                    if "fm" in skip:
                        break
                    pi = fm % 2
                    for kc in range(8):
                        b.mm(pf[pi][:], wc[:, kc, fm * 128:(fm + 1) * 128], uT[:, kc, :], kc == 0, kc == 7, uTk + ["wc"], [("pf", pi)])
                    if "ev" in skip:
                        continue
                    if fm < 4 or fm >= 6:
                        si = nf % 4
                        nf += 1
                        b.copy("act", fst[si][:], pf[pi][:], [("pf", pi)], [("fst", si)])
                        dst = (QS[fm, :, cols] if fm < 2 else KS[fm - 2, :, cols]) if fm < 4 else RK[fm - 6, :, cols]
                        if "st" not in skip:
                            b.dma("sp", dst, fst[si][:], [("fst", si)], [("FM", fm, g)])
                    else:
                        hd = fm - 4
                        for ver in range(3):
                            si = nf % 4
                            nf += 1
                            if ver == 0:
                                b.copy("act", fst[si][:], pf[pi][:], [("pf", pi)], [("fst", si)])
                            else:
                                b.tt("dve", fst[si][:].rearrange("p (a t) -> p a t", a=4), pf[pi][:].rearrange("p (a t) -> p a t", a=4),
                                     pat[:, hd * 2 + ver - 1, :].unsqueeze(1).to_broadcast([128, 4, 128]), ALU.mult,
                                     [("pf", pi), "pat"], [("fst", si)])
                            if "st" not in skip:
                                b.dma("sp", RQ[hd, ver, :, cols], fst[si][:], [("fst", si)], [("RQ", hd, ver, g)])
                ti = g % 2
                if "tm" in skip:
                    continue
                for sub in range(4):
                    for half in range(2):
                        pi = (sub * 2 + half) % 3
                        for kc in range(8):
                            b.mm(pt[pi][:], uT[:, kc, sub * 128:(sub + 1) * 128], wc[:, kc, 1024 + half * 512:1024 + (half + 1) * 512],
                                 kc == 0, kc == 7, uTk + ["wc"], [("pt", pi)])
                        wk = ("tst", ti, sub, half)
                        if half == 0:
                            b.copy("act", tst[ti][:, sub, 0:256], pt[pi][:, 0:256], [("pt", pi)], [wk])
                            for hd in range(2):
                                for par in range(2):
                                    o0 = 256 + (hd * 2 + par) * 128
                                    b.ts("dve", tst[ti][:, sub, o0:o0 + 128], pt[pi][:, 256 + hd * 128:256 + (hd + 1) * 128],
                                         kdec[:, hd * 2 + par:hd * 2 + par + 1], None, ALU.mult, None, [("pt", pi), "kdec"], [wk + (hd, par)])
                        else:
                            b.copy("dve", tst[ti][:, sub, 768:1024], pt[pi][:, 0:256], [("pt", pi)], [wk])
                            b.copy("dve", gtmp[:], pt[pi][:, 256:512], [("pt", pi)], ["gtmp"])
                            b.act(tst[ti][:, sub, 1024:1280], gtmp[:], AF.Silu, ["gtmp"], [wk + ("g",)])
                rk_ = [("tst", ti, sub, half) for sub in range(4) for half in range(2)] + \
                      [("tst", ti, sub, 0, hd, par) for sub in range(4) for hd in range(2) for par in range(2)] + \
                      [("tst", ti, sub, 1, "g") for sub in range(4)]
                rows = slice(g * 512, (g + 1) * 512)
                for (dst, c0, c1, nm) in ((VS, 0, 256, "VS"), (RKd, 256, 768, "RKd"), (RV, 768, 1024, "RV"), (RG, 1024, 1280, "RG")):
                    b.dma("sp", dst[rows, :].rearrange("(s p) c -> p s c", p=128), tst[ti][:, :, c0:c1], rk_, [(nm, g)])
        S.barrier()

        with ExitStack() as es:
            if 2 not in phases:
                npr = 0
            QT = b.sb(es, "QT", [128, 2, S_LEN], BF16)
            KT = b.sb(es, "KT", [128, 2, S_LEN], BF16)
            Vt = b.sb(es, "Vt", [128, NB, 256], BF16)
            triI = b.sb(es, "triI", [128, 128], BF16)
            onesr = b.sb(es, "onesr", [1, 128], BF16)
            maskd = b.sb(es, "maskd", [128, 4, 512], BF16)
            NS = 4
            E_ = [b.sb(es, f"E{i}", [128, 512], F32) for i in range(6)]
            SP = [b.sb(es, f"SP{i}", [128, 512], BF16) for i in range(6)]
            X_ = [b.sb(es, f"X{i}", [128, 512], F32) for i in range(NS)]
            C_ = [b.sb(es, f"C{i}", [128, 512], F32) for i in range(NS)]
            Z_ = [b.sb(es, f"Z{i}", [128, 512], F32) for i in range(NS)]
            W_ = [b.sb(es, f"W{i}", [128, 512], BF16) for i in range(NS)]
            car = [b.sb(es, f"car{i}", [1, 512], BF16) for i in range(2)]
            ost = [b.sb(es, f"ost{i}", [128, 4, 64], BF16) for i in range(2)]
            pz = [b.ps(es, f"pz{i}", [128, 512], F32) for i in range(NS)]
            pc = [b.ps(es, f"pc{i}", [128, 512], F32) for i in range(2)]
            poT = [b.ps(es, f"poT{i}", [64, 512], F32) for i in range(2)]
            OTs = [b.sb(es, f"OTs{i}", [64, 512], F32) for i in range(2)]
            identf = b.sb(es, "identf", [128, 128], F32)
            b.dma("sp", identf[:], ident_d, [], ["identf"])

            nqc = nq * 512
            for pr in range(2):
                b.dma("sp", QT[:, pr, 0:nqc], QS[pr, :, 0:nqc], [], [("QT", pr)])
                b.dma("sp", KT[:, pr, 0:nqc], KS[pr, :, 0:nqc], [], [("KT", pr)])
            for i8 in range(8):
                if i8 * 1024 >= nqc:
                    b.memset("dve", Vt[0:1, i8 * 8, 0:1], 0.0, [("Vt", i8)] if i8 else ["Vt"])
                    continue
                b.dma("sp", Vt[:, i8 * 8:(i8 + 1) * 8, :], VS[i8 * 1024:(i8 + 1) * 1024, :].rearrange("(n p) c -> p n c", p=128), [], ["Vt"] if i8 == 0 else [("Vt", i8)])
            S.op("sp", lambda e: None, reads=[("Vt", i8) for i8 in range(1, 8)], writes=["Vt"])
            b.dma("pool", triI[:], triI_d, [], ["triI"])
            b.dma("pool", maskd[:], maskd_d.rearrange("j p t -> p j t"), [], ["maskd"])
            b.memset("dve", onesr[:], 1.0, ["onesr"])
            zer = b.sb(es, "zer", [128, 256], BF16)
            b.memset("dve", zer[:], 0.0, ["zer"])

            for pr in range(npr):
                for qi in range(nq):
                    t0 = qi * 512
                    nkb = 4 * qi + 4
                    def stage_a(step):
                        kb = nkb - 1 - step
                        j = kb - 4 * qi
                        par = step % 2
                        for hp in range(2):
                            s = hp * 2 + par
                            ps_ = slice(hp * 64, (hp + 1) * 64)
                            b.mm(pz[s][:], KT[ps_, pr, kb * 128:(kb + 1) * 128], QT[ps_, pr, t0:t0 + 512], True, True,
                                 [("KT", pr), ("QT", pr)], [("pz", s)])
                        for hp in range(2):
                            s = hp * 2 + par
                            b.copy("dve", Z_[s][:], pz[s][:], [("pz", s)], [("Z", s)])
                        for hp in range(2):
                            s, s3 = hp * 2 + par, hp * 3 + step % 3
                            b.act(E_[s3][:], Z_[s][:], AF.Exp, [("Z", s)], [("E", s3)], scale=0.125)
                        for hp in range(2):
                            s3 = hp * 3 + step % 3
                            b.act(SP[s3][:], E_[s3][:], AF.Ln, [("E", s3)], [("SP", s3)], bias=1.0)
                        if j >= 0:
                            for hp in range(2):
                                s3 = hp * 3 + step % 3
                                b.tt("pool", SP[s3][:], SP[s3][:], maskd[:, j, :], ALU.mult, [("SP", s3), "maskd"], [("SP", s3)])
                                b.tt("pool", E_[s3][:], E_[s3][:], maskd[:, j, :], ALU.mult, [("E", s3), "maskd"], [("E", s3)])

                    def stage_b1(step):
                        par = step % 2
                        for hp in range(2):
                            s3 = hp * 3 + step % 3
                            b.mm(pc[hp][:], triI[:], SP[s3][:], True, step == 0, ["triI", ("SP", s3)], [("pc", hp)])
                            if step > 0:
                                b.mm(pc[hp][:], onesr[:], car[hp][:], False, True, ["onesr", ("car", hp)], [("pc", hp)])
                        for hp in range(2):
                            s = hp * 2 + par
                            if step < nkb - 1:
                                b.copy("dve", car[hp][:], pc[hp][0:1, :], [("pc", hp)], [("car", hp)])
                            b.copy("dve", C_[s][:], pc[hp][:], [("pc", hp)], [("C", s)])
                            b.act(X_[s][:], C_[s][:], AF.Exp, [("C", s)], [("X", s)], scale=-1.0)

                    def stage_b2(step):
                        kb = nkb - 1 - step
                        par = step % 2
                        for hp in range(2):
                            s, s3 = hp * 2 + par, hp * 3 + step % 3
                            b.tt("pool", W_[s][:], E_[s3][:], X_[s][:], ALU.mult, [("E", s3), ("X", s)], [("W", s)])
                        for hp in range(2):
                            s = hp * 2 + par
                            h = pr * 2 + hp
                            b.mm(poT[hp][:], Vt[:, kb, h * 64:(h + 1) * 64], W_[s][:], step == 0, step == nkb - 1, [("W", s), "Vt"], [("poT", hp)])

                    stage_a(0)
                    stage_a(1)
                    stage_b1(0)
                    for step in range(nkb):
                        if step + 2 < nkb:
                            stage_a(step + 2)
                        if step + 1 < nkb:
                            stage_b1(step + 1)
                        stage_b2(step)
                    for hp in range(2):
                        s = hp
                        h = pr * 2 + hp
                        b.copy("dve", OTs[hp][:], poT[hp][:], [("poT", hp)], [("OTs", hp)])
                        for sub in range(4):
                            b.tr(pc[hp][:, sub * 64:(sub + 1) * 64], OTs[hp][:, sub * 128:(sub + 1) * 128], identf[0:64, 0:64],
                                 [("OTs", hp), "identf"], [("pc", hp)])
                        b.copy("dve", ost[s][:].rearrange("p a c -> p (a c)"), pc[hp][:, 0:256], [("pc", hp)], [("ost", s)])
                        b.dma("sp", att[t0:t0 + 512, h * 64:(h + 1) * 64].rearrange("(s p) c -> p s c", p=128), ost[s][:],
                              [("ost", s)], [("att_sb", h, qi)])
        S.barrier()

        with ExitStack() as es:
            if 3 not in phases:
                return b.finish(["mod_d"] + [("att_sb", h, qi) for h in range(2 * npr) for qi in range(nq)])
            dm = b.sb(es, "dm", [128, 2, 128], F32)
            g64 = b.sb(es, "g64", [128, 2], F32)
            gng = b.sb(es, "gng", [128, 256], F32)
            epsb = b.sb(es, "epsb", [128, 1], F32)
            qt = [b.sb(es, f"rqt{i}", [128, 3, 512], BF16) for i in range(4)]
            kt = [b.sb(es, f"rkt{i}", [128, 512], BF16) for i in range(4)]
            kd = [b.sb(es, f"rkd{i}", [128, 4, 256], BF16) for i in range(4)]
            vt = [b.sb(es, f"rvt{i}", [128, 4, 128], BF16) for i in range(4)]
            gt = [b.sb(es, f"rgt{i}", [128, 4, 128], BF16) for i in range(4)]
            stf = [b.sb(es, f"stf{i}", [128, 128], F32) for i in range(2)]
            stb = [[b.sb(es, f"stb{i}{k}", [128, 128], BF16) for k in range(2)] for i in range(2)]
            Pm = [b.sb(es, f"Pm{i}", [128, 128], BF16) for i in range(2)]
            of = [b.sb(es, f"of{i}", [128, 128], F32) for i in range(2)]
            st_ = [b.sb(es, f"rst{i}", [128, 6], F32) for i in range(2)]
            mv = [b.sb(es, f"rmv{i}", [128, 2], F32) for i in range(2)]
            rs = [b.sb(es, f"rrs{i}", [128, 1], F32) for i in range(2)]
            gg = [b.sb(es, f"gg{i}", [128, 128], F32) for i in range(2)]
            oo = [b.sb(es, f"oo{i}", [128, 4, 128], BF16) for i in range(4)]
            psc = [b.ps(es, f"psc{i}", [128, 128], F32) for i in range(2)]
            pso = [b.ps(es, f"pso{i}", [128, 128], F32) for i in range(2)]
            pkv = [b.ps(es, f"pkv{i}", [128, 128], F32) for i in range(2)]

            b.dma("sp", dm[:], dm_d.rearrange("h s c -> s h c"), [], ["dm"])
            b.dma("sp", g64[:], g64_d, [], ["g64"])
            b.dma("sp", gng[:], gn_g.partition_broadcast(128), [], ["gng"])
            b.memset("dve", epsb[:], EPS, ["epsb"])
            for hd in range(2):
                b.memset("dve", stf[hd][:], 0.0, [("stf", hd)])
                b.memset("dve", stb[hd][0][:], 0.0, [("stb", hd, 0)])
            nst = [0, 0]
            for g in range(ng):
                cols = slice(g * 512, (g + 1) * 512)
                rows = slice(g * 512, (g + 1) * 512)
                for hd in range(2):
                    bi = hd * 2 + g % 2
                    b.dma("sp", qt[bi][:], RQ[hd, :, :, cols].rearrange("v p t -> p v t"), [], [("rqt", bi)])
                    b.dma("sp", kt[bi][:], RK[hd, :, cols], [], [("rkt", bi)])
                    b.dma("sp", kd[bi][:], RKd[rows, hd * 256:(hd + 1) * 256].rearrange("(s p) c -> p s c", p=128), [], [("rkd", bi)])
                    b.dma("sp", vt[bi][:], RV[rows, hd * 128:(hd + 1) * 128].rearrange("(s p) c -> p s c", p=128), [], [("rvt", bi)])
                    b.dma("sp", gt[bi][:], RG[rows, hd * 128:(hd + 1) * 128].rearrange("(s p) c -> p s c", p=128), [], [("rgt", bi)])
                for sub in range(4):
                    tc_ = slice(sub * 128, (sub + 1) * 128)
                    for hd in range(2):
                        bi = hd * 2 + g % 2
                        s = hd
                        b.mm(psc[s][:], kt[bi][:, tc_], qt[bi][:, 0, tc_], True, True, [("rkt", bi), ("rqt", bi)], [("psc", s)])
                        b.tt("dve", Pm[s][:], psc[s][:], dm[:, hd, :], ALU.mult, [("psc", s), "dm"], [("Pm", s)])
                        k0 = nst[hd] % 2
                        b.mm(pso[s][:], Pm[s][:], vt[bi][:, sub, :], True, False, [("Pm", s), ("rvt", bi)], [("pso", s)])
                        b.mm(pso[s][:], qt[bi][:, 1, tc_], stb[hd][k0][:], False, False, [("rqt", bi), ("stb", hd, k0)], [("pso", s)])
                        b.mm(pkv[s][:], kd[bi][:, sub, 0:128], vt[bi][:, sub, :], True, True, [("rkd", bi), ("rvt", bi)], [("pkv", s)])
                        b.stt("dve", stf[hd][:], stf[hd][:], g64[:, hd:hd + 1], pkv[s][:], ALU.mult, ALU.add, [("stf", hd), "g64", ("pkv", s)], [("stf", hd)])
                        b.copy("act", stb[hd][1 - k0][:], stf[hd][:], [("stf", hd)], [("stb", hd, 1 - k0)])
                        b.mm(pso[s][:], qt[bi][:, 2, tc_], stb[hd][1 - k0][:], False, True, [("rqt", bi), ("stb", hd, 1 - k0)], [("pso", s)])
                        b.mm(pkv[s][:], kd[bi][:, sub, 128:256], vt[bi][:, sub, :], True, True, [("rkd", bi), ("rvt", bi)], [("pkv", s)])
                        b.stt("dve", stf[hd][:], stf[hd][:], g64[:, hd:hd + 1], pkv[s][:], ALU.mult, ALU.add, [("stf", hd), "g64", ("pkv", s)], [("stf", hd)])
                        b.copy("act", stb[hd][k0][:], stf[hd][:], [("stf", hd)], [("stb", hd, k0)])
                        b.copy("act", of[s][:], pso[s][:], [("pso", s)], [("of", s)])
                        S.op("dve", lambda e, s=s: e.bn_stats(out=st_[s][:], in_=of[s][:]), [("of", s)], [("rst", s)])
                        S.op("dve", lambda e, s=s: e.bn_aggr(out=mv[s][:], in_=st_[s][:]), [("rst", s)], [("rmv", s)])
                        b.act(rs[s][:], mv[s][:, 1:2], AF.Ln, [("rmv", s), "epsb"], [("rrs", s)], bias=epsb[:])
                        b.act(rs[s][:], rs[s][:], AF.Exp, [("rrs", s)], [("rrs", s)], scale=-0.5)
                        b.tt("pool", gg[s][:], gt[bi][:, sub, :], gng[:, hd * 128:(hd + 1) * 128], ALU.mult, [("rgt", bi), "gng"], [("gg", s)])
                        b.ts("dve", of[s][:], of[s][:], mv[s][:, 0:1], rs[s][:], ALU.subtract, ALU.mult, [("of", s), ("rmv", s), ("rrs", s)], [("of", s)])
                        b.tt("dve", oo[bi][:, sub, :], of[s][:], gg[s][:], ALU.mult, [("of", s), ("gg", s)], [("oo", bi, sub)])
                for hd in range(2):
                    bi = hd * 2 + g % 2
                    b.dma("sp", att[rows, 256 + hd * 128:256 + (hd + 1) * 128].rearrange("(s p) c -> p s c", p=128), oo[bi][:],
                          [("oo", bi, sub) for sub in range(4)], [("att_r", hd, g)])
        outs = ["mod_d"] + [("att_sb", h, qi) for h in range(2 * npr) for qi in range(nq)] + [("att_r", hd, g) for hd in range(2) for g in range(ng)]
        if b.fused:
            S.barrier()
            return outs
        return b.finish(outs)


def attn0_consts(hh):
    pos = np.arange(128)
    same = (pos[:, None] // 64) == (pos[None, :] // 64)
    dmm = np.zeros((2, 128, 128), np.float32)
    pat = np.zeros((4, 128), np.float32)
    kdec = np.zeros((128, 4), np.float32)
    g64 = np.zeros((128, 2), np.float32)
    for hd in range(2):
        hr = 2 * hh + hd
        lg = np.log1p(-np.exp2(-5.0 - hr))
        dmm[hd] = np.where(same, np.exp(lg * np.abs(pos[:, None] - pos[None, :])), 0.0) * 128 ** -0.5
        for par in range(2):
            inpar = (pos // 64) == par
            pat[hd * 2 + par] = np.where(inpar, np.exp(lg * (pos % 64 + 1.0)), 0.0)
            kdec[:, hd * 2 + par] = np.where(inpar, np.exp(lg * (63 - pos % 64)), 0.0) * 128 ** -0.5
        g64[:, hd] = np.exp(lg * 64)
    s_ = np.arange(128)[:, None]
    t_ = np.arange(512)[None, :]
    maskd = np.stack([(128 * j + s_ < t_) for j in range(4)]).astype(np.float32)
    return dict(ident=np.eye(128, dtype=np.float32), triI=np.tril(np.ones((128, 128), np.float32)),
                maskd=maskd, dm=dmm, pat=pat, kdec=kdec, g64=g64)


LAMBDA_INIT = 0.8 - 0.6 * float(np.exp(-0.3 * 1))
ACT_PSUM = True


def build_attn1(ng=NG, nq=NG, nhb=2, b=None):
    b = b or B()
    b.no_act_copy = True
    S = b.S
    x = b.din("x", [S_LEN, D], F32)
    c_row = b.din("c_row", [D], F32)
    w_ada = b.din("w_ada", [D, 6 * D], F32)
    b_ada = b.din("b_ada", [6 * D], F32)
    w_in = b.din("w_in", [D, 1536], F32)
    lamv = b.din("lamv", [4, 64], F32)
    subg = b.din("subg", [128], F32)
    ident_d = b.din("ident", [128, 128], F32)
    bdiag_d = b.din("bdiag", [4, 4, 128, 512], F32)
    posb_d = b.din("posb", [128, 4, 64], F32)
    rrow_d = b.din("rrow", [4, S_LEN], F32)
    att = b.dout("att", [S_LEN, 512], BF16)
    mod_o = b.dout("mod", [6 * D], F32)
    modS = b.dscr("modS1", [6 * D], F32)
    b.shared["modS"] = modS
    b.shared["att"] = att
    lamS = b.dscr("lamS", [1], F32)
    QD = b.dscr("QD", [4, 128, S_LEN], BF16)
    KD = b.dscr("KD", [4, 128, S_LEN], BF16)
    VD = b.dscr("VD", [S_LEN, 4 * 129], BF16)

    with ExitStack() as es0:
        ident = b.sb(es0, "ident", [128, 128], BF16)
        neglam = b.sb(es0, "neglam", [128, 1], F32)
        gsub = b.sb(es0, "gsub", [128, 128], F32)
        epsb = b.sb(es0, "epsb", [128, 1], F32)
        b.dma("pool", ident[:], ident_d, [], ["ident"])
        b.memset("dve", epsb[:], EPS, ["epsb"])
        emit_mod(b, es0, c_row, w_ada, b_ada, mod_o, modS)
        with ExitStack() as es:
            lv = b.sb(es, "lv", [1, 4, 64], F32)
            lp = b.sb(es, "lp", [1, 2, 64], F32)
            ls = b.sb(es, "ls", [1, 2], F32)
            ll = b.sb(es, "ll", [1, 1], F32)
            b.dma("sp", lv[:], lamv.rearrange("(o a) n -> o a n", o=1), [], ["lv"])
            b.tt("dve", lp[:, 0, :], lv[:, 0, :], lv[:, 1, :], ALU.mult, ["lv"], ["lp0"])
            b.tt("dve", lp[:, 1, :], lv[:, 2, :], lv[:, 3, :], ALU.mult, ["lv"], ["lp1"])
            b.red("dve", ls[:], lp[:], "sum", ["lp0", "lp1"], ["ls"])
            b.act(ls[:], ls[:], AF.Exp, ["ls"], ["ls"])
            b.tt("dve", ll[:], ls[:, 1:2], ls[:, 0:1], ALU.subtract, ["ls"], ["ll"])
            b.ts("dve", ll[:], ll[:], -LAMBDA_INIT, None, ALU.add, None, ["ll"], ["ll"])
            b.dma("sp", lamS.rearrange("(o n) -> o n", o=1), ll[:], ["ll"], ["lamS"])
            b.dma("sp", neglam[:], lamS.partition_broadcast(128), ["lamS"], ["neglam"])
            b.dma("sp", gsub[:], subg.partition_broadcast(128), [], ["gsub"])
            b.ts("dve", gsub[:], gsub[:], 1.0 - LAMBDA_INIT, None, ALU.mult, None, ["gsub"], ["gsub"])
        S.barrier()

        with ExitStack() as es:
            sc1p = b.sb(es, "sc1p", [128, D], F32)
            sh1 = b.sb(es, "sh1", [128, D], F32)
            wcs = [b.sb(es, f"wc{h}", [128, 8, 512], BF16) for h in range(3)]
            xt = [b.sb(es, f"xt{i}", [128, D], F32) for i in range(2)]
            uf = [b.sb(es, f"uf{i}", [128, D], F32) for i in range(2)]
            ub = [b.sb(es, f"ub{i}", [128, D], BF16) for i in range(2)]
            uTs = [b.sb(es, f"uT{i}", [128, 8, 512], BF16) for i in range(2)]
            fst = [b.sb(es, f"fst{i}", [128, 512], BF16) for i in range(4)]
            vst = [b.sb(es, f"vst{i}", [128, 4, 4, 129], BF16) for i in range(2)]
            pT = [b.ps(es, f"pT{i}", [128, 8, 128], BF16) for i in range(2)]
            pf = [b.ps(es, f"pf{i}", [128, 512], F32) for i in range(2)]
            pt = [b.ps(es, f"pt{i}", [128, 512], F32) for i in range(2)]
            b.dma("sp", sh1[:], modS[0:D].partition_broadcast(128), ["mod_s"], ["sh1"])
            b.dma("sp", sc1p[:], modS[D:2 * D].partition_broadcast(128), ["mod_s"], ["sc1p"])
            b.ts("dve", sc1p[:], sc1p[:], 1.0, None, ALU.add, None, ["sc1p"], ["sc1p"])
            for h in range(3):
                b.dma("pool", wcs[h][:], w_in[:, h * 512:(h + 1) * 512].rearrange("(kc p) n -> p kc n", p=128), [], ["wc"] if h == 0 else [("wc", h)])
            S.op("sp", lambda e: None, reads=[("wc", 1), ("wc", 2)], writes=["wc"])
            for i in range(2):
                b.memset("dve", vst[i][:], 1.0, [("vst", i)])
            nf = 0
            emit_uT_group(b, 0, x, sc1p, sh1, ident, xt, uf, ub, pT, uTs[0], 0)
            for g in range(ng):
                cols = slice(g * 512, (g + 1) * 512)
                if g + 1 < ng:
                    emit_uT_group(b, g + 1, x, sc1p, sh1, ident, xt, uf, ub, pT, uTs[(g + 1) % 2], (g + 1) % 2)
                uT = uTs[g % 2]
                uTk = [("uT", g % 2, s_) for s_ in range(4)]
                for fm in range(8):
                    pi = fm % 2
                    wt = wcs[fm // 4]
                    c0 = (fm % 4) * 128
                    for kc in range(8):
                        b.mm(pf[pi][:], wt[:, kc, c0:c0 + 128], uT[:, kc, :], kc == 0, kc == 7, uTk + ["wc"], [("pf", pi)])
                    si = nf % 4
                    nf += 1
                    b.copy("dve", fst[si][:], pf[pi][:], [("pf", pi)], [("fst", si)])
                    dst = QD[fm, :, cols] if fm < 4 else KD[fm - 4, :, cols]
                    b.dma("sp", dst, fst[si][:], [("fst", si)], [("FM", fm, g)])
                ti = g % 2
                for sub in range(4):
                    pi = sub % 2
                    for kc in range(8):
                        b.mm(pt[pi][:], uT[:, kc, sub * 128:(sub + 1) * 128], wcs[2][:, kc, :], kc == 0, kc == 7, uTk + ["wc"], [("pt", pi)])
                    b.copy("dve", vst[ti][:, sub, :, 0:128], pt[pi][:].rearrange("p (h d) -> p h d", h=4), [("pt", pi), ("vst", ti)], [("vst", ti, sub)])
                rows = slice(g * 512, (g + 1) * 512)
                b.dma("sp", VD[rows, :].rearrange("(s p) c -> p s c", p=128), vst[ti][:].rearrange("p s h d -> p s (h d)"),
                      [("vst", ti, sub) for sub in range(4)], [("vst", ti)])
        S.barrier()

        with ExitStack() as es:
            QA = b.sb(es, "QA", [128, 2, S_LEN], BF16)
            KA = b.sb(es, "KA", [128, 2, S_LEN], BF16)
            Vt = b.sb(es, "Vt", [128, NB, 128], BF16)
            bdg = b.sb(es, "bdg", [128, 4, 512], F32)
            posb = b.sb(es, "posb", [128, 4, 64], F32)
            onec = b.sb(es, "onec", [128, 1], BF16)
            identf = b.sb(es, "identf", [128, 128], F32)
            T_ = [b.sb(es, f"T{i}", [128, 512], F32) for i in range(6)]
            P_ = [b.sb(es, f"P{i}", [128, 512], BF16) for i in range(6)]
            Lacc = [b.sb(es, f"Lacc{i}", [128, 512], F32) for i in range(2)]
            Lacd = [b.sb(es, f"Lacd{i}", [128, 512], F32) for i in range(2)]
            Lhi = [b.sb(es, f"Lhi{i}", [128, 512], BF16) for i in range(2)]
            Llo = [b.sb(es, f"Llo{i}", [128, 512], BF16) for i in range(2)]
            OTs = [b.sb(es, f"OTs{i}", [128, 512], F32) for i in range(2)]
            rec = [b.sb(es, f"rec{i}", [128, 4], F32) for i in range(2)]
            On = [b.sb(es, f"On{i}", [128, 4, 128], F32) for i in range(2)]
            aa = b.sb(es, "aa", [128, 4, 128], F32)
            sq = b.sb(es, "sq", [128, 4, 128], F32)
            ssum = b.sb(es, "ssum", [128, 4], F32)
            ost = [b.sb(es, f"ost{i}", [128, 4, 128], BF16) for i in range(2)]
            pz = [b.ps(es, f"pz{i}", [128, 512], F32) for i in range(4)]
            pOT = [b.ps(es, f"pOT{i}", [128, 512], F32) for i in range(2)]
            ptr = b.ps(es, "ptr", [128, 4, 128], F32)
            pl = b.ps(es, "pl", [128, 8], F32)

            b.dma("sp", posb[:], posb_d, [], ["posb"])
            b.dma("sp", identf[:], ident_d, [], ["identf"])
            b.memset("dve", onec[:], 1.0, ["onec"])
            nqc = nq * 512
            for m in range(2):
                b.memset("dve", KA[64:65, m, :], 1.0, [("KA1", m)])
            nout = 0
            for h in range(2 * nhb):
                for m in range(2):
                    b.dma("sp", QA[0:64, m, 0:nqc], QD[h, m * 64:(m + 1) * 64, 0:nqc], [], [("QA", m)])
                    b.dma("pool", QA[64:65, m, 0:nqc], rrow_d[h:h + 1, 0:nqc], [], [("QAr", m)])
                    b.dma("sp", KA[0:64, m, 0:nqc], KD[h, m * 64:(m + 1) * 64, 0:nqc], [], [("KA", m)])
                b.dma("sp", bdg[:], bdiag_d[h].rearrange("j p t -> p j t"), [], ["bdg"])
                for i8 in range(8):
                    if i8 * 1024 >= nqc:
                        continue
                    b.dma("sp", Vt[:, i8 * 8:(i8 + 1) * 8, :], VD[i8 * 1024:(i8 + 1) * 1024, h * 129:h * 129 + 128].rearrange("(n p) d -> p n d", p=128),
                          [], [("Vt", i8)])
                vtk = [("Vt", i8) for i8 in range(8)]
                for qi in range(nq):
                    t0 = qi * 512
                    nkb = 4 * qi + 4
                    def stage_a(step):
                        kb = nkb - 1 - step
                        j = kb - 4 * qi
                        par = step % 2
                        for m in range(2):
                            s_ = m * 2 + par
                            b.mm(pz[s_][:], KA[0:65, m, kb * 128:(kb + 1) * 128], QA[0:65, m, t0:t0 + 512], True, True,
                                 [("KA", m), ("KA1", m), ("QA", m), ("QAr", m)], [("pz", s_)])
                        for m in range(2):
                            s_, s3 = m * 2 + par, m * 3 + step % 3
                            if j >= 0:
                                b.stt("dve", T_[s3][:], pz[s_][:], 0.125, bdg[:, j, :], ALU.mult, ALU.add, [("pz", s_), "bdg"], [("T", s3)])
                            elif not ACT_PSUM:
                                b.copy("dve", T_[s3][:], pz[s_][:], [("pz", s_)], [("T", s3)])
                        for m in range(2):
                            s_, s3 = m * 2 + par, m * 3 + step % 3
                            if j >= 0:
                                b.act(P_[s3][:], T_[s3][:], AF.Exp, [("T", s3)], [("P", s3)])
                            elif ACT_PSUM:
                                off = 4 * qi - kb
                                b.act(P_[s3][:], pz[s_][:], AF.Exp, [("pz", s_), "posb"], [("P", s3)], bias=posb[:, h, off:off + 1], scale=0.125)
                            else:
                                off = 4 * qi - kb
                                b.act(P_[s3][:], T_[s3][:], AF.Exp, [("T", s3), "posb"], [("P", s3)], bias=posb[:, h, off:off + 1], scale=0.125)

                    def stage_b(step):
                        kb = nkb - 1 - step
                        par = step % 2
                        for m in range(2):
                            s_ = m * 3 + step % 3
                            b.mm(pOT[m][:], Vt[:, kb, :], P_[s_][:], step == 0, step == nkb - 1, [("P", s_)] + vtk, [("pOT", m)])
                            if step == 0:
                                b.copy("pool", Lacc[m][:], P_[s_][:], [("P", s_)], [("Lacc", m)])
                            elif step == 1:
                                b.copy("dve", Lacd[m][:], P_[s_][:], [("P", s_)], [("Lacd", m)])
                            elif step % 3 != 0:
                                b.tt("dve", Lacd[m][:], Lacd[m][:], P_[s_][:], ALU.add, [("Lacd", m), ("P", s_)], [("Lacd", m)])
                            else:
                                b.tt("pool", Lacc[m][:], Lacc[m][:], P_[s_][:], ALU.add, [("Lacc", m), ("P", s_)], [("Lacc", m)])

                    stage_a(0)
                    stage_a(1)
                    for step in range(nkb):
                        if step + 2 < nkb:
                            stage_a(step + 2)
                        stage_b(step)
                    oi = nout % 2
                    nout += 1
                    for m in range(2):
                        b.tt("pool", Lacc[m][:], Lacc[m][:], Lacd[m][:], ALU.add, [("Lacc", m), ("Lacd", m)], [("Lacc", m)])
                        b.copy("pool", Lhi[m][:], Lacc[m][:], [("Lacc", m)], [("Lhi", m)])
                        b.tt("pool", Llo[m][:], Lacc[m][:], Lhi[m][:], ALU.subtract, [("Lacc", m), ("Lhi", m)], [("Llo", m)])
                        for sub in range(4):
                            c_ = m * 4 + sub
                            b.mm(pl[:, c_:c_ + 1], Lhi[m][:, sub * 128:(sub + 1) * 128], onec[:], True, False, [("Lhi", m), "onec"], [("pl", c_)])
                            b.mm(pl[:, c_:c_ + 1], Llo[m][:, sub * 128:(sub + 1) * 128], onec[:], False, True, [("Llo", m), "onec"], [("pl", c_)])
                        S.op("dve", lambda e, m=m: e.reciprocal(out=rec[m][:], in_=pl[:, m * 4:(m + 1) * 4]),
                             [("pl", m * 4 + sub) for sub in range(4)], [("rec", m)])
                        b.copy("dve", OTs[m][:], pOT[m][:], [("pOT", m)], [("OTs", m)])
                        for sub in range(4):
                            b.tr(ptr[:, sub, :], OTs[m][:, sub * 128:(sub + 1) * 128], identf[:], [("OTs", m), "identf"], ["ptr"])
                        b.tt("dve", On[m][:], ptr[:], rec[m][:].unsqueeze(2).to_broadcast([128, 4, 128]), ALU.mult, ["ptr", ("rec", m)], [("On", m)])
                    b.stt("dve", aa[:], On[1][:], neglam[:, 0:1], On[0][:], ALU.mult, ALU.add, [("On", 0), ("On", 1), "neglam"], ["aa"])
                    b.tt("pool", sq[:], aa[:], aa[:], ALU.mult, ["aa"], ["sq"])
                    b.red("dve", ssum[:], sq[:], "sum", ["sq"], ["ssum"])
                    b.act(ssum[:], ssum[:], AF.Ln, ["ssum", "epsb"], ["ssum"], bias=epsb[:], scale=1.0 / 128.0)
                    b.act(ssum[:], ssum[:], AF.Exp, ["ssum"], ["ssum"], scale=-0.5)
                    b.tt("dve", aa[:], aa[:], ssum[:].unsqueeze(2).to_broadcast([128, 4, 128]), ALU.mult, ["aa", "ssum"], ["aa"])
                    b.tt("pool", ost[oi][:], aa[:], gsub[:].unsqueeze(1).to_broadcast([128, 4, 128]), ALU.mult, ["aa", "gsub"], [("ost", oi)])
                    b.dma("sp", att[t0:t0 + 512, h * 128:(h + 1) * 128].rearrange("(s p) c -> p s c", p=128), ost[oi][:],
                          [("ost", oi)], [("att", h, qi)])
        outs = ["mod_d"] + [("att", h, qi) for h in range(2 * nhb) for qi in range(nq)]
        if b.fused:
            S.barrier()
            return outs
        return b.finish(outs)


def attn1_consts(hh):
    bd = np.zeros((4, 4, 128, 512), np.float32)
    posb = np.zeros((128, 4, 64), np.float32)
    rrow = np.zeros((4, S_LEN), np.float32)
    s_ = np.arange(128)[:, None].astype(np.float64)
    t_ = np.arange(512)[None, :].astype(np.float64)
    for h in range(4):
        gh = 4 * hh + h
        slope = 2.0 ** (-(gh + 1.0))
        rrow[h] = (-8.0 * slope * (np.arange(S_LEN) % 512)).astype(np.float32)
        for j in range(4):
            s_abs = 128 * j + s_
            allowed = (s_abs // 64) <= (t_ // 64)
            bd[h, j] = np.where(allowed, -slope * np.abs(t_ - s_abs) + slope * t_, -30000.0)
        for off in range(64):
            posb[:, h, off] = slope * (np.arange(128) - 128.0 * off)
    return dict(ident=np.eye(128, dtype=np.float32), bdiag=bd, posb=posb, rrow=rrow)


def _run(nc, in_maps):
    res = run_bass_kernel_spmd(nc, in_maps, core_ids=list(range(NCORES)))
    return res.results


def _post_inputs(l, xfull, att_full, mods, w_out, inp):
    lnp = np.ascontiguousarray(np.stack([inp["ln1_g"][l], inp["ln1_b"][l], inp["ln2_g"][l], inp["ln2_b"][l]]).astype(np.float32))
    wr = np.ascontiguousarray(np.concatenate([inp["moe_w_group"][l], inp["moe_w_router"][l]], axis=1))
    br = np.ascontiguousarray(np.concatenate([inp["moe_b_group"][l], inp["moe_b_router"][l]]))
    cs = post_consts()
    maps = []
    for c in range(NCORES):
        b_, hh = c // 2, c % 2
        rows = slice(hh * TOK, (hh + 1) * TOK)
        d = dict(xs=np.ascontiguousarray(xfull[b_, rows]), att=np.ascontiguousarray(att_full[b_][rows]), mod=mods[b_],
                 w_out=w_out, lnp=lnp, wr=wr, br=br, w1=inp["moe_w1"][l], w3=inp["moe_w3"][l], w2=inp["moe_w2"][l])
        d.update(cs)
        maps.append(d)
    return maps


def kernel_unfused(**inp):
    inp = {k: np.asarray(v) for k, v in inp.items()}
    x = inp["x"]
    Bn = x.shape[0]
    w = inp["even_w_in"][0]
    maps = []
    for c in range(NCORES):
        b_, hh = c // 2, c % 2
        a = slice(hh * 256, (hh + 1) * 256)
        sq, sk, sv = w[:, 0:512], w[:, 512:1024], w[:, 1024:1536]
        rq, rk, rv, rg = w[:, 1536:2048], w[:, 2048:2560], w[:, 2560:3072], w[:, 3072:3584]
        w_in = np.ascontiguousarray(np.concatenate([sq[:, a], sk[:, a], rq[:, a], rk[:, a], sv[:, a], rk[:, a], rv[:, a], rg[:, a]], axis=1))
        d = dict(x=np.ascontiguousarray(x[b_]), c_row=np.ascontiguousarray(inp["c"][b_]), w_ada=inp["w_ada"][0], b_ada=inp["b_ada"][0],
                 w_in=w_in, gn_g=np.ascontiguousarray(inp["ret_gn_g"][0][a]))
        d.update(attn0_consts(hh))
        maps.append(d)
    r = _run(build_attn0(), maps)
    att_full, mods = [], []
    for b_ in range(Bn):
        a0, a1 = np.asarray(r[2 * b_]["att"]), np.asarray(r[2 * b_ + 1]["att"])
        att_full.append(np.concatenate([a0[:, :256], a1[:, :256], a0[:, 256:], a1[:, 256:]], axis=1))
        mods.append(np.asarray(r[2 * b_]["mod"]))
    post_nc = build_post()
    r = _run(post_nc, _post_inputs(0, x, att_full, mods, inp["even_w_out"][0], inp))
    x1 = np.stack([np.concatenate([np.asarray(r[2 * b_]["xo"]), np.asarray(r[2 * b_ + 1]["xo"])], axis=0) for b_ in range(Bn)])
    w = inp["odd_w_in"][0]
    lamv = np.ascontiguousarray(np.stack([inp["lambda_q1"][0], inp["lambda_k1"][0], inp["lambda_q2"][0], inp["lambda_k2"][0]]))
    maps = []
    for c in range(NCORES):
        b_, hh = c // 2, c % 2
        a = slice(hh * 512, (hh + 1) * 512)
        w_in = np.ascontiguousarray(np.concatenate([w[:, 0:1024][:, a], w[:, 1024:2048][:, a], w[:, 2048:3072][:, a]], axis=1))
        d = dict(x=np.ascontiguousarray(x1[b_]), c_row=np.ascontiguousarray(inp["c"][b_]), w_ada=inp["w_ada"][1], b_ada=inp["b_ada"][1],
                 w_in=w_in, lamv=lamv, subg=np.ascontiguousarray(inp["diff_subln_g"][0]))
        d.update(attn1_consts(hh))
        maps.append(d)
    r = _run(build_attn1(), maps)
    att_full, mods = [], []
    for b_ in range(Bn):
        att_full.append(np.concatenate([np.asarray(r[2 * b_]["att"]), np.asarray(r[2 * b_ + 1]["att"])], axis=1))
        mods.append(np.asarray(r[2 * b_]["mod"]))
    r = _run(build_post(), _post_inputs(1, x1, att_full, mods, inp["odd_w_out"][0], inp))
    out = np.stack([np.concatenate([np.asarray(r[2 * b_]["xo"]), np.asarray(r[2 * b_ + 1]["xo"])], axis=0) for b_ in range(Bn)])
    return out.astype(np.float32)


PAIRS = [[0, 1], [2, 3], [4, 5], [6, 7]]


def build_fused():
    b = B()
    b.fused = True
    b.no_act_copy = True
    S = b.S
    G0 = b.dscr("G0", [2 * S_LEN, 512], BF16)
    G1 = b.dscr("G1", [2 * S_LEN, 512], BF16)
    X1h = b.dscr("X1h", [TOK, D], F32)
    X1f = b.dscr("X1f", [S_LEN, D], F32)
    gidx = b.nc.dram_tensor("gidx", [128, 2, NT], I32, kind="ExternalInput").ap()
    out = b.nc.dram_tensor("out", [TOK, D], F32, kind="ExternalOutput").ap()

    agn = [0]

    def allgather(src, dst, R, C, dt, esz):
        rows = (2 * 1024 * 1024) // (C * esz)
        nch = R // rows
        agn[0] += 1
        ss = [b.nc.dram_tensor(f"ag{agn[0]}_s{i}", [rows, C], dt, kind="Internal").ap() for i in range(nch)]
        gg = [b.nc.dram_tensor(f"ag{agn[0]}_g{i}", [2 * rows, C], dt, kind="Internal").ap() for i in range(nch)]
        for i in range(nch):
            b.dma("sp", ss[i][:, :], src[i * rows:(i + 1) * rows, :], [], [])
        S.barrier()
        for i in range(nch):
            S.cc(lambda e, i=i: e.collective_compute("AllGather", ALU.bypass, replica_groups=PAIRS, ins=[ss[i][:, :]], outs=[gg[i][:, :]]), [], [])
        S.barrier()
        for i in range(nch):
            for r in range(2):
                b.dma("sp", dst[r * R + i * rows:r * R + (i + 1) * rows, :], gg[i][r * rows:(r + 1) * rows, :], [], [])
        S.barrier()

    b.pfx, b.ovr = "a0_", {}
    build_attn0(b=b)
    att0, mod0 = b.shared["att"], b.shared["modS"]
    allgather(att0, G0, S_LEN, 512, BF16, 2)
    b.pfx, b.ovr = "p0_", {"mod": mod0, "xo": X1h}
    build_post(b=b, att_gather=(G0, gidx))
    allgather(X1h, X1f, TOK, D, F32, 4)
    b.pfx, b.ovr = "a1_", {"x": X1f}
    build_attn1(b=b)
    att1, mod1 = b.shared["att"], b.shared["modS"]
    allgather(att1, G1, S_LEN, 512, BF16, 2)
    b.pfx, b.ovr = "p1_", {"mod": mod1, "xs": X1h, "xo": out}
    build_post(b=b, att_gather=(G1, gidx))
    return b.finish([])


def kernel(**inp):
    inp = {k: np.asarray(v) for k, v in inp.items()}
    x = inp["x"]
    Bn = x.shape[0]
    w0 = inp["even_w_in"][0]
    w1_ = inp["odd_w_in"][0]
    lamv = np.ascontiguousarray(np.stack([inp["lambda_q1"][0], inp["lambda_k1"][0], inp["lambda_q2"][0], inp["lambda_k2"][0]]))
    wo0 = inp["even_w_out"][0]
    wo0p = np.ascontiguousarray(np.concatenate([wo0[0:256], wo0[512:768], wo0[256:512], wo0[768:1024]], axis=0))
    pcs = post_consts()

    def post_in(l, w_out):
        return dict(w_out=w_out,
                    lnp=np.ascontiguousarray(np.stack([inp["ln1_g"][l], inp["ln1_b"][l], inp["ln2_g"][l], inp["ln2_b"][l]]).astype(np.float32)),
                    wr=np.ascontiguousarray(np.concatenate([inp["moe_w_group"][l], inp["moe_w_router"][l]], axis=1)),
                    br=np.ascontiguousarray(np.concatenate([inp["moe_b_group"][l], inp["moe_b_router"][l]])),
                    w1=inp["moe_w1"][l], w3=inp["moe_w3"][l], w2=inp["moe_w2"][l], tri=pcs["tri"], eoff=pcs["eoff"])
    p0, p1 = post_in(0, wo0p), post_in(1, inp["odd_w_out"][0])
    maps = []
    for c in range(NCORES):
        b_, hh = c // 2, c % 2
        a = slice(hh * 256, (hh + 1) * 256)
        sq, sk, sv = w0[:, 0:512], w0[:, 512:1024], w0[:, 1024:1536]
        rq, rk, rv, rg = w0[:, 1536:2048], w0[:, 2048:2560], w0[:, 2560:3072], w0[:, 3072:3584]
        w_in0 = np.ascontiguousarray(np.concatenate([sq[:, a], sk[:, a], rq[:, a], rk[:, a], sv[:, a], rk[:, a], rv[:, a], rg[:, a]], axis=1))
        a2 = slice(hh * 512, (hh + 1) * 512)
        w_in1 = np.ascontiguousarray(np.concatenate([w1_[:, 0:1024][:, a2], w1_[:, 1024:2048][:, a2], w1_[:, 2048:3072][:, a2]], axis=1))
        c0 = attn0_consts(hh)
        c1 = attn1_consts(hh)
        p_ = np.arange(128)[:, None, None]
        r_ = np.arange(2)[None, :, None]
        t_ = np.arange(NT)[None, None, :]
        gidx = (r_ * S_LEN + hh * TOK + t_ * 128 + p_).astype(np.int32)
        d = {"ident": c0["ident"], "gidx": np.ascontiguousarray(gidx)}
        d.update({"a0_x": np.ascontiguousarray(x[b_]), "a0_c_row": np.ascontiguousarray(inp["c"][b_]), "a0_w_ada": inp["w_ada"][0],
                  "a0_b_ada": inp["b_ada"][0], "a0_w_in": w_in0, "a0_gn_g": np.ascontiguousarray(inp["ret_gn_g"][0][a])})
        d.update({"a0_" + k: v for k, v in c0.items() if k != "ident"})
        d.update({"p0_xs": np.ascontiguousarray(x[b_, hh * TOK:(hh + 1) * TOK])})
        d.update({"p0_" + k: v for k, v in p0.items()})
        d.update({"a1_c_row": np.ascontiguousarray(inp["c"][b_]), "a1_w_ada": inp["w_ada"][1], "a1_b_ada": inp["b_ada"][1],
                  "a1_w_in": w_in1, "a1_lamv": lamv, "a1_subg": np.ascontiguousarray(inp["diff_subln_g"][0])})
        d.update({"a1_" + k: v for k, v in c1.items() if k != "ident"})
        d.update({"p1_" + k: v for k, v in p1.items()})
        maps.append(d)
    r = _run(build_fused(), maps)
    out = np.stack([np.concatenate([np.asarray(r[2 * b_]["out"]), np.asarray(r[2 * b_ + 1]["out"])], axis=0) for b_ in range(Bn)])
    return out.astype(np.float32)
```
